# Optimizing a Trainium2 kernel written in Bass

```python
import math
import jax, jax.numpy as jnp
from jax import lax
import numpy as np

D_MODEL = 1024
BATCH = 8
SEQ = 4096
DEPTH = 2

HEAD_DIM = 64
ATTN_HEADS = 8
ATTN_WIDTH = ATTN_HEADS * HEAD_DIM
DILATED_PATTERNS = ((128, 1), (512, 4), (2048, 16))
ATTN_BLOCK = 128
ROPE_THETA = 10000.0

SSD_HEADS = 8
SSD_HEAD_DIM = 64
SSD_WIDTH = SSD_HEADS * SSD_HEAD_DIM
SSD_GROUPS = 2
SSD_STATE = 128
SSD_CONV = 4
SSD_CHUNK = 128
SSD_CONV_DIM = SSD_WIDTH + 2 * SSD_GROUPS * SSD_STATE

SC_WIDTH = 512
SC_CONV = 3

MIX_WIDTH = ATTN_WIDTH + SSD_WIDTH + SC_WIDTH
IN_SPLITS = (ATTN_WIDTH, ATTN_WIDTH, ATTN_WIDTH,
             SSD_WIDTH, SSD_WIDTH, SSD_GROUPS * SSD_STATE, SSD_GROUPS * SSD_STATE, SSD_HEADS,
             SC_WIDTH, SC_WIDTH, SC_WIDTH)
IN_WIDTH = sum(IN_SPLITS)

N_EXPERTS = 32
TOP_K = 4
EXPERT_FF = D_MODEL
SWIGLU_ALPHA = 1.702
SWIGLU_LIMIT = 7.0
MOE_BLOCK = 128

ALPHA = (2.0 * DEPTH) ** 0.25
BETA = (8.0 * DEPTH) ** -0.25
LN_EPS = 1e-5
RMS_EPS = 1e-5

kernel_name = "hybrid_dilated_ssd_shortconv_moe_deepnorm"


def layer_norm(x, g, b):
    xf = x.astype(jnp.float32)
    mu = jnp.mean(xf, -1, keepdims=True)
    var = jnp.mean(jnp.square(xf - mu), -1, keepdims=True)
    return ((xf - mu) * lax.rsqrt(var + LN_EPS) * g + b).astype(x.dtype)


def rotary(x, pos):
    half = x.shape[-1] // 2
    inv = ROPE_THETA ** (-jnp.arange(half, dtype=jnp.float32) / half)
    ang = pos.astype(jnp.float32)[:, None] * inv[None, :]
    cos = jnp.cos(ang)[None, :, None, :]
    sin = jnp.sin(ang)[None, :, None, :]
    x1, x2 = x[..., :half], x[..., half:]
    return jnp.concatenate([x1 * cos - x2 * sin, x2 * cos + x1 * sin], -1).astype(x.dtype)


def causal_depthwise_conv(x, w):
    width = w.shape[0]
    return lax.conv_general_dilated(
        x, w.astype(x.dtype)[:, None, :], window_strides=(1,), padding=[(width - 1, 0)],
        dimension_numbers=("NWC", "WIO", "NWC"), feature_group_count=x.shape[-1])


def dilated_window_attention(q, k, v, dilation, span):
    B, S, H, hd = q.shape
    L = S // dilation
    nb = L // ATTN_BLOCK

    def strided(t):
        return (t.reshape(B, L, dilation, H, hd).transpose(0, 2, 1, 3, 4)
                .reshape(B * dilation, nb, ATTN_BLOCK, H, hd))

    def with_prev(t):
        prev = jnp.pad(t[:, :-1], ((0, 0), (1, 0), (0, 0), (0, 0), (0, 0)))
        return jnp.concatenate([prev, t], axis=2)

    qb = strided(q)
    kk = with_prev(strided(k))
    vv = with_prev(strided(v))
    s = jnp.einsum("bnqhd,bnkhd->bnhqk", qb, kk).astype(jnp.float32) * (hd ** -0.5)
    qi = jnp.arange(ATTN_BLOCK)[:, None]
    kj = jnp.arange(2 * ATTN_BLOCK)[None, :]
    dist = qi + ATTN_BLOCK - kj
    band = (dist >= 0) & (dist <= span)
    has_prev = (jnp.arange(nb)[:, None, None] > 0) | (kj >= ATTN_BLOCK)[None]
    valid = band[None] & has_prev
    s = jnp.where(valid[None, :, None], s, -jnp.inf)
    m = jnp.max(s, -1, keepdims=True)
    p = jnp.exp(s - m)
    l = jnp.sum(p, -1, keepdims=True)
    o = jnp.einsum("bnhqk,bnkhd->bnqhd", p, vv.astype(jnp.float32)) / l.transpose(0, 1, 3, 2, 4)
    lse = (m + jnp.log(l))[..., 0].transpose(0, 1, 3, 2)
    o = o.reshape(B, dilation, L, H, hd).transpose(0, 2, 1, 3, 4).reshape(B, S, H, hd)
    lse = lse.reshape(B, dilation, L, H).transpose(0, 2, 1, 3).reshape(B, S, H)
    return o, lse


def dilated_mixture_attention(q, k, v):
    B, S, H, hd = q.shape
    mult = ATTN_BLOCK * max(d for _, d in DILATED_PATTERNS)
    s_pad = -(-S // mult) * mult
    padw = ((0, 0), (0, s_pad - S), (0, 0), (0, 0))
    qp, kp, vp = jnp.pad(q, padw), jnp.pad(k, padw), jnp.pad(v, padw)
    outs, lses = [], []
    for window, dilation in DILATED_PATTERNS:
        o, lse = dilated_window_attention(qp, kp, vp, dilation, window // dilation)
        outs.append(o)
        lses.append(lse)
    w = jax.nn.softmax(jnp.stack(lses, 0), axis=0)
    o = jnp.sum(w[..., None] * jnp.stack(outs, 0), axis=0)
    return o[:, :S].astype(q.dtype)


def ssd_chunked(x, dt, a, bm, cm):
    B, S, H, P = x.shape
    N = bm.shape[-1]
    nc = S // SSD_CHUNK
    X = (x * dt[..., None]).reshape(B, nc, SSD_CHUNK, H, P)
    A = (dt * a).reshape(B, nc, SSD_CHUNK, H).transpose(0, 3, 1, 2)
    Bc = bm.reshape(B, nc, SSD_CHUNK, H, N)
    Cc = cm.reshape(B, nc, SSD_CHUNK, H, N)
    a_cum = jnp.cumsum(A, axis=-1)
    seg = a_cum[..., :, None] - a_cum[..., None, :]
    causal = jnp.tril(jnp.ones((SSD_CHUNK, SSD_CHUNK), dtype=bool))
    Lmat = jnp.exp(jnp.where(causal, seg, -jnp.inf))
    scores = jnp.einsum("bclhn,bcshn->bhcls", Cc, Bc) * Lmat
    y_diag = jnp.einsum("bhcls,bcshp->bclhp", scores, X)
    decay_states = jnp.exp(a_cum[..., -1:] - a_cum)
    states = jnp.einsum("bclhn,bhcl,bclhp->bchpn", Bc, decay_states, X)
    chunk_decay = jnp.exp(a_cum[..., -1])

    def step(h, inp):
        st, dec = inp
        return h * dec[..., None, None] + st, h

    h0 = jnp.zeros((B, H, P, N), jnp.float32)
    _, h_in = lax.scan(step, h0, (states.transpose(1, 0, 2, 3, 4), chunk_decay.transpose(2, 0, 1)))
    h_in = h_in.transpose(1, 0, 2, 3, 4)
    y_off = jnp.einsum("bclhn,bchpn,bhcl->bclhp", Cc, h_in, jnp.exp(a_cum))
    return (y_diag + y_off).reshape(B, S, H, P)


def mamba2_mixer(z, xs, bs, cs, dt_raw, conv_w, conv_b, dt_bias, a_log, d_skip, norm_g):
    B, S, _ = xs.shape
    xbc = causal_depthwise_conv(jnp.concatenate([xs, bs, cs], -1), conv_w) + conv_b.astype(xs.dtype)
    xbc = jax.nn.silu(xbc.astype(jnp.float32))
    gn = SSD_GROUPS * SSD_STATE
    xh = xbc[..., :SSD_WIDTH].reshape(B, S, SSD_HEADS, SSD_HEAD_DIM)
    rep = SSD_HEADS // SSD_GROUPS
    bm = jnp.repeat(xbc[..., SSD_WIDTH:SSD_WIDTH + gn].reshape(B, S, SSD_GROUPS, SSD_STATE), rep, axis=2)
    cm = jnp.repeat(xbc[..., SSD_WIDTH + gn:].reshape(B, S, SSD_GROUPS, SSD_STATE), rep, axis=2)
    dt = jax.nn.softplus(dt_raw.astype(jnp.float32) + dt_bias.astype(jnp.float32))
    a = -jnp.exp(a_log.astype(jnp.float32))
    y = ssd_chunked(xh, dt, a, bm, cm) + d_skip.astype(jnp.float32)[:, None] * xh
    y = y.reshape(B, S, SSD_WIDTH) * jax.nn.silu(z.astype(jnp.float32))
    yg = y.reshape(B, S, SSD_GROUPS, SSD_WIDTH // SSD_GROUPS)
    yg = yg * lax.rsqrt(jnp.mean(jnp.square(yg), -1, keepdims=True) + RMS_EPS)
    return (yg.reshape(B, S, SSD_WIDTH) * norm_g).astype(xs.dtype)


def short_gated_conv(b_gate, c_gate, h, conv_w):
    return b_gate * causal_depthwise_conv(c_gate * h, conv_w)


def mixing_sublayer(x, w_in, ssd_conv_w, ssd_conv_b, ssd_dt_bias, ssd_a_log, ssd_d, ssd_norm_g,
                    sc_conv_w, w_out):
    B, S, _ = x.shape
    proj = x @ w_in.astype(x.dtype)
    cuts = np.cumsum(IN_SPLITS)[:-1].tolist()
    q, k, v, z, xs, bs, cs, dt_raw, sb, sc, sh = jnp.split(proj, cuts, axis=-1)
    pos = jnp.arange(S)
    q = rotary(q.reshape(B, S, ATTN_HEADS, HEAD_DIM), pos)
    k = rotary(k.reshape(B, S, ATTN_HEADS, HEAD_DIM), pos)
    v = v.reshape(B, S, ATTN_HEADS, HEAD_DIM)
    attn = dilated_mixture_attention(q, k, v).reshape(B, S, ATTN_WIDTH)
    ssd = mamba2_mixer(z, xs, bs, cs, dt_raw, ssd_conv_w, ssd_conv_b, ssd_dt_bias, ssd_a_log,
                       ssd_d, ssd_norm_g)
    conv = short_gated_conv(sb, sc, sh, sc_conv_w)
    mixed = jnp.concatenate([attn.astype(x.dtype), ssd, conv.astype(x.dtype)], -1)
    return mixed @ w_out.astype(x.dtype)


def moe_sublayer(x, router_w, router_b, w_gu, b_gu, w_down, b_down):
    Bsz, S, D = x.shape
    N = Bsz * S
    xt = x.reshape(N, D)
    logits = (xt @ router_w.astype(x.dtype) + router_b.astype(x.dtype)).astype(jnp.float32)
    top_val, top_idx = lax.top_k(logits, TOP_K)
    gates = jax.nn.softmax(top_val, axis=-1)
    e_flat = top_idx.reshape(-1).astype(jnp.int32)
    tok_flat = jnp.arange(N * TOP_K, dtype=jnp.int32) // TOP_K
    g_flat = gates.reshape(-1)
    order = jnp.argsort(e_flat)
    e_sorted = e_flat[order]
    counts = jnp.zeros((N_EXPERTS,), jnp.int32).at[e_flat].add(1)
    padded = (counts + MOE_BLOCK - 1) // MOE_BLOCK * MOE_BLOCK
    start = jnp.cumsum(counts) - counts
    pend = jnp.cumsum(padded)
    pstart = pend - padded
    dest = pstart[e_sorted] + (jnp.arange(N * TOP_K, dtype=jnp.int32) - start[e_sorted])
    P = N * TOP_K + N_EXPERTS * MOE_BLOCK
    nblk = P // MOE_BLOCK
    buf_tok = jnp.full((P,), N, jnp.int32).at[dest].set(tok_flat[order])
    buf_gate = jnp.zeros((P,), jnp.float32).at[dest].set(g_flat[order])
    blk_expert = jnp.minimum(
        jnp.searchsorted(pend, jnp.arange(nblk, dtype=jnp.int32) * MOE_BLOCK, side="right"),
        N_EXPERTS - 1).astype(jnp.int32)
    x_pad = jnp.concatenate([xt, jnp.zeros((1, D), xt.dtype)], 0)

    def expert_block(args):
        tok, e = args
        xb = x_pad[tok]
        gu = xb @ w_gu[e].astype(xb.dtype) + b_gu[e].astype(xb.dtype)
        gate = jnp.minimum(gu[:, :EXPERT_FF], SWIGLU_LIMIT)
        up = jnp.clip(gu[:, EXPERT_FF:], -SWIGLU_LIMIT, SWIGLU_LIMIT)
        h = (up + 1.0) * gate * jax.nn.sigmoid(SWIGLU_ALPHA * gate)
        return h @ w_down[e].astype(xb.dtype) + b_down[e].astype(xb.dtype)

    y = lax.map(expert_block, (buf_tok.reshape(nblk, MOE_BLOCK), blk_expert))
    y = y.reshape(P, D) * buf_gate[:, None].astype(x.dtype)
    out = jnp.zeros((N + 1, D), x.dtype).at[buf_tok].add(y)
    return out[:N].reshape(Bsz, S, D)


def setup_inputs(seed: int = 0) -> dict:
    key = jax.random.key(seed)
    ks = jax.random.split(key, 20)
    f32 = jnp.float32
    L = DEPTH

    def nrm(k, shape, scale):
        return jax.random.normal(k, shape, f32) * scale

    x = nrm(ks[0], (BATCH, SEQ, D_MODEL), 1.0)
    w_in = nrm(ks[1], (L, D_MODEL, IN_WIDTH), D_MODEL ** -0.5)
    w_in = w_in.at[:, :, 2 * ATTN_WIDTH:3 * ATTN_WIDTH].multiply(BETA)
    ssd_conv_w = nrm(ks[2], (L, SSD_CONV, SSD_CONV_DIM), SSD_CONV ** -0.5)
    ssd_conv_b = nrm(ks[3], (L, SSD_CONV_DIM), 0.02)
    dt0 = jnp.exp(jax.random.uniform(ks[4], (L, SSD_HEADS), f32, math.log(1e-3), math.log(1e-1)))
    ssd_dt_bias = dt0 + jnp.log(-jnp.expm1(-dt0))
    ssd_a_log = jnp.log(jax.random.uniform(ks[5], (L, SSD_HEADS), f32, 1.0, 16.0))
    ssd_d = 1.0 + nrm(ks[6], (L, SSD_HEADS), 0.1)
    ssd_norm_g = 1.0 + nrm(ks[7], (L, SSD_WIDTH), 0.05)
    sc_conv_w = nrm(ks[8], (L, SC_CONV, SC_WIDTH), SC_CONV ** -0.5)
    w_out = nrm(ks[9], (L, MIX_WIDTH, D_MODEL), MIX_WIDTH ** -0.5 * BETA)
    ln1_g = 1.0 + nrm(ks[10], (L, D_MODEL), 0.05)
    ln1_b = nrm(ks[11], (L, D_MODEL), 0.02)
    router_w = nrm(ks[12], (L, D_MODEL, N_EXPERTS), D_MODEL ** -0.5)
    router_b = nrm(ks[13], (L, N_EXPERTS), 0.01)
    exp_w_gu = nrm(ks[14], (L, N_EXPERTS, D_MODEL, 2 * EXPERT_FF), D_MODEL ** -0.5)
    exp_b_gu = nrm(ks[15], (L, N_EXPERTS, 2 * EXPERT_FF), 0.01)
    exp_w_down = nrm(ks[16], (L, N_EXPERTS, EXPERT_FF, D_MODEL), EXPERT_FF ** -0.5 * BETA)
    exp_b_down = nrm(ks[17], (L, N_EXPERTS, D_MODEL), 0.01)
    ln2_g = 1.0 + nrm(ks[18], (L, D_MODEL), 0.05)
    ln2_b = nrm(ks[19], (L, D_MODEL), 0.02)
    return {"x": x, "w_in": w_in, "ssd_conv_w": ssd_conv_w, "ssd_conv_b": ssd_conv_b,
            "ssd_dt_bias": ssd_dt_bias, "ssd_a_log": ssd_a_log, "ssd_d": ssd_d,
            "ssd_norm_g": ssd_norm_g, "sc_conv_w": sc_conv_w, "w_out": w_out,
            "ln1_g": ln1_g, "ln1_b": ln1_b, "router_w": router_w, "router_b": router_b,
            "exp_w_gu": exp_w_gu, "exp_b_gu": exp_b_gu, "exp_w_down": exp_w_down,
            "exp_b_down": exp_b_down, "ln2_g": ln2_g, "ln2_b": ln2_b}


def reference(x, w_in, ssd_conv_w, ssd_conv_b, ssd_dt_bias, ssd_a_log, ssd_d, ssd_norm_g,
              sc_conv_w, w_out, ln1_g, ln1_b, router_w, router_b, exp_w_gu, exp_b_gu,
              exp_w_down, exp_b_down, ln2_g, ln2_b):
    for layer in range(DEPTH):
        mix = mixing_sublayer(x, w_in[layer], ssd_conv_w[layer], ssd_conv_b[layer],
                              ssd_dt_bias[layer], ssd_a_log[layer], ssd_d[layer],
                              ssd_norm_g[layer], sc_conv_w[layer], w_out[layer])
        x = layer_norm(ALPHA * x + mix, ln1_g[layer], ln1_b[layer])
        ffn = moe_sublayer(x, router_w[layer], router_b[layer], exp_w_gu[layer], exp_b_gu[layer],
                           exp_w_down[layer], exp_b_down[layer])
        x = layer_norm(ALPHA * x + ffn, ln2_g[layer], ln2_b[layer])
    return x
```

```python
import contextlib
import numpy as np
import ml_dtypes
import concourse.bass as bass
import concourse.mybir as mybir
from concourse.bass_utils import run_bass_kernel_spmd

F32 = mybir.dt.float32
BF16 = mybir.dt.bfloat16
I32 = mybir.dt.int32
U32 = mybir.dt.uint32
AF = mybir.ActivationFunctionType
ALU = mybir.AluOpType
AX = mybir.AxisListType

S = 4096
D = 1024
L = 2
NT = S // 128
INW = 4616
NE = 32
CAP = 768
NSLOT = NE * CAP
XGW = 1024
ALPHA = (2.0 * L) ** 0.25
LN_EPS = 1e-5
RMS_EPS = 1e-5
PATTERNS = (1, 4, 16)

C_Q, C_K, C_V, C_Z, C_XS, C_B, C_C, C_DT, C_SB, C_SC, C_SH = (
    0, 512, 1024, 1536, 2048, 2560, 2816, 3072, 3080, 3592, 4104)

PP_CW, PP_CB, PP_SCW, PP_BGU = 0, 32, 40, 52
PPW = 52 + NE * 16
BC_DTB, BC_ALOG, BC_D, BC_NG, BC_RB, BC_L1G, BC_L1B, BC_L2G, BC_L2B = (
    0, 8, 16, 24, 536, 568, 1592, 2616, 3640)
BCW = 4664


class Sched:
    def __init__(self, nc, n_dma_sems=40):
        self.nc = nc
        self.h = {"pe": nc.tensor, "act": nc.scalar, "dve": nc.vector, "pool": nc.gpsimd, "sp": nc.sync}
        self.sem = {k: nc.alloc_semaphore("prog_" + k) for k in self.h}
        self.cnt = {k: 0 for k in self.h}
        self.seen = {k: {} for k in self.h}
        self.dsem = [nc.alloc_semaphore("dma%d" % i) for i in range(n_dma_sems)]
        self.dval = [0] * n_dma_sems
        self.dpool = {"sp": list(range(0, 16)), "pool": list(range(16, 32)), "act": list(range(32, n_dma_sems))}
        self.drr = {"sp": 0, "pool": 0, "act": 0}
        self.res = {}

    def _wait(self, eng, tok):
        kind, key, val = tok
        if kind == "e" and key == eng:
            return
        sk = (kind, key)
        if self.seen[eng].get(sk, 0) >= val:
            return
        sem = self.sem[key] if kind == "e" else self.dsem[key]
        self.h[eng].wait_ge(sem, val)
        self.seen[eng][sk] = val

    def _deps(self, eng, r, w):
        for k in r:
            st = self.res.get(k)
            if st and st[0] is not None:
                tok = st[0]
                if tok[0] == "e" and tok[1] == eng:
                    if tok[2] > self.cnt[eng] - 2 and eng != "pe":
                        sk = ("e", eng)
                        if self.seen[eng].get(sk, 0) < tok[2]:
                            self.h[eng].wait_ge(self.sem[eng], tok[2])
                            self.seen[eng][sk] = tok[2]
                else:
                    self._wait(eng, tok)
        for k in w:
            st = self.res.get(k)
            if st:
                if st[0] is not None:
                    self._wait(eng, st[0])
                for tok in st[1]:
                    self._wait(eng, tok)

    def _commit(self, tok, r, w):
        for k in r:
            st = self.res.setdefault(k, [None, []])
            st[1] = [t for t in st[1] if (t[0], t[1]) != (tok[0], tok[1])] + [tok]
        for k in w:
            self.res[k] = [tok, []]

    def op(self, eng, fn, r=(), w=()):
        self._deps(eng, r, w)
        ins = fn()
        self.cnt[eng] += 1
        ins.then_inc(self.sem[eng], 1)
        self._commit(("e", eng, self.cnt[eng]), r, w)
        return ins

    def dma(self, q, out, in_, r=(), w=(), indirect=None):
        pl = self.dpool[q]
        i = pl[self.drr[q] % len(pl)]
        self.drr[q] += 1
        if self.dval[i] > 0:
            self._wait(q, ("d", i, self.dval[i]))
        self._deps(q, r, w)
        if indirect is None:
            ins = self.h[q].dma_start(out=out, in_=in_)
        else:
            ins = self.nc.gpsimd.indirect_dma_start(out=out, in_=in_, **indirect)
        self.dval[i] += 16
        ins.then_inc(self.dsem[i], 16)
        self._commit(("d", i, self.dval[i]), r, w)
        return ins

    def barrier(self):
        for e in self.h:
            for o in self.h:
                if o != e and self.cnt[o] > 0:
                    self._wait(e, ("e", o, self.cnt[o]))
            for i, v in enumerate(self.dval):
                if v > 0:
                    self._wait(e, ("d", i, v))
        self.res = {}


class Builder:
    def __init__(self, debug=(), nlayers=L, stop_after=None, with_moe=True):
        self.debug = set(debug)
        self.with_moe = with_moe
        self.nlayers = nlayers
        self.stop_after = stop_after
        self.nc = nc = bass.Bass("TRN2", target_bir_lowering=False)
        self.K = Sched(nc)
        self.uid = 0
        ein = lambda n, s, d: nc.dram_tensor(n, s, d, kind="ExternalInput").ap()
        self.x = ein("x", [S, D], F32)
        self.w_in = ein("w_in", [L, D, INW], F32)
        self.w_qkp = ein("w_qkp", [L, D, 1024], F32)
        self.w_out = ein("w_out", [L, 1536, D], F32)
        self.router_w = ein("router_w", [L, D, NE], F32)
        if with_moe:
            self.w_gu = ein("exp_w_gu", [L, NE, D, 2 * D], F32)
            self.w_dn = ein("exp_w_down", [L, NE, D, D], F32)
            self.b_dn = ein("exp_b_down", [L, NE, D], F32)
        self.pp = ein("pp", [L, 128, PPW], F32)
        self.bc = ein("bc", [L, BCW], F32)
        self.cos_t = ein("cos_t", [128, S], F32)
        self.sin_t = ein("sin_t", [128, S], F32)
        self.cst_bf = ein("cst_bf", [128, 1024], BF16)
        self.cst_f = ein("cst_f", [128, 1024], F32)
        self.y = nc.dram_tensor("y", [S, D], F32, kind="ExternalOutput").ap()
        self.qT = self.scr("qT", [4, 128, S], BF16)
        self.kT = self.scr("kT", [4, 128, S], BF16)
        self.v_s = self.scr("v_s", [S, 8 * 66], BF16)
        self.z_s = self.scr("z_s", [S, 512], F32)
        self.dt_s = self.scr("dt_s", [S, 8], F32)
        self.xbc = self.scr("xbc", [8, 128, S], BF16)
        self.mixT = self.scr("mixT", [12, 128, S], BF16)
        self.o_s = self.scr("o_s", [3, S, 8 * 65], F32)
        self.x1 = self.scr("x1", [S, D], F32)
        self.xg = self.scr("xg", [NSLOT, XGW], BF16)
        self.yb = self.scr("yb", [NSLOT, D], F32)
        self.offs = self.scr("offs", [S, 4], I32)
        self.gate_s = self.scr("gate_s", [S, 4], F32)
        self.xres = self.scr("xres", [S, D], F32)

    def scr(self, name, shape, dt):
        kind = "ExternalOutput" if name in self.debug else "Internal"
        return self.nc.dram_tensor(name, shape, dt, kind=kind).ap()

    def sb(self, es, name, shape, dt):
        self.uid += 1
        return es.enter_context(self.nc.sbuf_tensor("%s_%d" % (name, self.uid), shape, dt))

    def ps(self, es, name, shape, dt):
        self.uid += 1
        return es.enter_context(self.nc.psum_tensor("%s_%d" % (name, self.uid), shape, dt))

    def build(self):
        nc, K = self.nc, self.K
        with contextlib.ExitStack() as es:
            self.cbf = self.sb(es, "cbf", [128, 1024], BF16)
            self.cf = self.sb(es, "cf", [128, 1024], F32)
            K.dma("sp", self.cbf[:], self.cst_bf[:, :], w=["cbf"])
            K.dma("sp", self.cf[:], self.cst_f[:, :], w=["cf"])
            self.ident = self.cbf[:, 0:128]
            self.maskpc = self.cbf[:, 128:384]
            self.ones_bf = self.cbf[:, 384:512]
            self.sl_bf = self.cbf[:, 512:640]
            self.maskcp = self.cbf[:, 640:896]
            self.U_f = self.cf[:, 0:128]
            self.SL_f = self.cf[:, 128:256]
            self.ones_f = self.cf[:, 256:384]
            self.mls_f = self.cf[:, 384:512]
            self.iota_e = self.cf[:, 512:544]
            self.zr_f = self.cf[:, 800:928]
            for l in range(self.nlayers):
                src = self.x if l == 0 else self.xres
                dst = self.y if l == self.nlayers - 1 else self.xres
                self.layer(l, src, dst)
                if self.stop_after is not None:
                    break
            K.barrier()
        return nc

    def layer(self, l, src, dst):
        K = self.K
        phases = [self.phase_proj, self.phase_attn, self.phase_ssd, self.phase_outln,
                  self.phase_route, self.phase_experts, self.phase_combine]
        for i, ph in enumerate(phases):
            ph(l, src, dst)
            K.barrier()
            if self.stop_after is not None and i >= self.stop_after:
                if getattr(self, "exp_es", None) is not None and 3 <= i < 5:
                    self.exp_es.close()
                if i == 1:
                    self.ssd_es.close()
                return

    def phase_proj(self, l, src, dst):
        nc, K = self.nc, self.K
        with contextlib.ExitStack() as es:
            xT = self.sb(es, "xT", [128, 8, S], BF16)
            ppt = self.sb(es, "ppt", [128, 52], F32)
            bct = self.sb(es, "bct", [128, 8], F32)
            K.dma("sp", ppt[:], self.pp[l, :, 0:52], w=["ppt"])
            K.dma("sp", bct[:], self.bc[l, BC_DTB:BC_DTB + 8].partition_broadcast(128), w=["bct"])
            sup_src = [("q", self.w_in[l, :, C_Q:C_Q + 512]), ("qp", self.w_qkp[l, :, 0:512]),
                       ("k", self.w_in[l, :, C_K:C_K + 512]), ("kp", self.w_qkp[l, :, 512:1024]),
                       ("xs0", self.w_in[l, :, C_XS:C_XS + 512]), ("xs1", self.w_in[l, :, C_XS + 512:C_XS + 1024]),
                       ("sc", self.w_in[l, :, C_SC:C_SC + 512]), ("sh", self.w_in[l, :, C_SH:C_SH + 512]),
                       ("sb", self.w_in[l, :, C_SB:C_SB + 512])]
            sup = {}
            for nm, _ in sup_src:
                sup[nm] = self.sb(es, "sup_" + nm, [128, 8, 512], BF16)
            sup_when = {6: ["q", "qp"], 12: ["k", "kp"], 18: ["xs0", "xs1"], 24: ["sc", "sh", "sb"]}
            sup_ap = dict(sup_src)

            def load_sup(nm):
                K.dma("pool", sup[nm][:], sup_ap[nm].rearrange("(k p) c -> p k c", p=128), w=[("sup", nm)])

            with contextlib.ExitStack() as es1:
                xtb = [self.sb(es1, "xtb", [128, D], BF16) for _ in range(3)]
                tp = [self.ps(es1, "tp", [128, 8, 128], BF16) for _ in range(2)]
                for t in range(NT):
                    xb = xtb[t % 3]
                    K.dma("pool", xb[:], src[t * 128:(t + 1) * 128, :], w=[("xtb", t % 3)])
                    for nm in sup_when.get(t, []):
                        load_sup(nm)
                    p = tp[t % 2]
                    for kc in range(8):
                        K.op("pe", lambda: nc.tensor.transpose(p[:, kc, :], xb[:, kc * 128:(kc + 1) * 128], self.ident),
                             r=[("xtb", t % 3), "cbf"], w=[("tp", t % 2)])
                    eng = "act" if t % 2 == 0 else "dve"
                    if eng == "act":
                        K.op("act", lambda: nc.scalar.copy(out=xT[:, :, t * 128:(t + 1) * 128], in_=p[:]),
                             r=[("tp", t % 2)], w=[("xT", t // 4)])
                    else:
                        K.op("dve", lambda: nc.vector.tensor_copy(out=xT[:, :, t * 128:(t + 1) * 128], in_=p[:]),
                             r=[("tp", t % 2)], w=[("xT", t // 4)])
                K.barrier()

            def proj_fm(es2, wsrc_list, evac, nacc=1, tag="fm"):
                acc = [[self.ps(es2, "acc", [128, 512], F32) for _ in range(nacc)] for _ in range(2 if nacc > 1 else 4)]
                nb = len(acc)
                it = 0
                for j, grp in enumerate(wsrc_list):
                    for n in range(8):
                        b = it % nb
                        it += 1
                        for a in range(nacc):
                            nm, c0 = grp[a]
                            for kc in range(8):
                                K.op("pe", lambda: nc.tensor.matmul(
                                    acc[b][a][:], lhsT=sup[nm][:, kc, c0:c0 + 128],
                                    rhs=xT[:, kc, n * 512:(n + 1) * 512], start=(kc == 0), stop=(kc == 7)),
                                    r=[("sup", nm), ("xT", n)], w=[(tag + "acc", b, a)])
                        evac(j, n, acc[b], [(tag + "acc", b, a) for a in range(nacc)])

            with contextlib.ExitStack() as es2:
                cos = self.sb(es2, "cos", [128, S], F32)
                sin = self.sb(es2, "sin", [128, S], F32)
                K.dma("sp", cos[:], self.cos_t[:, :], w=["cos"])
                K.dma("sp", sin[:], self.sin_t[:, :], w=["sin"])
                t1 = [self.sb(es2, "t1", [128, 512], F32) for _ in range(2)]
                t2 = [self.sb(es2, "t2", [128, 512], F32) for _ in range(2)]
                ob = [self.sb(es2, "ob", [128, 512], BF16) for _ in range(3)]
                groups = []
                for a_, b_ in (("q", "qp"), ("k", "kp")):
                    for jp in range(4):
                        groups.append([(a_, jp * 128), (b_, jp * 128)])
                cnt = [0]

                def evac_qk(j, n, ps, psr):
                    i = cnt[0]
                    cnt[0] += 1
                    a, b, o = t1[i % 2], t2[i % 2], ob[i % 3]
                    K.op("dve", lambda: nc.vector.tensor_tensor(out=a[:], in0=ps[0][:], in1=cos[:, n * 512:(n + 1) * 512], op=ALU.mult),
                         r=[psr[0], "cos"], w=[("t1", i % 2)])
                    K.op("dve", lambda: nc.vector.tensor_tensor(out=b[:], in0=ps[1][:], in1=sin[:, n * 512:(n + 1) * 512], op=ALU.mult),
                         r=[psr[1], "sin"], w=[("t2", i % 2)])
                    K.op("pool", lambda: nc.gpsimd.tensor_tensor(out=o[:], in0=a[:], in1=b[:], op=ALU.add),
                         r=[("t1", i % 2), ("t2", i % 2)], w=[("ob", i % 3)])
                    dstT = self.qT if j < 4 else self.kT
                    K.dma("sp", dstT[j % 4, :, n * 512:(n + 1) * 512], o[:], r=[("ob", i % 3)], w=[("qk", j, n)])

                proj_fm(es2, groups, evac_qk, nacc=2, tag="qk")
                K.barrier()

            with contextlib.ExitStack() as es2:
                R = [self.sb(es2, "R", [128, S + 4], F32) for _ in range(2)]
                accb = [self.sb(es2, "accb", [128, 1024], F32) for _ in range(2)]
                outb = [self.sb(es2, "outb", [128, 1024], BF16) for _ in range(2)]
                stg = [self.sb(es2, "stg", [128, 512], BF16) for _ in range(2)]
                for i in range(2):
                    K.op("dve", lambda: nc.vector.memset(R[i][:, 0:4], 0.0), w=[("R", i)])
                segc = [0]

                def conv_row(Rb, rkey, wcols, nk, seg_out):
                    for sgi in range(4):
                        i = segc[0]
                        segc[0] += 1
                        a = accb[i % 2]
                        t0 = sgi * 1024
                        for k in range(nk):
                            sh = 4 - (nk - 1) + k
                            src_ap = Rb[:, t0 + sh: t0 + sh + 1024]
                            if k == 0:
                                K.op("dve", lambda: nc.vector.tensor_scalar(out=a[:], in0=src_ap, scalar1=wcols[k], scalar2=None, op0=ALU.mult),
                                     r=[rkey, "ppt"], w=[("accb", i % 2)])
                            else:
                                K.op("dve", lambda: nc.vector.scalar_tensor_tensor(out=a[:], in0=src_ap, scalar=wcols[k], in1=a[:], op0=ALU.mult, op1=ALU.add),
                                     r=[rkey, "ppt", ("accb", i % 2)], w=[("accb", i % 2)])
                        seg_out(sgi, a, ("accb", i % 2), i)

                def evac_xbc(j, n, ps, psr):
                    K.op("act", lambda: nc.scalar.copy(out=R[j % 2][:, 4 + n * 512: 4 + (n + 1) * 512], in_=ps[0][:]),
                         r=[psr[0]], w=[("R", j % 2)])
                    if n == 7:
                        def seg_out(sgi, a, akey, i):
                            o = outb[i % 2]
                            K.op("act", lambda: nc.scalar.activation(out=o[:], in_=a[:], func=AF.Silu, bias=ppt[:, PP_CB + j: PP_CB + j + 1], scale=1.0),
                                 r=[akey, "ppt"], w=[("outb", i % 2)])
                            K.dma("sp", self.xbc[j, :, sgi * 1024:(sgi + 1) * 1024], o[:], r=[("outb", i % 2)], w=[("xbc", j, sgi)])
                        conv_row(R[j % 2], ("R", j % 2), [ppt[:, PP_CW + j * 4 + k: PP_CW + j * 4 + k + 1] for k in range(4)], 4, seg_out)

                groups = [[("xs%d" % (j // 4), (j % 4) * 128)] for j in range(8)]
                proj_fm(es2, groups, evac_xbc, nacc=1, tag="xbc")

                def evac_sconv(jj, n, ps, psr):
                    j, which = jj // 3, jj % 3
                    cols = slice(4 + n * 512, 4 + (n + 1) * 512)
                    if which == 0:
                        K.op("act", lambda: nc.scalar.copy(out=R[0][:, cols], in_=ps[0][:]), r=[psr[0]], w=[("R", 0)])
                    elif which == 1:
                        K.op("dve", lambda: nc.vector.tensor_tensor(out=R[1][:, cols], in0=ps[0][:], in1=R[0][:, cols], op=ALU.mult),
                             r=[psr[0], ("R", 0)], w=[("R", 1)])
                        if n == 7:
                            def seg_out(sgi, a, akey, i):
                                K.op("pool", lambda: nc.gpsimd.tensor_copy(out=R[0][:, 4 + sgi * 1024: 4 + (sgi + 1) * 1024], in_=a[:]),
                                     r=[akey], w=[("R", 0)])
                            conv_row(R[1], ("R", 1), [ppt[:, PP_SCW + j * 3 + k: PP_SCW + j * 3 + k + 1] for k in range(3)], 3, seg_out)
                    else:
                        i = segc[0]
                        segc[0] += 1
                        o = stg[i % 2]
                        K.op("dve", lambda: nc.vector.tensor_tensor(out=o[:], in0=ps[0][:], in1=R[0][:, cols], op=ALU.mult),
                             r=[psr[0], ("R", 0)], w=[("stg", i % 2)])
                        K.dma("sp", self.mixT[8 + j, :, n * 512:(n + 1) * 512], o[:], r=[("stg", i % 2)], w=[("mixT", 8 + j, n)])

                groups = []
                for j in range(4):
                    for nm in ("sc", "sh", "sb"):
                        groups.append([(nm, j * 128)])
                proj_fm(es2, groups, evac_sconv, nacc=1, tag="sc")
                K.barrier()

            with contextlib.ExitStack() as es2:
                wv = self.sb(es2, "wv", [128, 8, 512], BF16)
                wz = self.sb(es2, "wz", [128, 8, 512], BF16)
                wd = self.sb(es2, "wd", [128, 8, 8], BF16)
                K.dma("pool", wv[:], self.w_in[l, :, C_V:C_V + 512].rearrange("(k p) c -> p k c", p=128), w=["wv"])
                K.dma("pool", wz[:], self.w_in[l, :, C_Z:C_Z + 512].rearrange("(k p) c -> p k c", p=128), w=["wz"])
                K.dma("pool", wd[:], self.w_in[l, :, C_DT:C_DT + 8].rearrange("(k p) c -> p k c", p=128), w=["wd"])
                pv = [self.ps(es2, "pv", [128, 512], F32) for _ in range(2)]
                pz = [self.ps(es2, "pz", [128, 512], F32) for _ in range(2)]
                pd = [self.ps(es2, "pd", [128, 8], F32) for _ in range(2)]
                vst = [self.sb(es2, "vst", [128, 8, 66], BF16) for _ in range(2)]
                zst = [self.sb(es2, "zst", [128, 512], F32) for _ in range(2)]
                dtall = self.sb(es2, "dtall", [128, NT, 8], F32)
                for i in range(2):
                    K.op("dve", lambda: nc.vector.memset(vst[i][:], 1.0), w=[("vst", i)])
                for t in range(NT):
                    b = t % 2
                    tok = slice(t * 128, (t + 1) * 128)
                    for (wt_, wk, pt, pk, ncol) in ((wv, "wv", pv, "pv", 512), (wz, "wz", pz, "pz", 512), (wd, "wd", pd, "pd", 8)):
                        for kc in range(8):
                            K.op("pe", lambda: nc.tensor.matmul(pt[b][:], lhsT=xT[:, kc, tok], rhs=wt_[:, kc, :], start=(kc == 0), stop=(kc == 7)),
                                 r=[wk, ("xT", t // 4)], w=[(pk, b)])
                    K.op("act", lambda: nc.scalar.copy(out=vst[b][:, :, 0:64], in_=pv[b][:].rearrange("p (h c) -> p h c", h=8)),
                         r=[("pv", b)], w=[("vst", b)])
                    K.dma("sp", self.v_s[tok, :], vst[b][:].rearrange("p h c -> p (h c)"), r=[("vst", b)], w=[("v_s", t)])
                    K.op("act", lambda: nc.scalar.activation(out=zst[b][:], in_=pz[b][:], func=AF.Silu),
                         r=[("pz", b)], w=[("zst", b)])
                    K.dma("sp", self.z_s[tok, :], zst[b][:], r=[("zst", b)], w=[("z_s", t)])
                    K.op("dve", lambda: nc.vector.tensor_tensor(out=dtall[:, t, :], in0=pd[b][:], in1=bct[:], op=ALU.add),
                         r=[("pd", b), "bct"], w=["dtall"])
                dtf = dtall[:].rearrange("p t h -> p (t h)")
                K.op("act", lambda: nc.scalar.activation(out=dtf, in_=dtf, func=AF.Exp), r=["dtall"], w=["dtall"])
                K.op("act", lambda: nc.scalar.activation(out=dtf, in_=dtf, func=AF.Ln, bias=1.0, scale=1.0), r=["dtall"], w=["dtall"])
                K.dma("sp", self.dt_s.rearrange("(t p) h -> p t h", p=128), dtall[:], r=["dtall"], w=["dt_s"])
                K.barrier()

    def phase_attn(self, l, src, dst):
        nc, K = self.nc, self.K
        self.ssd_es = contextlib.ExitStack()
        self.XB = self.sb(self.ssd_es, "XB", [128, 8, S], BF16)
        with contextlib.ExitStack() as es:
            QT = self.sb(es, "QT", [128, 4, S], BF16)
            KT = self.sb(es, "KT", [128, 4, S], BF16)
            for p in range(4):
                K.dma("sp", QT[:, p, :], self.qT[p, :, :], w=["QT"])
                K.dma("sp", KT[:, p, :], self.kT[p, :, :], w=["KT"])
            for j in range(8):
                K.dma("act", self.XB[:, j, :], self.xbc[j, :, :], w=["XBpre"])
            Vd = self.sb(es, "Vd", [128, 32, 8 * 66], BF16)
            pS = [self.ps(es, "pS", [128, 256], F32) for _ in range(4)]
            pO = [self.ps(es, "pO", [128, 4 * 65], F32) for _ in range(4)]
            pt = [self.sb(es, "pt", [128, 256], BF16) for _ in range(6)]
            pmk = [self.sb(es, "pmk", [128, 256], BF16) for _ in range(24)]
            ost = [self.sb(es, "ost", [128, 8, 65], F32) for _ in range(2)]
            LAG = 3
            NBUF = 6
            for di, d in enumerate(PATTERNS):
                nb = 32 // d
                vsrc = self.v_s.rearrange("(n j dd) c -> dd j n c", j=128, dd=d)
                for r in range(d):
                    K.dma("sp", Vd[:, r * nb:(r + 1) * nb, :], vsrc[r], w=["Vd"])
                items = [(r, n, h) for r in range(d) for n in range(nb) for h in range(8)]
                N = len(items)

                def geom(r, n):
                    b = r * nb + n
                    base = r + d * 128 * n
                    cols = slice(base, base + d * 127 + 1, d)
                    pcols = slice(base - d * 128, base - d * 128 + d * 127 + 1, d)
                    return b, cols, pcols

                for s_ in range(N + LAG):
                    if s_ < N:
                        r, n, h = items[s_]
                        b, cols, pcols = geom(r, n)
                        i = s_ % NBUF
                        sl_ = (n % 3) * 8 + h
                        width = 256 if n + 1 < nb else 128
                        base = r + d * 128 * n
                        qcols = slice(base, base + d * (width - 1) + 1, d)
                        pair, pb = h // 2, 64 * (h % 2)
                        K.op("pe", lambda: nc.tensor.matmul(pS[i % 4][:, 0:width], lhsT=KT[pb:pb + 64, pair, cols],
                                                            rhs=QT[pb:pb + 64, pair, qcols], start=True, stop=True),
                             r=["QT", "KT"], w=[("pS", i % 4)])
                        K.op("act", lambda: nc.scalar.activation(out=pt[i][:, 0:width], in_=pS[i % 4][:, 0:width], func=AF.Exp, scale=0.125),
                             r=[("pS", i % 4)], w=[("pt", i)])
                        K.op("dve", lambda: nc.vector.tensor_tensor(out=pmk[sl_][:, 0:width], in0=pt[i][:, 0:width], in1=self.maskcp[:, 0:width], op=ALU.mult),
                             r=[("pt", i), "cbf"], w=[("pmk", sl_)])
                    if s_ >= LAG:
                        r, n, h = items[s_ - LAG]
                        b, cols, pcols = geom(r, n)
                        bi = b
                        oi = (bi % 2) * 2 + h // 4
                        O = pO[oi][:, (h % 4) * 65:(h % 4 + 1) * 65]
                        slc = (n % 3) * 8 + h
                        slp = ((n - 1) % 3) * 8 + h
                        if n > 0:
                            K.op("pe", lambda: nc.tensor.matmul(O, lhsT=pmk[slp][:, 128:256], rhs=Vd[:, b - 1, h * 66:h * 66 + 65], start=True, stop=False),
                                 r=[("pmk", slp), "Vd"], w=[("pO", oi)])
                        K.op("pe", lambda: nc.tensor.matmul(O, lhsT=pmk[slc][:, 0:128], rhs=Vd[:, b, h * 66:h * 66 + 65], start=(n == 0), stop=True),
                             r=[("pmk", slc), "Vd"], w=[("pO", oi)])
                        if h % 4 == 3:
                            hh = h // 4
                            dst_ap = ost[bi % 2][:, hh * 4:(hh + 1) * 4, :]
                            src_ap = pO[oi][:].rearrange("p (h c) -> p h c", h=4)
                            K.op("act", lambda: nc.scalar.copy(out=dst_ap, in_=src_ap), r=[("pO", oi)], w=[("ost", bi % 2, hh)])
                        if h == 7:
                            K.dma("sp", self.o_s[di, cols, :], ost[bi % 2][:].rearrange("p h c -> p (h c)"),
                                  r=[("ost", bi % 2, 0), ("ost", bi % 2, 1)], w=[("o_s", di, b)])
            K.barrier()
        with contextlib.ExitStack() as es:
            o3 = [self.sb(es, "o3", [128, 3, 520], F32) for _ in range(2)]
            nm = [self.sb(es, "nm", [128, 8, 65], F32) for _ in range(2)]
            rd = [self.sb(es, "rd", [128, 8], F32) for _ in range(2)]
            at = [self.sb(es, "at", [128, 8, 64], BF16) for _ in range(2)]
            tpo = [self.ps(es, "tpo", [128, 4, 128], BF16) for _ in range(2)]
            atT = [self.sb(es, "atT", [128, 4, 512], BF16) for _ in range(2)]
            for t in range(NT):
                b = t % 2
                tok = slice(t * 128, (t + 1) * 128)
                K.dma("sp", o3[b][:], self.o_s[:, tok, :].rearrange("d t c -> t d c"), w=[("o3", b)])
                nmf = nm[b][:].rearrange("p h c -> p (h c)")
                K.op("dve", lambda: nc.vector.tensor_tensor(out=nmf, in0=o3[b][:, 0, :], in1=o3[b][:, 1, :], op=ALU.add),
                     r=[("o3", b)], w=[("nm", b)])
                K.op("dve", lambda: nc.vector.tensor_tensor(out=nmf, in0=nmf, in1=o3[b][:, 2, :], op=ALU.add),
                     r=[("o3", b), ("nm", b)], w=[("nm", b)])
                K.op("dve", lambda: nc.vector.reciprocal(out=rd[b][:], in_=nm[b][:, :, 64]), r=[("nm", b)], w=[("rd", b)])
                K.op("dve", lambda: nc.vector.tensor_tensor(out=at[b][:], in0=nm[b][:, :, 0:64],
                                                            in1=rd[b][:].unsqueeze(2).broadcast_to([128, 8, 64]), op=ALU.mult),
                     r=[("nm", b), ("rd", b)], w=[("at", b)])
                atf = at[b][:].rearrange("p h c -> p (h c)")
                for c in range(4):
                    K.op("pe", lambda: nc.tensor.transpose(tpo[b][:, c, :], atf[:, c * 128:(c + 1) * 128], self.ident),
                         r=[("at", b), "cbf"], w=[("tpo", b)])
                g = (t // 4) % 2
                K.op("act", lambda: nc.scalar.copy(out=atT[g][:, :, (t % 4) * 128:(t % 4 + 1) * 128], in_=tpo[b][:]),
                     r=[("tpo", b)], w=[("atT", g)])
                if t % 4 == 3:
                    K.dma("sp", self.mixT[0:4, :, (t // 4) * 512:(t // 4 + 1) * 512].rearrange("c p t -> p c t"), atT[g][:],
                          r=[("atT", g)], w=[("mixT", 0, t // 4)])
            K.barrier()

    def phase_ssd(self, l, src, dst):
        nc, K = self.nc, self.K
        bcast = lambda ap, shape, ax: ap.unsqueeze(ax).broadcast_to(shape)
        with contextlib.ExitStack() as es:
            XB = self.XB
            dtt = self.sb(es, "dtt", [128, 32, 8], F32)
            K.dma("sp", dtt[:], self.dt_s.rearrange("(c p) h -> p c h", p=128), w=["dtt"])
            prm = self.sb(es, "prm", [128, 24 + 512], F32)
            K.dma("sp", prm[:, 0:8], self.bc[l, BC_ALOG:BC_ALOG + 8].partition_broadcast(128), w=["prm"])
            K.dma("sp", prm[:, 16:24], self.bc[l, BC_D:BC_D + 8].partition_broadcast(128), w=["prm"])
            K.dma("sp", prm[:, 24:536], self.bc[l, BC_NG:BC_NG + 512].partition_broadcast(128), w=["prm"])
            a_bc, d_bc, ng_bc = prm[:, 8:16], prm[:, 16:24], prm[:, 24:536]
            A = self.sb(es, "A", [128, 32, 8], F32)
            cum = self.sb(es, "cum", [128, 32, 8], F32)
            clast = self.sb(es, "clast", [128, 32, 8], F32)
            decst = self.sb(es, "decst", [128, 32, 8], F32)
            dtdec = self.sb(es, "dtdec", [128, 32, 8], F32)
            ecum = self.sb(es, "ecum", [128, 32, 8], F32)
            cdec = self.sb(es, "cdec", [128, 32, 8], F32)
            fl = lambda t: t[:].rearrange("p c h -> p (c h)")
            K.op("act", lambda: nc.scalar.activation(out=prm[:, 8:16], in_=prm[:, 0:8], func=AF.Exp), r=["prm"], w=["prm2"])
            K.op("dve", lambda: nc.vector.tensor_scalar(out=prm[:, 8:16], in0=prm[:, 8:16], scalar1=-1.0, scalar2=None, op0=ALU.mult),
                 r=["prm2"], w=["prm2"])
            K.op("dve", lambda: nc.vector.tensor_tensor(out=A[:], in0=dtt[:], in1=bcast(a_bc, [128, 32, 8], 1), op=ALU.mult),
                 r=["prm2", "dtt"], w=["A"])
            with contextlib.ExitStack() as es1:
                pc = self.ps(es1, "pc", [128, 256], F32)
                pl = self.ps(es1, "pl", [128, 256], F32)
                K.op("pe", lambda: nc.tensor.matmul(pc[:], lhsT=self.U_f, rhs=fl(A), start=True, stop=True), r=["A", "cf"], w=["pc"])
                K.op("pe", lambda: nc.tensor.matmul(pl[:], lhsT=self.ones_f, rhs=fl(A), start=True, stop=True), r=["A", "cf"], w=["pl"])
                K.op("dve", lambda: nc.vector.tensor_copy(out=fl(cum), in_=pc[:]), r=["pc"], w=["cum"])
                K.op("dve", lambda: nc.vector.tensor_copy(out=fl(clast), in_=pl[:]), r=["pl"], w=["clast"])
                K.op("dve", lambda: nc.vector.tensor_tensor(out=fl(decst), in0=fl(clast), in1=fl(cum), op=ALU.subtract),
                     r=["cum", "clast"], w=["decst"])
                K.op("act", lambda: nc.scalar.activation(out=fl(decst), in_=fl(decst), func=AF.Exp), r=["decst"], w=["decst"])
                K.op("act", lambda: nc.scalar.activation(out=fl(ecum), in_=fl(cum), func=AF.Exp), r=["cum"], w=["ecum"])
                K.op("act", lambda: nc.scalar.activation(out=fl(cdec), in_=fl(clast), func=AF.Exp), r=["clast"], w=["cdec"])
                K.op("dve", lambda: nc.vector.tensor_tensor(out=fl(dtdec), in0=fl(decst), in1=fl(dtt), op=ALU.mult),
                     r=["decst", "dtt"], w=["dtdec"])
                K.barrier()
            tpx = self.ps(es, "tpx", [128, 6, 128], BF16)
            pG = self.ps(es, "pG", [128, 2, 128], F32)
            pSeg = [self.ps(es, "pSeg", [128, 4, 128], F32) for _ in range(2)]
            pYd = self.ps(es, "pYd", [128, 8, 64], F32)
            pYo = self.ps(es, "pYo", [128, 8, 64], F32)
            pSt = self.ps(es, "pSt", [128, 8, 64], F32)
            tpy = self.ps(es, "tpy", [128, 4, 128], BF16)
            X = [self.sb(es, "X", [128, 8, 64], BF16) for _ in range(3)]
            Xd = [self.sb(es, "Xd", [128, 8, 64], BF16) for _ in range(3)]
            xh = [self.sb(es, "xh", [128, 8, 64], BF16) for _ in range(3)]
            Bt = [self.sb(es, "Bt", [128, 2, 128], BF16) for _ in range(3)]
            GmT = [self.sb(es, "GmT", [128, 2, 128], F32) for _ in range(3)]
            lh = [self.sb(es, "lh", [128, 128], F32) for _ in range(4)]
            eL = [self.sb(es, "eL", [128, 4, 128], F32) for _ in range(6)]
            scT = [self.sb(es, "scT", [128, 4, 128], BF16) for _ in range(6)]
            yo = [self.sb(es, "yo", [128, 8, 64], F32) for _ in range(2)]
            yy = [self.sb(es, "yy", [128, 8, 64], F32) for _ in range(2)]
            td = [self.sb(es, "td", [128, 8, 64], F32) for _ in range(2)]
            zt = [self.sb(es, "zt", [128, 512], F32) for _ in range(3)]
            junk = self.sb(es, "junk", [128, 256], F32)
            ss = [self.sb(es, "ss", [128, 2], F32) for _ in range(2)]
            yn = [self.sb(es, "yn", [128, 512], BF16) for _ in range(2)]
            sdT = [self.sb(es, "sdT", [128, 4, 512], BF16) for _ in range(2)]
            hf = self.sb(es, "hf", [128, 8, 64], F32)
            hbf = [self.sb(es, "hbf", [128, 8, 64], BF16) for _ in range(2)]
            lic = [0]

            def stage_a(c):
                b = c % 2
                b3 = c % 3
                tok = slice(c * 128, (c + 1) * 128)
                K.dma("sp", zt[b3][:], self.z_s[tok, :], w=[("zt", b3)])
                for j in range(6):
                    K.op("pe", lambda: nc.tensor.transpose(tpx[:, j, :], XB[:, j, tok], self.ident), r=["XB", "cbf"], w=["tpx"])
                tx = tpx[:, 0:4, :].rearrange("p a (b c) -> p (a b) c", b=2)
                K.op("act", lambda: nc.scalar.copy(out=xh[b3][:], in_=tx), r=["tpx"], w=[("xh", b3)])
                K.op("act", lambda: nc.scalar.copy(out=Bt[b3][:], in_=tpx[:, 4:6, :]), r=["tpx"], w=[("Bt", b3)])
                K.op("dve", lambda: nc.vector.tensor_tensor(out=X[b3][:], in0=xh[b3][:], in1=bcast(dtt[:, c, :], [128, 8, 64], 2), op=ALU.mult),
                     r=[("xh", b3), "dtt"], w=[("X", b3)])
                K.op("dve", lambda: nc.vector.tensor_tensor(out=Xd[b3][:], in0=xh[b3][:], in1=bcast(dtdec[:, c, :], [128, 8, 64], 2), op=ALU.mult),
                     r=[("xh", b3), "dtdec"], w=[("Xd", b3)])
                for g in range(2):
                    K.op("pe", lambda: nc.tensor.matmul(pG[:, g, :], lhsT=XB[:, 4 + g, tok], rhs=XB[:, 6 + g, tok], start=True, stop=True),
                         r=["XB"], w=["pG"])
                K.op("dve", lambda: nc.vector.tensor_tensor(out=GmT[b3][:], in0=pG[:], in1=bcast(self.mls_f, [128, 2, 128], 1), op=ALU.mult),
                     r=["pG", "cf"], w=[("GmT", b3)])
                for hh in range(2):
                    e = (c * 2 + hh) % 6
                    for h4 in range(4):
                        h = hh * 4 + h4
                        i = lic[0] % 4
                        lic[0] += 1
                        veng = "dve"
                        vh = nc.vector
                        K.op(veng, lambda: vh.tensor_scalar(out=lh[i][:], in0=self.SL_f, scalar1=A[:, c, h:h + 1], scalar2=None, op0=ALU.mult),
                             r=["A", "cf"], w=[("lh", i)])
                        K.op("pe", lambda: nc.tensor.matmul(pSeg[hh][:, h4, :], lhsT=lh[i][:], rhs=self.U_f, start=True, stop=True),
                             r=[("lh", i), "cf"], w=[("pSeg", hh)])
                    K.op("act", lambda: nc.scalar.activation(out=eL[e][:], in_=pSeg[hh][:], func=AF.Exp), r=[("pSeg", hh)], w=[("eL", e)])
                    K.op("pool", lambda: nc.gpsimd.tensor_tensor(out=scT[e][:], in0=eL[e][:], in1=bcast(GmT[b3][:, hh, :], [128, 4, 128], 1), op=ALU.mult),
                         r=[("eL", e), ("GmT", b3)], w=[("scT", e)])

            def stage_b(c):
                b = c % 2
                b3 = c % 3
                tok = slice(c * 128, (c + 1) * 128)
                for h in range(8):
                    e = (c * 2 + h // 4) % 6
                    K.op("pe", lambda: nc.tensor.matmul(pYd[:, h, :], lhsT=scT[e][:, h % 4, :], rhs=X[b3][:, h, :], start=True, stop=True),
                         r=[("scT", e), ("X", b3)], w=["pYd"])
                if c > 0:
                    for h in range(8):
                        K.op("pe", lambda: nc.tensor.matmul(pYo[:, h, :], lhsT=XB[:, 6 + h // 4, tok], rhs=hbf[b][:, h, :], start=True, stop=True),
                             r=["XB", ("hbf", b)], w=["pYo"])
                    K.op("dve", lambda: nc.vector.tensor_tensor(out=yo[b][:], in0=pYo[:], in1=bcast(ecum[:, c, :], [128, 8, 64], 2), op=ALU.mult),
                         r=["pYo", "ecum"], w=[("yo", b)])
                    K.op("dve", lambda: nc.vector.tensor_tensor(out=yy[b][:], in0=pYd[:], in1=yo[b][:], op=ALU.add),
                         r=["pYd", ("yo", b)], w=[("yy", b)])
                else:
                    K.op("dve", lambda: nc.vector.tensor_copy(out=yy[b][:], in_=pYd[:]), r=["pYd"], w=[("yy", b)])
                K.op("pool", lambda: nc.gpsimd.tensor_tensor(out=td[b][:], in0=xh[b3][:], in1=bcast(d_bc, [128, 8, 64], 2), op=ALU.mult),
                     r=[("xh", b3), "prm"], w=[("td", b)])
                K.op("pool", lambda: nc.gpsimd.tensor_tensor(out=yy[b][:], in0=yy[b][:], in1=td[b][:], op=ALU.add),
                     r=[("yy", b), ("td", b)], w=[("yy", b)])
                yf = yy[b][:].rearrange("p h c -> p (h c)")
                K.op("dve", lambda: nc.vector.tensor_tensor(out=yf, in0=yf, in1=zt[b3][:], op=ALU.mult),
                     r=[("yy", b), ("zt", b3)], w=[("yy", b)])
                for g in range(2):
                    K.op("act", lambda: nc.scalar.activation(out=junk[:], in_=yf[:, g * 256:(g + 1) * 256], func=AF.Square, accum_out=ss[b][:, g:g + 1]),
                         r=[("yy", b)], w=[("ss", b, g), "junk"])
                K.op("dve", lambda: nc.vector.tensor_scalar(out=ss[b][:], in0=ss[b][:], scalar1=1.0 / 256.0, scalar2=RMS_EPS, op0=ALU.mult, op1=ALU.add),
                     r=[("ss", b, 0), ("ss", b, 1)], w=[("ss", b, 0), ("ss", b, 1)])
                K.op("act", lambda: nc.scalar.activation(out=ss[b][:], in_=ss[b][:], func=AF.Ln),
                     r=[("ss", b, 0), ("ss", b, 1)], w=[("ss", b, 0), ("ss", b, 1)])
                K.op("act", lambda: nc.scalar.activation(out=ss[b][:], in_=ss[b][:], func=AF.Exp, scale=-0.5),
                     r=[("ss", b, 0), ("ss", b, 1)], w=[("ss", b, 0), ("ss", b, 1)])
                for g in range(2):
                    K.op("dve", lambda: nc.vector.scalar_tensor_tensor(out=yn[b][:, g * 256:(g + 1) * 256], in0=yf[:, g * 256:(g + 1) * 256],
                                                                        scalar=ss[b][:, g:g + 1], in1=ng_bc[:, g * 256:(g + 1) * 256],
                                                                        op0=ALU.mult, op1=ALU.mult),
                         r=[("yy", b), ("ss", b, 0), ("ss", b, 1), "prm"], w=[("yn", b)])

            def stage_t(c):
                b = c % 2
                b3 = c % 3
                for j in range(4):
                    K.op("pe", lambda: nc.tensor.transpose(tpy[:, j, :], yn[b][:, j * 128:(j + 1) * 128], self.ident), r=[("yn", b), "cbf"], w=["tpy"])
                g4 = (c // 4) % 2
                K.op("act", lambda: nc.scalar.copy(out=sdT[g4][:, :, (c % 4) * 128:(c % 4 + 1) * 128], in_=tpy[:]), r=["tpy"], w=[("sdT", g4)])
                if c % 4 == 3:
                    K.dma("sp", self.mixT[4:8, :, (c // 4) * 512:(c // 4 + 1) * 512].rearrange("c p t -> p c t"), sdT[g4][:],
                          r=[("sdT", g4)], w=[("mixT", 4, c // 4)])

            def stage_s(c):
                b = c % 2
                b3 = c % 3
                if c < NT - 1:
                    for h in range(8):
                        K.op("pe", lambda: nc.tensor.matmul(pSt[:, h, :], lhsT=Bt[b3][:, h // 4, :], rhs=Xd[b3][:, h, :], start=True, stop=True),
                             r=[("Bt", b3), ("Xd", b3)], w=["pSt"])
                    if c == 0:
                        K.op("dve", lambda: nc.vector.tensor_copy(out=hf[:], in_=pSt[:]), r=["pSt"], w=["hf"])
                    else:
                        K.op("pool", lambda: nc.gpsimd.tensor_tensor(out=hf[:], in0=hf[:], in1=bcast(cdec[:, c, :], [128, 8, 64], 2), op=ALU.mult),
                             r=["hf", "cdec"], w=["hf"])
                        K.op("dve", lambda: nc.vector.tensor_tensor(out=hf[:], in0=hf[:], in1=pSt[:], op=ALU.add), r=["hf", "pSt"], w=["hf"])
                    K.op("act", lambda: nc.scalar.copy(out=hbf[1 - b][:], in_=hf[:]), r=["hf"], w=[("hbf", 1 - b)])

            stage_a(0)
            stage_a(1)
            for c in range(NT):
                stage_s(c)
                stage_b(c)
                if c + 2 < NT:
                    stage_a(c + 2)
                if c >= 1:
                    stage_t(c - 1)
            stage_t(NT - 1)
            K.barrier()
        self.ssd_es.close()

    def layernorm(self, es, name, beng="pool", nbuf=3):
        nc, K = self.nc, self.K
        st = [self.sb(es, name + "st", [128, 2, 6], F32) for _ in range(nbuf)]
        mv = [self.sb(es, name + "mv", [128, 4], F32) for _ in range(nbuf)]
        bh = nc.gpsimd if beng == "pool" else nc.vector

        def stats(i, h, hk):
            mk = (name + "mv", i)
            for c in range(2):
                K.op("dve", lambda: nc.vector.bn_stats(out=st[i][:, c, :], in_=h[:, c * 512:(c + 1) * 512]), r=[hk], w=[(name + "st", i, c)])
            K.op("dve", lambda: nc.vector.bn_aggr(out=mv[i][:, 0:2], in_=st[i][:].rearrange("p a b -> p (a b)")),
                 r=[(name + "st", i, 0), (name + "st", i, 1)], w=[mk])
            K.op("dve", lambda: nc.vector.tensor_scalar(out=mv[i][:, 1:2], in0=mv[i][:, 1:2], scalar1=LN_EPS, scalar2=None, op0=ALU.add), r=[mk], w=[mk])
            K.op("act", lambda: nc.scalar.activation(out=mv[i][:, 1:2], in_=mv[i][:, 1:2], func=AF.Ln), r=[mk], w=[mk])
            K.op("act", lambda: nc.scalar.activation(out=mv[i][:, 2:3], in_=mv[i][:, 1:2], func=AF.Exp, scale=-0.5), r=[mk], w=[mk])
            K.op("dve", lambda: nc.vector.tensor_scalar(out=mv[i][:, 3:4], in0=mv[i][:, 0:1], scalar1=mv[i][:, 2:3], scalar2=-1.0, op0=ALU.mult, op1=ALU.mult),
                 r=[mk], w=[mk])

        def apply(i, h, hk, out, ok, g_bc, b_bc, pk):
            mk = (name + "mv", i)
            K.op("act", lambda: nc.scalar.activation(out=h, in_=h, func=AF.Identity, bias=mv[i][:, 3:4], scale=mv[i][:, 2:3]), r=[hk, mk], w=[hk])
            K.op("dve", lambda: nc.vector.tensor_tensor(out=h, in0=h, in1=g_bc, op=ALU.mult), r=[hk, pk], w=[hk])
            K.op(beng, lambda: bh.tensor_tensor(out=out, in0=h, in1=b_bc, op=ALU.add), r=[hk, pk], w=[ok])
        return stats, apply

    def phase_outln(self, l, src, dst):
        nc, K = self.nc, self.K
        self.exp_es = contextlib.ExitStack()
        self.exp_pre = None
        if self.with_moe:
            ees = self.exp_es
            self.exp_pre = (self.sb(ees, "wgu0", [128, 8, 2 * D], BF16), self.sb(ees, "wdn0", [128, 8, D], BF16), self.sb(ees, "bdn0", [1, D], BF16))
        with contextlib.ExitStack() as es:
            wo = self.sb(es, "wo", [128, 12, D], BF16)
            for c in range(3):
                K.dma("pool", wo[:, c * 4:(c + 1) * 4, :], self.w_out[l, c * 512:(c + 1) * 512, :].rearrange("(c p) d -> p c d", p=128), w=["wo"])
            wr = self.sb(es, "wr", [128, 8, NE], BF16)
            K.dma("pool", wr[:], self.router_w[l].rearrange("(k p) e -> p k e", p=128), w=["wr"])
            if self.exp_pre is not None:
                w0, d0, b0 = self.exp_pre
                for c in range(2):
                    K.dma("pool", w0[:, :, c * D:(c + 1) * D], self.w_gu[l, 0, :, c * D:(c + 1) * D].rearrange("(k p) f -> p k f", p=128), w=[("wgu", 0)])
                K.dma("pool", d0[:], self.w_dn[l, 0].rearrange("(k p) f -> p k f", p=128), w=[("wdn", 0)])
                K.dma("pool", b0[:], self.b_dn[l, 0:1, :], w=[("bdn", 0)])
            lnp = self.sb(es, "lnp", [128, 2 * D + NE], F32)
            K.dma("sp", lnp[:, 0:D], self.bc[l, BC_L1G:BC_L1G + D].partition_broadcast(128), w=["lnp"])
            K.dma("sp", lnp[:, D:2 * D], self.bc[l, BC_L1B:BC_L1B + D].partition_broadcast(128), w=["lnp"])
            K.dma("sp", lnp[:, 2 * D:], self.bc[l, BC_RB:BC_RB + NE].partition_broadcast(128), w=["lnp"])
            g_bc, b_bc, rb_bc = lnp[:, 0:D], lnp[:, D:2 * D], lnp[:, 2 * D:]
            ecap = self.sb(es, "ecap", [128, NE], F32)
            K.op("dve", lambda: nc.vector.tensor_scalar(out=ecap[:], in0=self.iota_e, scalar1=float(CAP), scalar2=None, op0=ALU.mult), r=["cf"], w=["ecap"])
            cntb = self.sb(es, "cntb", [128, NE], F32)
            K.op("dve", lambda: nc.vector.memset(cntb[:], 0.0), w=["cntb"])
            ln_stats, ln_apply = self.layernorm(es, "ln1", beng="dve")
            NB = 3
            TB = 4
            NXB = 4 * TB
            mt = [self.sb(es, "mt", [128, 12, 512], BF16) for _ in range(2)]
            xr = [self.sb(es, "xr", [128, D], F32) for _ in range(NB)]
            hb = [self.sb(es, "hb", [128, D], F32) for _ in range(NB)]
            x1t = [self.sb(es, "x1t", [128, D], F32) for _ in range(NB)]
            x1b = [self.sb(es, "x1b", [128, D], BF16) for _ in range(NXB)]
            x1T = [self.sb(es, "x1T", [128, 8, 128], BF16) for _ in range(2)]
            pm = [self.ps(es, "pm", [128, 512], F32) for _ in range(4)]
            tpr = [self.ps(es, "tpr", [128, 8, 128], BF16) for _ in range(2)]
            plg = self.ps(es, "plg", [128, TB, NE], F32)
            ppos = self.ps(es, "ppos", [128, 2, TB, NE], F32)
            lg = self.sb(es, "lg", [128, TB, NE], F32)
            mx8 = self.sb(es, "mx8", [128, TB, 8], F32)
            ix8 = self.sb(es, "ix8", [128, TB, 8], U32)
            e4 = self.sb(es, "e4", [128, TB, 4], F32)
            rs = self.sb(es, "rs", [128, TB], F32)
            g4 = [self.sb(es, "g4", [128, TB, 4], F32) for _ in range(2)]
            idxf = self.sb(es, "idxf", [128, TB, 4], F32)
            oh = self.sb(es, "oh", [128, TB, 4, NE], F32)
            mskb = self.sb(es, "mskb", [128, TB, NE], BF16)
            cs = self.sb(es, "cs", [128, NE, TB], F32)
            inc = self.sb(es, "inc", [128, NE, TB], F32)
            slot = self.sb(es, "slot", [128, TB, NE], F32)
            tmp = self.sb(es, "tmp", [128, TB, 4, NE], F32)
            off = self.sb(es, "off", [128, TB, 4], F32)
            offi = [self.sb(es, "offi", [128, TB, 4], I32) for _ in range(2)]

            def stage_a(t):
                b = t % NB
                bx = t % NXB
                tok = slice(t * 128, (t + 1) * 128)
                g4i = (t // 4) % 2
                if t == 0:
                    K.dma("sp", mt[0][:], self.mixT[:, :, 0:512].rearrange("c p t -> p c t"), w=[("mt", 0)])
                    K.dma("sp", xr[0][:], src[0:128, :], w=[("xr", 0)])
                if t % 4 == 0 and t + 4 < NT:
                    gn = t // 4 + 1
                    K.dma("sp", mt[gn % 2][:], self.mixT[:, :, gn * 512:(gn + 1) * 512].rearrange("c p t -> p c t"), w=[("mt", gn % 2)])
                if t + 1 < NT:
                    K.dma("sp", xr[(t + 1) % NB][:], src[(t + 1) * 128:(t + 2) * 128, :], w=[("xr", (t + 1) % NB)])
                for half in range(2):
                    pi = (t % 2) * 2 + half
                    for mc in range(12):
                        K.op("pe", lambda: nc.tensor.matmul(pm[pi][:], lhsT=mt[g4i][:, mc, (t % 4) * 128:(t % 4 + 1) * 128],
                                                            rhs=wo[:, mc, half * 512:(half + 1) * 512], start=(mc == 0), stop=(mc == 11)),
                             r=[("mt", g4i), "wo"], w=[("pm", pi)])
                    K.op("dve", lambda: nc.vector.scalar_tensor_tensor(out=hb[b][:, half * 512:(half + 1) * 512], in0=xr[b][:, half * 512:(half + 1) * 512],
                                                                        scalar=ALPHA, in1=pm[pi][:], op0=ALU.mult, op1=ALU.add),
                         r=[("xr", b), ("pm", pi)], w=[("hb", b)])
                ln_stats(b, hb[b][:], ("hb", b))

            def stage_a2(t):
                b = t % NB
                bx = t % NXB
                tok = slice(t * 128, (t + 1) * 128)
                ln_apply(b, hb[b][:], ("hb", b), x1t[b][:], ("x1t", b), g_bc, b_bc, "lnp")
                K.dma("sp", self.x1[tok, :], x1t[b][:], r=[("x1t", b)], w=[("x1", t)])
                K.op("act", lambda: nc.scalar.copy(out=x1b[bx][:], in_=x1t[b][:]), r=[("x1t", b)], w=[("x1b", bx)])

            def stage_r(t):
                b = t % 2
                bx = t % NXB
                for kc in range(8):
                    K.op("pe", lambda: nc.tensor.transpose(tpr[b][:, kc, :], x1b[bx][:, kc * 128:(kc + 1) * 128], self.ident), r=[("x1b", bx), "cbf"], w=[("tpr", b)])
                K.op("act", lambda: nc.scalar.copy(out=x1T[b][:], in_=tpr[b][:]), r=[("tpr", b)], w=[("x1T", b)])
                for kc in range(8):
                    K.op("pe", lambda: nc.tensor.matmul(plg[:, t % TB, :], lhsT=x1T[b][:, kc, :], rhs=wr[:, kc, :], start=(kc == 0), stop=(kc == 7)),
                         r=[("x1T", b), "wr"], w=["plg"])

            def stage_b(tb):
                gb = tb % 2
                bc4 = lambda ap: ap.unsqueeze(3).broadcast_to([128, TB, 4, NE])
                K.op("dve", lambda: nc.vector.tensor_tensor(out=lg[:], in0=plg[:], in1=rb_bc.unsqueeze(1).broadcast_to([128, TB, NE]), op=ALU.add),
                     r=["plg", "lnp"], w=["lg"])
                for i in range(TB):
                    K.op("dve", lambda: nc.vector.max(out=mx8[:, i, :], in_=lg[:, i, :]), r=["lg"], w=[("mx8", i)])
                yield
                for i in range(TB):
                    K.op("dve", lambda: nc.vector.max_index(out=ix8[:, i, :], in_max=mx8[:, i, :], in_values=lg[:, i, :]), r=["lg", ("mx8", i)], w=[("ix8", i)])
                yield
                allmx = [("mx8", i) for i in range(TB)]
                allix = [("ix8", i) for i in range(TB)]
                K.op("dve", lambda: nc.vector.tensor_tensor(out=e4[:], in0=mx8[:, :, 0:4], in1=mx8[:, :, 0:1].broadcast_to([128, TB, 4]), op=ALU.subtract),
                     r=allmx, w=["e4"])
                K.op("act", lambda: nc.scalar.activation(out=e4[:], in_=e4[:], func=AF.Exp), r=["e4"], w=["e4"])
                K.op("dve", lambda: nc.vector.tensor_reduce(out=rs[:], in_=e4[:], axis=AX.X, op=ALU.add), r=["e4"], w=["rs"])
                K.op("dve", lambda: nc.vector.reciprocal(out=rs[:], in_=rs[:]), r=["rs"], w=["rs"])
                K.op("dve", lambda: nc.vector.tensor_tensor(out=g4[gb][:], in0=e4[:], in1=rs[:].unsqueeze(2).broadcast_to([128, TB, 4]), op=ALU.mult),
                     r=["e4", "rs"], w=[("g4", gb)])
                K.dma("act", self.gate_s[tb * TB * 128:(tb + 1) * TB * 128, :].rearrange("(t p) k -> p t k", p=128), g4[gb][:], r=[("g4", gb)], w=[("gate_s", tb)])
                yield
                K.op("dve", lambda: nc.vector.tensor_copy(out=idxf[:], in_=ix8[:, :, 0:4]), r=allix, w=["idxf"])
                K.op("dve", lambda: nc.vector.tensor_tensor(out=oh[:], in0=self.iota_e.unsqueeze(1).unsqueeze(1).broadcast_to([128, TB, 4, NE]),
                                                            in1=bc4(idxf[:]), op=ALU.is_equal), r=["idxf", "cf"], w=["oh"])
                with nc.allow_low_precision(reason="0/1 one-hot sums are exact in bf16"):
                    K.op("dve", lambda: nc.vector.tensor_reduce(out=mskb[:], in_=oh[:].rearrange("p t k e -> p t e k"), axis=AX.X, op=ALU.add), r=["oh"], w=["mskb"])
                yield
                mflat = mskb[:].rearrange("p t e -> p (t e)")
                K.op("pe", lambda: nc.tensor.matmul(ppos[:, 0, :, :].rearrange("p t e -> p (t e)"), lhsT=self.sl_bf, rhs=mflat, start=True, stop=True),
                     r=["mskb", "cbf"], w=["ppos"])
                K.op("pe", lambda: nc.tensor.matmul(ppos[:, 1, :, :].rearrange("p t e -> p (t e)"), lhsT=self.ones_bf, rhs=mflat, start=True, stop=True),
                     r=["mskb", "cbf"], w=["ppos"])
                K.op("dve", lambda: nc.vector.tensor_copy(out=cs[:], in_=ppos[:, 1, :, :].rearrange("p t e -> p e t")), r=["ppos"], w=["cs"])
                K.op("dve", lambda: nc.vector.tensor_tensor_scan(out=inc[:].rearrange("p e t -> p (e t)"), data0=self.zr_f,
                                                                  data1=cs[:].rearrange("p e t -> p (e t)"), initial=0.0, op0=ALU.mult, op1=ALU.add),
                     r=["cs", "cf"], w=["inc"])
                yield
                K.op("dve", lambda: nc.vector.tensor_tensor(out=slot[:], in0=ppos[:, 0, :, :], in1=inc[:].rearrange("p e t -> p t e"), op=ALU.add),
                     r=["ppos", "inc"], w=["slot"])
                K.op("dve", lambda: nc.vector.tensor_tensor(out=slot[:], in0=slot[:], in1=cs[:].rearrange("p e t -> p t e"), op=ALU.subtract),
                     r=["slot", "cs"], w=["slot"])
                K.op("dve", lambda: nc.vector.tensor_tensor(out=slot[:], in0=slot[:], in1=cntb[:].unsqueeze(1).broadcast_to([128, TB, NE]), op=ALU.add),
                     r=["slot", "cntb"], w=["slot"])
                K.op("dve", lambda: nc.vector.tensor_scalar(out=slot[:], in0=slot[:], scalar1=float(CAP - 1), scalar2=None, op0=ALU.min),
                     r=["slot"], w=["slot"])
                K.op("dve", lambda: nc.vector.tensor_tensor(out=slot[:], in0=slot[:], in1=ecap[:].unsqueeze(1).broadcast_to([128, TB, NE]), op=ALU.add),
                     r=["slot", "ecap"], w=["slot"])
                K.op("dve", lambda: nc.vector.tensor_tensor(out=cntb[:], in0=cntb[:], in1=inc[:, :, TB - 1], op=ALU.add), r=["cntb", "inc", "slot"], w=["cntb"])
                yield
                K.op("dve", lambda: nc.vector.tensor_tensor(out=tmp[:], in0=oh[:], in1=slot[:].unsqueeze(2).broadcast_to([128, TB, 4, NE]), op=ALU.mult),
                     r=["oh", "slot"], w=["tmp"])
                K.op("dve", lambda: nc.vector.tensor_reduce(out=off[:], in_=tmp[:], axis=AX.X, op=ALU.add), r=["tmp"], w=["off"])
                K.op("dve", lambda: nc.vector.tensor_copy(out=offi[gb][:], in_=off[:]), r=["off"], w=[("offi", gb)])
                K.dma("act", self.offs[tb * TB * 128:(tb + 1) * TB * 128, :].rearrange("(t p) k -> p t k", p=128), offi[gb][:], r=[("offi", gb)], w=[("offs", tb)])
                yield
                for i in range(TB):
                    bx = (tb * TB + i) % NXB
                    for k in range(4):
                        K.dma("pool", self.xg[:, :], x1b[bx][:], r=[("x1b", bx), ("offi", gb)], w=["xg"],
                              indirect=dict(out_offset=bass.IndirectOffsetOnAxis(offi[gb][:, i, k:k + 1], 0), in_offset=None))

            pending = []
            for t in range(NT + 2):
                if 2 <= t <= NT + 1:
                    stage_r(t - 2)
                    if (t - 1) % TB == 0:
                        for _ in stage_b((t - 1) // TB - 1):
                            pass
                if 1 <= t <= NT:
                    stage_a2(t - 1)
                for g in list(pending):
                    try:
                        next(g)
                    except StopIteration:
                        pending.remove(g)
                if t < NT:
                    stage_a(t)
            for g in pending:
                for _ in g:
                    pass
            K.barrier()

    def phase_route(self, l, src, dst):
        pass

    def phase_experts(self, l, src, dst):
        nc, K = self.nc, self.K
        NCG, CGW = 2, CAP // 2
        NST = CAP // 128
        with contextlib.ExitStack() as es:
            bgu = self.sb(es, "bgu", [128, NE * 16], F32)
            K.dma("sp", bgu[:], self.pp[l, :, PP_BGU:PP_BGU + NE * 16], w=["bgu"])
            wgu = [self.exp_pre[0], self.sb(es, "wgu", [128, 8, 2 * D], BF16)]
            wdn = [self.exp_pre[1], self.sb(es, "wdn", [128, 8, D], BF16)]
            bdn = [self.exp_pre[2], self.sb(es, "bdn", [1, D], BF16)]
            xgt = [self.sb(es, "xgt", [128, XGW], BF16) for _ in range(3)]
            xgT = [self.sb(es, "xgT", [128, 8, CAP], BF16) for _ in range(2)]
            hT = [self.sb(es, "hT", [128, 8, CAP], BF16) for _ in range(2)]
            g1 = [self.sb(es, "g1", [128, CGW], F32) for _ in range(2)]
            sg = [self.sb(es, "sg", [128, CGW], F32) for _ in range(2)]
            u1 = [self.sb(es, "u1", [128, CGW], F32) for _ in range(2)]
            yt = [self.sb(es, "yt", [128, D], F32) for _ in range(2)]
            tpx = [self.ps(es, "tpx", [128, 8, 128], BF16) for _ in range(2)]
            pg = [self.ps(es, "pg", [128, 512], F32) for _ in range(2)]
            pu = [self.ps(es, "pu", [128, 512], F32) for _ in range(2)]
            py = [self.ps(es, "py", [128, 512], F32) for _ in range(2)]
            cnt = {"xi": 0, "ei": 0, "yi": 0, "ti": 0}

            def prep(e):
                wb = e % 2
                if e > 0:
                    for c in range(2):
                        K.dma("pool", wgu[wb][:, :, c * D:(c + 1) * D], self.w_gu[l, e, :, c * D:(c + 1) * D].rearrange("(k p) f -> p k f", p=128), w=[("wgu", wb)])
                    K.dma("pool", wdn[wb][:], self.w_dn[l, e].rearrange("(k p) f -> p k f", p=128), w=[("wdn", wb)])
                    K.dma("pool", bdn[wb][:], self.b_dn[l, e:e + 1, :], w=[("bdn", wb)])
                for i in range(NST):
                    xi = cnt["xi"]
                    cnt["xi"] += 1
                    x_ = xgt[xi % 3]
                    xk = ("xgt", xi % 3)
                    r0 = e * CAP + i * 128
                    K.dma("sp", x_[:], self.xg[r0:r0 + 128, :], w=[xk])
                    ti = cnt["ti"] % 2
                    cnt["ti"] += 1
                    for kc in range(8):
                        K.op("pe", lambda: nc.tensor.transpose(tpx[ti][:, kc, :], x_[:, kc * 128:(kc + 1) * 128], self.ident), r=[xk, "cbf"], w=[("tpx", ti)])
                    K.op("act", lambda: nc.scalar.copy(out=xgT[wb][:, :, i * 128:(i + 1) * 128], in_=tpx[ti][:]), r=[("tpx", ti)], w=[("xgT", wb)])

            def gateup(e):
                wb = e % 2
                for cg in range(NCG):
                    cs = slice(cg * CGW, (cg + 1) * CGW)
                    for j in range(8):
                        p = cnt["ei"] % 2
                        cnt["ei"] += 1
                        for kc in range(8):
                            K.op("pe", lambda: nc.tensor.matmul(pg[p][:, 0:CGW], lhsT=wgu[wb][:, kc, j * 128:(j + 1) * 128], rhs=xgT[wb][:, kc, cs],
                                                                start=(kc == 0), stop=(kc == 7)), r=[("wgu", wb), ("xgT", wb)], w=[("pg", p)])
                        for kc in range(8):
                            K.op("pe", lambda: nc.tensor.matmul(pu[p][:, 0:CGW], lhsT=wgu[wb][:, kc, D + j * 128:D + (j + 1) * 128], rhs=xgT[wb][:, kc, cs],
                                                                start=(kc == 0), stop=(kc == 7)), r=[("wgu", wb), ("xgT", wb)], w=[("pu", p)])
                        bg = bgu[:, e * 16 + j:e * 16 + j + 1]
                        bu = bgu[:, e * 16 + 8 + j:e * 16 + 8 + j + 1]
                        K.op("dve", lambda: nc.vector.tensor_scalar(out=g1[p][:], in0=pg[p][:, 0:CGW], scalar1=bg, scalar2=7.0, op0=ALU.add, op1=ALU.min),
                             r=[("pg", p), "bgu"], w=[("g1", p)])
                        K.op("act", lambda: nc.scalar.activation(out=sg[p][:], in_=g1[p][:], func=AF.Sigmoid, scale=1.702), r=[("g1", p)], w=[("sg", p)])
                        K.op("dve", lambda: nc.vector.tensor_scalar(out=u1[p][:], in0=pu[p][:, 0:CGW], scalar1=bu, scalar2=7.0, op0=ALU.add, op1=ALU.min),
                             r=[("pu", p), "bgu"], w=[("u1", p)])
                        K.op("dve", lambda: nc.vector.tensor_scalar(out=u1[p][:], in0=u1[p][:], scalar1=-7.0, scalar2=1.0, op0=ALU.max, op1=ALU.add),
                             r=[("u1", p)], w=[("u1", p)])
                        K.op("dve", lambda: nc.vector.tensor_tensor(out=g1[p][:], in0=g1[p][:], in1=sg[p][:], op=ALU.mult), r=[("g1", p), ("sg", p)], w=[("g1", p)])
                        K.op("dve", lambda: nc.vector.tensor_tensor(out=hT[wb][:, j, cs], in0=g1[p][:], in1=u1[p][:], op=ALU.mult),
                             r=[("g1", p), ("u1", p)], w=[("hT", wb)])

            def down(e):
                wb = e % 2
                for i in range(NST):
                    yi = cnt["yi"]
                    cnt["yi"] += 1
                    yb_ = yt[yi % 2]
                    yk = ("yt", yi % 2)
                    for half in range(2):
                        hs = slice(half * 512, (half + 1) * 512)
                        K.op("pe", lambda: nc.tensor.matmul(py[half][:], lhsT=self.ones_bf[0:1, :], rhs=bdn[wb][0:1, hs], start=True, stop=False),
                             r=[("bdn", wb), "cbf"], w=[("py", half)])
                        for fc in range(8):
                            K.op("pe", lambda: nc.tensor.matmul(py[half][:], lhsT=hT[wb][:, fc, i * 128:(i + 1) * 128], rhs=wdn[wb][:, fc, hs],
                                                                start=False, stop=(fc == 7)), r=[("hT", wb), ("wdn", wb)], w=[("py", half)])
                        if half == 0:
                            K.op("act", lambda: nc.scalar.copy(out=yb_[:, hs], in_=py[half][:]), r=[("py", half)], w=[yk])
                        else:
                            K.op("dve", lambda: nc.vector.tensor_copy(out=yb_[:, hs], in_=py[half][:]), r=[("py", half)], w=[yk])
                    r0 = e * CAP + i * 128
                    K.dma("sp", self.yb[r0:r0 + 128, :], yb_[:], r=[yk], w=[("yb", e, i)])

            prep(0)
            for e in range(NE):
                gateup(e)
                if e + 1 < NE:
                    prep(e + 1)
                down(e)
            K.barrier()
        self.exp_es.close()

    def phase_combine(self, l, src, dst):
        nc, K = self.nc, self.K
        with contextlib.ExitStack() as es:
            lnp = self.sb(es, "lnp2", [128, 2 * D], F32)
            K.dma("sp", lnp[:, 0:D], self.bc[l, BC_L2G:BC_L2G + D].partition_broadcast(128), w=["lnp2"])
            K.dma("sp", lnp[:, D:2 * D], self.bc[l, BC_L2B:BC_L2B + D].partition_broadcast(128), w=["lnp2"])
            ln_stats, ln_apply = self.layernorm(es, "ln2", beng="dve")
            NB = 3
            offi = [self.sb(es, "offi2", [128, 4], I32) for _ in range(NB)]
            gt = [self.sb(es, "gt", [128, 4], F32) for _ in range(NB)]
            yg = [self.sb(es, "yg", [128, 4, D], F32) for _ in range(NB)]
            x1t = [self.sb(es, "x1c", [128, D], F32) for _ in range(NB)]
            hb = [self.sb(es, "hb2", [128, D], F32) for _ in range(3)]
            ot = [self.sb(es, "ot", [128, D], F32) for _ in range(2)]
            def loads(t):
                b = t % NB
                tok = slice(t * 128, (t + 1) * 128)
                K.dma("sp", offi[b][:], self.offs[tok, :], w=[("offi2", b)])
                K.dma("sp", gt[b][:], self.gate_s[tok, :], w=[("gt", b)])
                K.dma("sp", x1t[b][:], self.x1[tok, :], w=[("x1c", b)])

            def gathers(t):
                b = t % NB
                for k in range(4):
                    K.dma("pool", yg[b][:, k, :], self.yb[:, :], r=[("offi2", b)], w=[("yg", b, k)],
                          indirect=dict(out_offset=None, in_offset=bass.IndirectOffsetOnAxis(offi[b][:, k:k + 1], 0)))

            def c1(t):
                b = t % NB
                K.op("act", lambda: nc.scalar.activation(out=hb[b][:], in_=yg[b][:, 0, :], func=AF.Copy, scale=gt[b][:, 0:1]),
                     r=[("yg", b, 0), ("gt", b)], w=[("hb2", b)])
                for k in range(1, 4):
                    K.op("dve", lambda: nc.vector.scalar_tensor_tensor(out=hb[b][:], in0=yg[b][:, k, :], scalar=gt[b][:, k:k + 1], in1=hb[b][:],
                                                                        op0=ALU.mult, op1=ALU.add),
                         r=[("yg", b, k), ("gt", b), ("hb2", b)], w=[("hb2", b)])
                K.op("dve", lambda: nc.vector.scalar_tensor_tensor(out=hb[b][:], in0=x1t[b][:], scalar=ALPHA, in1=hb[b][:], op0=ALU.mult, op1=ALU.add),
                     r=[("x1c", b), ("hb2", b)], w=[("hb2", b)])
                ln_stats(b, hb[b][:], ("hb2", b))

            def c2(t):
                b = t % NB
                b2 = t % 2
                tok = slice(t * 128, (t + 1) * 128)
                ln_apply(b, hb[b][:], ("hb2", b), ot[b2][:], ("ot", b2), lnp[:, 0:D], lnp[:, D:2 * D], "lnp2")
                K.dma("act", dst[tok, :], ot[b2][:], r=[("ot", b2)], w=[("dst", t)])

            loads(0)
            loads(1)
            gathers(0)
            for t in range(NT + 1):
                if t + 2 < NT:
                    loads(t + 2)
                if t + 1 < NT:
                    gathers(t + 1)
                if t < NT:
                    c1(t)
                if t >= 1:
                    c2(t - 1)
            K.barrier()


def host_consts():
    bf = ml_dtypes.bfloat16
    k = np.arange(128)[:, None]
    q = np.arange(128)[None, :]
    cb = np.zeros((128, 1024), np.float32)
    cb[:, 0:128] = np.eye(128)
    cb[:, 128:256] = (k >= q)
    cb[:, 256:384] = (k <= q)
    cb[:, 384:512] = 1.0
    cb[:, 512:640] = (k < q)
    cb[:, 640:768] = (k <= q)
    cb[:, 768:896] = (k >= q)
    cf = np.zeros((128, 1024), np.float32)
    cf[:, 0:128] = (k <= q)
    cf[:, 128:256] = (k > q)
    cf[:, 256:384] = 1.0
    cf[:, 384:512] = (q >= k)
    cf[:, 512:544] = np.arange(32)[None, :]
    zr = np.ones((32, 8), np.float32)
    zr[:, 0] = 0.0
    cf[:, 544:800] = zr.reshape(1, 256)
    zr4 = np.ones((32, 4), np.float32)
    zr4[:, 0] = 0.0
    cf[:, 800:928] = zr4.reshape(1, 128)
    half = 32
    inv = (10000.0 ** (-np.arange(half, dtype=np.float32) / half)).astype(np.float32)
    ang = np.arange(S, dtype=np.float32)[None, :] * inv[:, None]
    cos = np.cos(ang).astype(np.float32)
    sin = np.sin(ang).astype(np.float32)
    cos_t = np.concatenate([cos, cos, cos, cos], 0)
    sin_t = np.concatenate([-sin, sin, -sin, sin], 0)
    return cb.astype(bf), cf, np.ascontiguousarray(cos_t), np.ascontiguousarray(sin_t)


def host_layout(inp):
    f = lambda a: np.ascontiguousarray(np.asarray(a, dtype=np.float32))
    w_in = f(inp["w_in"])
    qk = w_in[:, :, 0:1024].reshape(L, D, 16, 2, 32)
    w_qkp = np.ascontiguousarray(qk[:, :, :, ::-1, :].reshape(L, D, 1024))
    pp = np.zeros((L, 128, PPW), np.float32)
    cw = f(inp["ssd_conv_w"])
    pp[:, :, PP_CW:PP_CW + 32] = cw.reshape(L, 4, 8, 128).transpose(0, 3, 2, 1).reshape(L, 128, 32)
    pp[:, :, PP_CB:PP_CB + 8] = f(inp["ssd_conv_b"]).reshape(L, 8, 128).transpose(0, 2, 1)
    scw = f(inp["sc_conv_w"])
    pp[:, :, PP_SCW:PP_SCW + 12] = scw.reshape(L, 3, 4, 128).transpose(0, 3, 2, 1).reshape(L, 128, 12)
    bgu = f(inp["exp_b_gu"])
    pp[:, :, PP_BGU:] = bgu.reshape(L, NE, 16, 128).transpose(0, 3, 1, 2).reshape(L, 128, NE * 16)
    bc = np.concatenate([f(inp["ssd_dt_bias"]), f(inp["ssd_a_log"]), f(inp["ssd_d"]), f(inp["ssd_norm_g"]),
                         f(inp["router_b"]), f(inp["ln1_g"]), f(inp["ln1_b"]), f(inp["ln2_g"]), f(inp["ln2_b"])], axis=1)
    assert bc.shape == (L, BCW)
    cb, cf, cos_t, sin_t = host_consts()
    shared = {
        "w_in": w_in, "w_qkp": w_qkp, "w_out": f(inp["w_out"]), "router_w": f(inp["router_w"]),
        "exp_w_gu": f(inp["exp_w_gu"]), "exp_w_down": f(inp["exp_w_down"]), "exp_b_down": f(inp["exp_b_down"]),
        "pp": pp, "bc": np.ascontiguousarray(bc), "cos_t": cos_t, "sin_t": sin_t, "cst_bf": cb, "cst_f": cf,
    }
    return shared


def kernel(**inputs):
    x = np.ascontiguousarray(np.asarray(inputs["x"], dtype=np.float32))
    shared = host_layout(inputs)
    nc = Builder().build()
    in_maps = [dict(shared, x=x[b]) for b in range(8)]
    res = run_bass_kernel_spmd(nc, in_maps, core_ids=list(range(8)))
    return np.stack([np.asarray(r["y"], dtype=np.float32) for r in res.results], axis=0)
```

```python
import contextlib
import numpy as np
import ml_dtypes
import concourse.bass as bass
import concourse.mybir as mybir
from concourse.bass_utils import run_bass_kernel_spmd

F32 = mybir.dt.float32
BF16 = mybir.dt.bfloat16
I32 = mybir.dt.int32
U32 = mybir.dt.uint32
AF = mybir.ActivationFunctionType
ALU = mybir.AluOpType
AX = mybir.AxisListType

S = 4096
D = 1024
L = 2
NT = S // 128
INW = 4616
NE = 32
CAP = 768
NSLOT = NE * CAP
XGW = 1024
ALPHA = (2.0 * L) ** 0.25
LN_EPS = 1e-5
RMS_EPS = 1e-5
PATTERNS = (1, 4, 16)

C_Q, C_K, C_V, C_Z, C_XS, C_B, C_C, C_DT, C_SB, C_SC, C_SH = (
    0, 512, 1024, 1536, 2048, 2560, 2816, 3072, 3080, 3592, 4104)

PP_CW, PP_CB, PP_SCW, PP_BGU = 0, 32, 40, 52
PPW = 52 + NE * 16
BC_DTB, BC_ALOG, BC_D, BC_NG, BC_RB, BC_L1G, BC_L1B, BC_L2G, BC_L2B = (
    0, 8, 16, 24, 536, 568, 1592, 2616, 3640)
BCW = 4664


class Sched:
    def __init__(self, nc, n_dma_sems=40):
        self.nc = nc
        self.h = {"pe": nc.tensor, "act": nc.scalar, "dve": nc.vector, "pool": nc.gpsimd, "sp": nc.sync}
        self.sem = {k: nc.alloc_semaphore("prog_" + k) for k in self.h}
        self.cnt = {k: 0 for k in self.h}
        self.seen = {k: {} for k in self.h}
        self.dsem = [nc.alloc_semaphore("dma%d" % i) for i in range(n_dma_sems)]
        self.dval = [0] * n_dma_sems
        self.dpool = {"sp": list(range(0, 16)), "pool": list(range(16, 32)), "act": list(range(32, n_dma_sems))}
        self.drr = {"sp": 0, "pool": 0, "act": 0}
        self.res = {}

    def _wait(self, eng, tok):
        kind, key, val = tok
        if kind == "e" and key == eng:
            return
        sk = (kind, key)
        if self.seen[eng].get(sk, 0) >= val:
            return
        sem = self.sem[key] if kind == "e" else self.dsem[key]
        self.h[eng].wait_ge(sem, val)
        self.seen[eng][sk] = val

    def _deps(self, eng, r, w):
        for k in r:
            st = self.res.get(k)
            if st and st[0] is not None:
                tok = st[0]
                if tok[0] == "e" and tok[1] == eng:
                    if tok[2] > self.cnt[eng] - 2 and eng != "pe":
                        sk = ("e", eng)
                        if self.seen[eng].get(sk, 0) < tok[2]:
                            self.h[eng].wait_ge(self.sem[eng], tok[2])
                            self.seen[eng][sk] = tok[2]
                else:
                    self._wait(eng, tok)
        for k in w:
            st = self.res.get(k)
            if st:
                if st[0] is not None:
                    self._wait(eng, st[0])
                for tok in st[1]:
                    self._wait(eng, tok)

    def _commit(self, tok, r, w):
        for k in r:
            st = self.res.setdefault(k, [None, []])
            st[1] = [t for t in st[1] if (t[0], t[1]) != (tok[0], tok[1])] + [tok]
        for k in w:
            self.res[k] = [tok, []]

    def op(self, eng, fn, r=(), w=()):
        self._deps(eng, r, w)
        ins = fn()
        self.cnt[eng] += 1
        ins.then_inc(self.sem[eng], 1)
        self._commit(("e", eng, self.cnt[eng]), r, w)
        return ins

    def dma(self, q, out, in_, r=(), w=(), indirect=None):
        pl = self.dpool[q]
        i = pl[self.drr[q] % len(pl)]
        self.drr[q] += 1
        if self.dval[i] > 0:
            self._wait(q, ("d", i, self.dval[i]))
        self._deps(q, r, w)
        if indirect is None:
            ins = self.h[q].dma_start(out=out, in_=in_)
        else:
            ins = self.nc.gpsimd.indirect_dma_start(out=out, in_=in_, **indirect)
        self.dval[i] += 16
        ins.then_inc(self.dsem[i], 16)
        self._commit(("d", i, self.dval[i]), r, w)
        return ins

    def barrier(self):
        for e in self.h:
            for o in self.h:
                if o != e and self.cnt[o] > 0:
                    self._wait(e, ("e", o, self.cnt[o]))
            for i, v in enumerate(self.dval):
                if v > 0:
                    self._wait(e, ("d", i, v))
        self.res = {}


class Builder:
    def __init__(self, debug=(), nlayers=L, stop_after=None, with_moe=True):
        self.debug = set(debug)
        self.with_moe = with_moe
        self.nlayers = nlayers
        self.stop_after = stop_after
        self.nc = nc = bass.Bass("TRN2", target_bir_lowering=False)
        self.K = Sched(nc)
        self.uid = 0
        ein = lambda n, s, d: nc.dram_tensor(n, s, d, kind="ExternalInput").ap()
        self.x = ein("x", [S, D], F32)
        self.w_in = ein("w_in", [L, D, INW], F32)
        self.w_qkp = ein("w_qkp", [L, D, 1024], F32)
        self.w_out = ein("w_out", [L, 1536, D], F32)
        self.router_w = ein("router_w", [L, D, NE], F32)
        if with_moe:
            self.w_gu = ein("exp_w_gu", [L, NE, D, 2 * D], F32)
            self.w_dn = ein("exp_w_down", [L, NE, D, D], F32)
            self.b_dn = ein("exp_b_down", [L, NE, D], F32)
        self.pp = ein("pp", [L, 128, PPW], F32)
        self.bc = ein("bc", [L, BCW], F32)
        self.cos_t = ein("cos_t", [128, S], F32)
        self.sin_t = ein("sin_t", [128, S], F32)
        self.cst_bf = ein("cst_bf", [128, 1024], BF16)
        self.cst_f = ein("cst_f", [128, 1024], F32)
        self.y = nc.dram_tensor("y", [S, D], F32, kind="ExternalOutput").ap()
        self.qT = self.scr("qT", [4, 128, S], BF16)
        self.kT = self.scr("kT", [4, 128, S], BF16)
        self.v_s = self.scr("v_s", [S, 8 * 66], BF16)
        self.z_s = self.scr("z_s", [S, 512], F32)
        self.dt_s = self.scr("dt_s", [S, 8], F32)
        self.xbc = self.scr("xbc", [8, 128, S], BF16)
        self.mixT = self.scr("mixT", [12, 128, S], BF16)
        self.o_s = self.scr("o_s", [3, S, 8 * 65], F32)
        self.x1 = self.scr("x1", [S, D], F32)
        self.xg = self.scr("xg", [NSLOT, XGW], BF16)
        self.yb = self.scr("yb", [NSLOT, D], F32)
        self.offs = self.scr("offs", [S, 4], I32)
        self.gate_s = self.scr("gate_s", [S, 4], F32)
        self.xres = self.scr("xres", [S, D], F32)

    def scr(self, name, shape, dt):
        kind = "ExternalOutput" if name in self.debug else "Internal"
        return self.nc.dram_tensor(name, shape, dt, kind=kind).ap()

    def sb(self, es, name, shape, dt):
        self.uid += 1
        return es.enter_context(self.nc.sbuf_tensor("%s_%d" % (name, self.uid), shape, dt))

    def ps(self, es, name, shape, dt):
        self.uid += 1
        return es.enter_context(self.nc.psum_tensor("%s_%d" % (name, self.uid), shape, dt))

    def build(self):
        nc, K = self.nc, self.K
        with contextlib.ExitStack() as es:
            self.cbf = self.sb(es, "cbf", [128, 1024], BF16)
            self.cf = self.sb(es, "cf", [128, 1024], F32)
            K.dma("sp", self.cbf[:], self.cst_bf[:, :], w=["cbf"])
            K.dma("sp", self.cf[:], self.cst_f[:, :], w=["cf"])
            self.ident = self.cbf[:, 0:128]
            self.maskpc = self.cbf[:, 128:384]
            self.ones_bf = self.cbf[:, 384:512]
            self.sl_bf = self.cbf[:, 512:640]
            self.maskcp = self.cbf[:, 640:896]
            self.U_f = self.cf[:, 0:128]
            self.SL_f = self.cf[:, 128:256]
            self.ones_f = self.cf[:, 256:384]
            self.mls_f = self.cf[:, 384:512]
            self.iota_e = self.cf[:, 512:544]
            self.zr_f = self.cf[:, 800:928]
            for l in range(self.nlayers):
                src = self.x if l == 0 else self.xres
                dst = self.y if l == self.nlayers - 1 else self.xres
                self.layer(l, src, dst)
                if self.stop_after is not None:
                    break
            K.barrier()
        return nc

    def layer(self, l, src, dst):
        K = self.K
        phases = [self.phase_proj, self.phase_attn, self.phase_ssd, self.phase_outln,
                  self.phase_route, self.phase_experts, self.phase_combine]
        for i, ph in enumerate(phases):
            ph(l, src, dst)
            K.barrier()
            if self.stop_after is not None and i >= self.stop_after:
                if getattr(self, "exp_es", None) is not None and 3 <= i < 5:
                    self.exp_es.close()
                if i == 1:
                    self.ssd_es.close()
                return

    def phase_proj(self, l, src, dst):
        nc, K = self.nc, self.K
        with contextlib.ExitStack() as es:
            xT = self.sb(es, "xT", [128, 8, S], BF16)
            ppt = self.sb(es, "ppt", [128, 52], F32)
            bct = self.sb(es, "bct", [128, 8], F32)
            K.dma("sp", ppt[:], self.pp[l, :, 0:52], w=["ppt"])
            K.dma("sp", bct[:], self.bc[l, BC_DTB:BC_DTB + 8].partition_broadcast(128), w=["bct"])
            sup_src = [("q", self.w_in[l, :, C_Q:C_Q + 512]), ("qp", self.w_qkp[l, :, 0:512]),
                       ("k", self.w_in[l, :, C_K:C_K + 512]), ("kp", self.w_qkp[l, :, 512:1024]),
                       ("xs0", self.w_in[l, :, C_XS:C_XS + 512]), ("xs1", self.w_in[l, :, C_XS + 512:C_XS + 1024]),
                       ("sc", self.w_in[l, :, C_SC:C_SC + 512]), ("sh", self.w_in[l, :, C_SH:C_SH + 512]),
                       ("sb", self.w_in[l, :, C_SB:C_SB + 512])]
            sup = {}
            for nm, _ in sup_src:
                sup[nm] = self.sb(es, "sup_" + nm, [128, 8, 512], BF16)
            sup_when = {6: ["q", "qp"], 12: ["k", "kp"], 18: ["xs0", "xs1"], 24: ["sc", "sh", "sb"]}
            sup_ap = dict(sup_src)

            def load_sup(nm):
                K.dma("pool", sup[nm][:], sup_ap[nm].rearrange("(k p) c -> p k c", p=128), w=[("sup", nm)])

            with contextlib.ExitStack() as es1:
                xtb = [self.sb(es1, "xtb", [128, D], BF16) for _ in range(3)]
                tp = [self.ps(es1, "tp", [128, 8, 128], BF16) for _ in range(2)]
                for t in range(NT):
                    xb = xtb[t % 3]
                    K.dma("pool", xb[:], src[t * 128:(t + 1) * 128, :], w=[("xtb", t % 3)])
                    for nm in sup_when.get(t, []):
                        load_sup(nm)
                    p = tp[t % 2]
                    for kc in range(8):
                        K.op("pe", lambda: nc.tensor.transpose(p[:, kc, :], xb[:, kc * 128:(kc + 1) * 128], self.ident),
                             r=[("xtb", t % 3), "cbf"], w=[("tp", t % 2)])
                    eng = "act" if t % 2 == 0 else "dve"
                    if eng == "act":
                        K.op("act", lambda: nc.scalar.copy(out=xT[:, :, t * 128:(t + 1) * 128], in_=p[:]),
                             r=[("tp", t % 2)], w=[("xT", t // 4)])
                    else:
                        K.op("dve", lambda: nc.vector.tensor_copy(out=xT[:, :, t * 128:(t + 1) * 128], in_=p[:]),
                             r=[("tp", t % 2)], w=[("xT", t // 4)])
                K.barrier()

            def proj_fm(es2, wsrc_list, evac, nacc=1, tag="fm"):
                acc = [[self.ps(es2, "acc", [128, 512], F32) for _ in range(nacc)] for _ in range(2 if nacc > 1 else 4)]
                nb = len(acc)
                it = 0
                for j, grp in enumerate(wsrc_list):
                    for n in range(8):
                        b = it % nb
                        it += 1
                        for a in range(nacc):
                            nm, c0 = grp[a]
                            for kc in range(8):
                                K.op("pe", lambda: nc.tensor.matmul(
                                    acc[b][a][:], lhsT=sup[nm][:, kc, c0:c0 + 128],
                                    rhs=xT[:, kc, n * 512:(n + 1) * 512], start=(kc == 0), stop=(kc == 7)),
                                    r=[("sup", nm), ("xT", n)], w=[(tag + "acc", b, a)])
                        evac(j, n, acc[b], [(tag + "acc", b, a) for a in range(nacc)])

            with contextlib.ExitStack() as es2:
                cos = self.sb(es2, "cos", [128, S], F32)
                sin = self.sb(es2, "sin", [128, S], F32)
                K.dma("sp", cos[:], self.cos_t[:, :], w=["cos"])
                K.dma("sp", sin[:], self.sin_t[:, :], w=["sin"])
                t1 = [self.sb(es2, "t1", [128, 512], F32) for _ in range(2)]
                t2 = [self.sb(es2, "t2", [128, 512], F32) for _ in range(2)]
                ob = [self.sb(es2, "ob", [128, 512], BF16) for _ in range(3)]
                groups = []
                for a_, b_ in (("q", "qp"), ("k", "kp")):
                    for jp in range(4):
                        groups.append([(a_, jp * 128), (b_, jp * 128)])
                cnt = [0]

                def evac_qk(j, n, ps, psr):
                    i = cnt[0]
                    cnt[0] += 1
                    a, b, o = t1[i % 2], t2[i % 2], ob[i % 3]
                    K.op("dve", lambda: nc.vector.tensor_tensor(out=a[:], in0=ps[0][:], in1=cos[:, n * 512:(n + 1) * 512], op=ALU.mult),
                         r=[psr[0], "cos"], w=[("t1", i % 2)])
                    K.op("dve", lambda: nc.vector.tensor_tensor(out=b[:], in0=ps[1][:], in1=sin[:, n * 512:(n + 1) * 512], op=ALU.mult),
                         r=[psr[1], "sin"], w=[("t2", i % 2)])
                    K.op("pool", lambda: nc.gpsimd.tensor_tensor(out=o[:], in0=a[:], in1=b[:], op=ALU.add),
                         r=[("t1", i % 2), ("t2", i % 2)], w=[("ob", i % 3)])
                    dstT = self.qT if j < 4 else self.kT
                    K.dma("sp", dstT[j % 4, :, n * 512:(n + 1) * 512], o[:], r=[("ob", i % 3)], w=[("qk", j, n)])

                proj_fm(es2, groups, evac_qk, nacc=2, tag="qk")
                K.barrier()

            with contextlib.ExitStack() as es2:
                R = [self.sb(es2, "R", [128, S + 4], F32) for _ in range(2)]
                accb = [self.sb(es2, "accb", [128, 1024], F32) for _ in range(2)]
                outb = [self.sb(es2, "outb", [128, 1024], BF16) for _ in range(2)]
                stg = [self.sb(es2, "stg", [128, 512], BF16) for _ in range(2)]
                for i in range(2):
                    K.op("dve", lambda: nc.vector.memset(R[i][:, 0:4], 0.0), w=[("R", i)])
                segc = [0]

                def conv_row(Rb, rkey, wcols, nk, seg_out):
                    for sgi in range(4):
                        i = segc[0]
                        segc[0] += 1
                        a = accb[i % 2]
                        t0 = sgi * 1024
                        for k in range(nk):
                            sh = 4 - (nk - 1) + k
                            src_ap = Rb[:, t0 + sh: t0 + sh + 1024]
                            if k == 0:
                                K.op("dve", lambda: nc.vector.tensor_scalar(out=a[:], in0=src_ap, scalar1=wcols[k], scalar2=None, op0=ALU.mult),
                                     r=[rkey, "ppt"], w=[("accb", i % 2)])
                            else:
                                K.op("dve", lambda: nc.vector.scalar_tensor_tensor(out=a[:], in0=src_ap, scalar=wcols[k], in1=a[:], op0=ALU.mult, op1=ALU.add),
                                     r=[rkey, "ppt", ("accb", i % 2)], w=[("accb", i % 2)])
                        seg_out(sgi, a, ("accb", i % 2), i)

                def evac_xbc(j, n, ps, psr):
                    K.op("act", lambda: nc.scalar.copy(out=R[j % 2][:, 4 + n * 512: 4 + (n + 1) * 512], in_=ps[0][:]),
                         r=[psr[0]], w=[("R", j % 2)])
                    if n == 7:
                        def seg_out(sgi, a, akey, i):
                            o = outb[i % 2]
                            K.op("act", lambda: nc.scalar.activation(out=o[:], in_=a[:], func=AF.Silu, bias=ppt[:, PP_CB + j: PP_CB + j + 1], scale=1.0),
                                 r=[akey, "ppt"], w=[("outb", i % 2)])
                            K.dma("sp", self.xbc[j, :, sgi * 1024:(sgi + 1) * 1024], o[:], r=[("outb", i % 2)], w=[("xbc", j, sgi)])
                        conv_row(R[j % 2], ("R", j % 2), [ppt[:, PP_CW + j * 4 + k: PP_CW + j * 4 + k + 1] for k in range(4)], 4, seg_out)

                groups = [[("xs%d" % (j // 4), (j % 4) * 128)] for j in range(8)]
                proj_fm(es2, groups, evac_xbc, nacc=1, tag="xbc")

                def evac_sconv(jj, n, ps, psr):
                    j, which = jj // 3, jj % 3
                    cols = slice(4 + n * 512, 4 + (n + 1) * 512)
                    if which == 0:
                        K.op("act", lambda: nc.scalar.copy(out=R[0][:, cols], in_=ps[0][:]), r=[psr[0]], w=[("R", 0)])
                    elif which == 1:
                        K.op("dve", lambda: nc.vector.tensor_tensor(out=R[1][:, cols], in0=ps[0][:], in1=R[0][:, cols], op=ALU.mult),
                             r=[psr[0], ("R", 0)], w=[("R", 1)])
                        if n == 7:
                            def seg_out(sgi, a, akey, i):
                                K.op("pool", lambda: nc.gpsimd.tensor_copy(out=R[0][:, 4 + sgi * 1024: 4 + (sgi + 1) * 1024], in_=a[:]),
                                     r=[akey], w=[("R", 0)])
                            conv_row(R[1], ("R", 1), [ppt[:, PP_SCW + j * 3 + k: PP_SCW + j * 3 + k + 1] for k in range(3)], 3, seg_out)
                    else:
                        i = segc[0]
                        segc[0] += 1
                        o = stg[i % 2]
                        K.op("dve", lambda: nc.vector.tensor_tensor(out=o[:], in0=ps[0][:], in1=R[0][:, cols], op=ALU.mult),
                             r=[psr[0], ("R", 0)], w=[("stg", i % 2)])
                        K.dma("sp", self.mixT[8 + j, :, n * 512:(n + 1) * 512], o[:], r=[("stg", i % 2)], w=[("mixT", 8 + j, n)])

                groups = []
                for j in range(4):
                    for nm in ("sc", "sh", "sb"):
                        groups.append([(nm, j * 128)])
                proj_fm(es2, groups, evac_sconv, nacc=1, tag="sc")
                K.barrier()

            with contextlib.ExitStack() as es2:
                wv = self.sb(es2, "wv", [128, 8, 512], BF16)
                wz = self.sb(es2, "wz", [128, 8, 512], BF16)
                wd = self.sb(es2, "wd", [128, 8, 8], BF16)
                K.dma("pool", wv[:], self.w_in[l, :, C_V:C_V + 512].rearrange("(k p) c -> p k c", p=128), w=["wv"])
                K.dma("pool", wz[:], self.w_in[l, :, C_Z:C_Z + 512].rearrange("(k p) c -> p k c", p=128), w=["wz"])
                K.dma("pool", wd[:], self.w_in[l, :, C_DT:C_DT + 8].rearrange("(k p) c -> p k c", p=128), w=["wd"])
                pv = [self.ps(es2, "pv", [128, 512], F32) for _ in range(2)]
                pz = [self.ps(es2, "pz", [128, 512], F32) for _ in range(2)]
                pd = [self.ps(es2, "pd", [128, 8], F32) for _ in range(2)]
                vst = [self.sb(es2, "vst", [128, 8, 66], BF16) for _ in range(2)]
                zst = [self.sb(es2, "zst", [128, 512], F32) for _ in range(2)]
                dtall = self.sb(es2, "dtall", [128, NT, 8], F32)
                for i in range(2):
                    K.op("dve", lambda: nc.vector.memset(vst[i][:], 1.0), w=[("vst", i)])
                for t in range(NT):
                    b = t % 2
                    tok = slice(t * 128, (t + 1) * 128)
                    for (wt_, wk, pt, pk, ncol) in ((wv, "wv", pv, "pv", 512), (wz, "wz", pz, "pz", 512), (wd, "wd", pd, "pd", 8)):
                        for kc in range(8):
                            K.op("pe", lambda: nc.tensor.matmul(pt[b][:], lhsT=xT[:, kc, tok], rhs=wt_[:, kc, :], start=(kc == 0), stop=(kc == 7)),
                                 r=[wk, ("xT", t // 4)], w=[(pk, b)])
                    K.op("act", lambda: nc.scalar.copy(out=vst[b][:, :, 0:64], in_=pv[b][:].rearrange("p (h c) -> p h c", h=8)),
                         r=[("pv", b)], w=[("vst", b)])
                    K.dma("sp", self.v_s[tok, :], vst[b][:].rearrange("p h c -> p (h c)"), r=[("vst", b)], w=[("v_s", t)])
                    K.op("act", lambda: nc.scalar.activation(out=zst[b][:], in_=pz[b][:], func=AF.Silu),
                         r=[("pz", b)], w=[("zst", b)])
                    K.dma("sp", self.z_s[tok, :], zst[b][:], r=[("zst", b)], w=[("z_s", t)])
                    K.op("dve", lambda: nc.vector.tensor_tensor(out=dtall[:, t, :], in0=pd[b][:], in1=bct[:], op=ALU.add),
                         r=[("pd", b), "bct"], w=["dtall"])
                dtf = dtall[:].rearrange("p t h -> p (t h)")
                K.op("act", lambda: nc.scalar.activation(out=dtf, in_=dtf, func=AF.Exp), r=["dtall"], w=["dtall"])
                K.op("act", lambda: nc.scalar.activation(out=dtf, in_=dtf, func=AF.Ln, bias=1.0, scale=1.0), r=["dtall"], w=["dtall"])
                K.dma("sp", self.dt_s.rearrange("(t p) h -> p t h", p=128), dtall[:], r=["dtall"], w=["dt_s"])
                K.barrier()

    def phase_attn(self, l, src, dst):
        nc, K = self.nc, self.K
        self.ssd_es = contextlib.ExitStack()
        self.XB = self.sb(self.ssd_es, "XB", [128, 8, S], BF16)
        with contextlib.ExitStack() as es:
            QT = self.sb(es, "QT", [128, 4, S], BF16)
            KT = self.sb(es, "KT", [128, 4, S], BF16)
            for p in range(4):
                K.dma("sp", QT[:, p, :], self.qT[p, :, :], w=["QT"])
                K.dma("sp", KT[:, p, :], self.kT[p, :, :], w=["KT"])
            for j in range(8):
                K.dma("act", self.XB[:, j, :], self.xbc[j, :, :], w=["XBpre"])
            Vd = self.sb(es, "Vd", [128, 32, 8 * 66], BF16)
            pS = [self.ps(es, "pS", [128, 256], F32) for _ in range(4)]
            pO = [self.ps(es, "pO", [128, 4 * 65], F32) for _ in range(4)]
            pt = [self.sb(es, "pt", [128, 256], BF16) for _ in range(6)]
            pmk = [self.sb(es, "pmk", [128, 256], BF16) for _ in range(24)]
            ost = [self.sb(es, "ost", [128, 8, 65], F32) for _ in range(2)]
            LAG = 3
            NBUF = 6
            for di, d in enumerate(PATTERNS):
                nb = 32 // d
                vsrc = self.v_s.rearrange("(n j dd) c -> dd j n c", j=128, dd=d)
                for r in range(d):
                    K.dma("sp", Vd[:, r * nb:(r + 1) * nb, :], vsrc[r], w=["Vd"])
                items = [(r, n, h) for r in range(d) for n in range(nb) for h in range(8)]
                N = len(items)

                def geom(r, n):
                    b = r * nb + n
                    base = r + d * 128 * n
                    cols = slice(base, base + d * 127 + 1, d)
                    pcols = slice(base - d * 128, base - d * 128 + d * 127 + 1, d)
                    return b, cols, pcols

                for s_ in range(N + LAG):
                    if s_ < N:
                        r, n, h = items[s_]
                        b, cols, pcols = geom(r, n)
                        i = s_ % NBUF
                        sl_ = (n % 3) * 8 + h
                        width = 256 if n + 1 < nb else 128
                        base = r + d * 128 * n
                        qcols = slice(base, base + d * (width - 1) + 1, d)
                        pair, pb = h // 2, 64 * (h % 2)
                        K.op("pe", lambda: nc.tensor.matmul(pS[i % 4][:, 0:width], lhsT=KT[pb:pb + 64, pair, cols],
                                                            rhs=QT[pb:pb + 64, pair, qcols], start=True, stop=True),
                             r=["QT", "KT"], w=[("pS", i % 4)])
                        K.op("act", lambda: nc.scalar.activation(out=pt[i][:, 0:width], in_=pS[i % 4][:, 0:width], func=AF.Exp, scale=0.125),
                             r=[("pS", i % 4)], w=[("pt", i)])
                        K.op("dve", lambda: nc.vector.tensor_tensor(out=pmk[sl_][:, 0:width], in0=pt[i][:, 0:width], in1=self.maskcp[:, 0:width], op=ALU.mult),
                             r=[("pt", i), "cbf"], w=[("pmk", sl_)])
                    if s_ >= LAG:
                        r, n, h = items[s_ - LAG]
                        b, cols, pcols = geom(r, n)
                        bi = b
                        oi = (bi % 2) * 2 + h // 4
                        O = pO[oi][:, (h % 4) * 65:(h % 4 + 1) * 65]
                        slc = (n % 3) * 8 + h
                        slp = ((n - 1) % 3) * 8 + h
                        if n > 0:
                            K.op("pe", lambda: nc.tensor.matmul(O, lhsT=pmk[slp][:, 128:256], rhs=Vd[:, b - 1, h * 66:h * 66 + 65], start=True, stop=False),
                                 r=[("pmk", slp), "Vd"], w=[("pO", oi)])
                        K.op("pe", lambda: nc.tensor.matmul(O, lhsT=pmk[slc][:, 0:128], rhs=Vd[:, b, h * 66:h * 66 + 65], start=(n == 0), stop=True),
                             r=[("pmk", slc), "Vd"], w=[("pO", oi)])
                        if h % 4 == 3:
                            hh = h // 4
                            dst_ap = ost[bi % 2][:, hh * 4:(hh + 1) * 4, :]
                            src_ap = pO[oi][:].rearrange("p (h c) -> p h c", h=4)
                            K.op("dve", lambda: nc.vector.tensor_copy(out=dst_ap, in_=src_ap), r=[("pO", oi)], w=[("ost", bi % 2, hh)])
                        if h == 7:
                            K.dma("sp", self.o_s[di, cols, :], ost[bi % 2][:].rearrange("p h c -> p (h c)"),
                                  r=[("ost", bi % 2, 0), ("ost", bi % 2, 1)], w=[("o_s", di, b)])
            K.barrier()
        with contextlib.ExitStack() as es:
            o3 = [self.sb(es, "o3", [128, 3, 520], F32) for _ in range(4)]
            nm = [self.sb(es, "nm", [128, 8, 65], F32) for _ in range(2)]
            rd = [self.sb(es, "rd", [128, 8], F32) for _ in range(2)]
            at = [self.sb(es, "at", [128, 8, 64], BF16) for _ in range(2)]
            tpo = [self.ps(es, "tpo", [128, 4, 128], BF16) for _ in range(2)]
            atT = [self.sb(es, "atT", [128, 4, 512], BF16) for _ in range(2)]
            for t in range(NT):
                b = t % 2
                b4 = t % 4
                tok = slice(t * 128, (t + 1) * 128)
                K.dma("sp", o3[b4][:], self.o_s[:, tok, :].rearrange("d t c -> t d c"), w=[("o3", b4)])
                nmf = nm[b][:].rearrange("p h c -> p (h c)")
                K.op("dve", lambda: nc.vector.tensor_tensor(out=nmf, in0=o3[b4][:, 0, :], in1=o3[b4][:, 1, :], op=ALU.add),
                     r=[("o3", b4)], w=[("nm", b)])
                K.op("dve", lambda: nc.vector.tensor_tensor(out=nmf, in0=nmf, in1=o3[b4][:, 2, :], op=ALU.add),
                     r=[("o3", b4), ("nm", b)], w=[("nm", b)])
                K.op("dve", lambda: nc.vector.reciprocal(out=rd[b][:], in_=nm[b][:, :, 64]), r=[("nm", b)], w=[("rd", b)])
                K.op("dve", lambda: nc.vector.tensor_tensor(out=at[b][:], in0=nm[b][:, :, 0:64],
                                                            in1=rd[b][:].unsqueeze(2).broadcast_to([128, 8, 64]), op=ALU.mult),
                     r=[("nm", b), ("rd", b)], w=[("at", b)])
                atf = at[b][:].rearrange("p h c -> p (h c)")
                for c in range(4):
                    K.op("pe", lambda: nc.tensor.transpose(tpo[b][:, c, :], atf[:, c * 128:(c + 1) * 128], self.ident),
                         r=[("at", b), "cbf"], w=[("tpo", b)])
                g = (t // 4) % 2
                K.op("act", lambda: nc.scalar.copy(out=atT[g][:, :, (t % 4) * 128:(t % 4 + 1) * 128], in_=tpo[b][:]),
                     r=[("tpo", b)], w=[("atT", g)])
                if t % 4 == 3:
                    K.dma("act", self.mixT[0:4, :, (t // 4) * 512:(t // 4 + 1) * 512].rearrange("c p t -> p c t"), atT[g][:],
                          r=[("atT", g)], w=[("mixT", 0, t // 4)])
            K.barrier()

    def phase_ssd(self, l, src, dst):
        nc, K = self.nc, self.K
        bcast = lambda ap, shape, ax: ap.unsqueeze(ax).broadcast_to(shape)
        with contextlib.ExitStack() as es:
            XB = self.XB
            dtt = self.sb(es, "dtt", [128, 32, 8], F32)
            K.dma("sp", dtt[:], self.dt_s.rearrange("(c p) h -> p c h", p=128), w=["dtt"])
            prm = self.sb(es, "prm", [128, 24 + 512], F32)
            K.dma("sp", prm[:, 0:8], self.bc[l, BC_ALOG:BC_ALOG + 8].partition_broadcast(128), w=["prm"])
            K.dma("sp", prm[:, 16:24], self.bc[l, BC_D:BC_D + 8].partition_broadcast(128), w=["prm"])
            K.dma("sp", prm[:, 24:536], self.bc[l, BC_NG:BC_NG + 512].partition_broadcast(128), w=["prm"])
            a_bc, d_bc, ng_bc = prm[:, 8:16], prm[:, 16:24], prm[:, 24:536]
            A = self.sb(es, "A", [128, 32, 8], F32)
            cum = self.sb(es, "cum", [128, 32, 8], F32)
            clast = self.sb(es, "clast", [128, 32, 8], F32)
            decst = self.sb(es, "decst", [128, 32, 8], F32)
            dtdec = self.sb(es, "dtdec", [128, 32, 8], F32)
            ecum = self.sb(es, "ecum", [128, 32, 8], F32)
            cdec = self.sb(es, "cdec", [128, 32, 8], F32)
            fl = lambda t: t[:].rearrange("p c h -> p (c h)")
            K.op("act", lambda: nc.scalar.activation(out=prm[:, 8:16], in_=prm[:, 0:8], func=AF.Exp), r=["prm"], w=["prm2"])
            K.op("dve", lambda: nc.vector.tensor_scalar(out=prm[:, 8:16], in0=prm[:, 8:16], scalar1=-1.0, scalar2=None, op0=ALU.mult),
                 r=["prm2"], w=["prm2"])
            K.op("dve", lambda: nc.vector.tensor_tensor(out=A[:], in0=dtt[:], in1=bcast(a_bc, [128, 32, 8], 1), op=ALU.mult),
                 r=["prm2", "dtt"], w=["A"])
            with contextlib.ExitStack() as es1:
                pc = self.ps(es1, "pc", [128, 256], F32)
                pl = self.ps(es1, "pl", [128, 256], F32)
                K.op("pe", lambda: nc.tensor.matmul(pc[:], lhsT=self.U_f, rhs=fl(A), start=True, stop=True), r=["A", "cf"], w=["pc"])
                K.op("pe", lambda: nc.tensor.matmul(pl[:], lhsT=self.ones_f, rhs=fl(A), start=True, stop=True), r=["A", "cf"], w=["pl"])
                K.op("dve", lambda: nc.vector.tensor_copy(out=fl(cum), in_=pc[:]), r=["pc"], w=["cum"])
                K.op("dve", lambda: nc.vector.tensor_copy(out=fl(clast), in_=pl[:]), r=["pl"], w=["clast"])
                K.op("dve", lambda: nc.vector.tensor_tensor(out=fl(decst), in0=fl(clast), in1=fl(cum), op=ALU.subtract),
                     r=["cum", "clast"], w=["decst"])
                K.op("act", lambda: nc.scalar.activation(out=fl(decst), in_=fl(decst), func=AF.Exp), r=["decst"], w=["decst"])
                K.op("act", lambda: nc.scalar.activation(out=fl(ecum), in_=fl(cum), func=AF.Exp), r=["cum"], w=["ecum"])
                K.op("act", lambda: nc.scalar.activation(out=fl(cdec), in_=fl(clast), func=AF.Exp), r=["clast"], w=["cdec"])
                K.op("dve", lambda: nc.vector.tensor_tensor(out=fl(dtdec), in0=fl(decst), in1=fl(dtt), op=ALU.mult),
                     r=["decst", "dtt"], w=["dtdec"])
                K.barrier()
            tpx = self.ps(es, "tpx", [128, 6, 128], BF16)
            pG = self.ps(es, "pG", [128, 2, 128], F32)
            pSeg = [self.ps(es, "pSeg", [128, 4, 128], F32) for _ in range(2)]
            pYd = self.ps(es, "pYd", [128, 8, 64], F32)
            pYo = self.ps(es, "pYo", [128, 8, 64], F32)
            pSt = self.ps(es, "pSt", [128, 8, 64], F32)
            tpy = self.ps(es, "tpy", [128, 4, 128], BF16)
            X = [self.sb(es, "X", [128, 8, 64], BF16) for _ in range(3)]
            Xd = [self.sb(es, "Xd", [128, 8, 64], BF16) for _ in range(3)]
            xh = [self.sb(es, "xh", [128, 8, 64], BF16) for _ in range(3)]
            Bt = [self.sb(es, "Bt", [128, 2, 128], BF16) for _ in range(3)]
            GmT = [self.sb(es, "GmT", [128, 2, 128], F32) for _ in range(3)]
            lh = [self.sb(es, "lh", [128, 128], F32) for _ in range(4)]
            eL = [self.sb(es, "eL", [128, 4, 128], F32) for _ in range(6)]
            scT = [self.sb(es, "scT", [128, 4, 128], BF16) for _ in range(6)]
            yo = [self.sb(es, "yo", [128, 8, 64], F32) for _ in range(2)]
            yy = [self.sb(es, "yy", [128, 8, 64], F32) for _ in range(2)]
            td = [self.sb(es, "td", [128, 8, 64], F32) for _ in range(2)]
            zt = [self.sb(es, "zt", [128, 512], F32) for _ in range(3)]
            junk = self.sb(es, "junk", [128, 256], F32)
            ss = [self.sb(es, "ss", [128, 2], F32) for _ in range(2)]
            yn = [self.sb(es, "yn", [128, 512], BF16) for _ in range(2)]
            sdT = [self.sb(es, "sdT", [128, 4, 512], BF16) for _ in range(2)]
            hf = self.sb(es, "hf", [128, 8, 64], F32)
            hbf = [self.sb(es, "hbf", [128, 8, 64], BF16) for _ in range(2)]
            lic = [0]

            def stage_a(c):
                b = c % 2
                b3 = c % 3
                tok = slice(c * 128, (c + 1) * 128)
                K.dma("sp", zt[b3][:], self.z_s[tok, :], w=[("zt", b3)])
                for j in range(6):
                    K.op("pe", lambda: nc.tensor.transpose(tpx[:, j, :], XB[:, j, tok], self.ident), r=["XB", "cbf"], w=["tpx"])
                tx = tpx[:, 0:4, :].rearrange("p a (b c) -> p (a b) c", b=2)
                K.op("act", lambda: nc.scalar.copy(out=xh[b3][:], in_=tx), r=["tpx"], w=[("xh", b3)])
                K.op("act", lambda: nc.scalar.copy(out=Bt[b3][:], in_=tpx[:, 4:6, :]), r=["tpx"], w=[("Bt", b3)])
                K.op("dve", lambda: nc.vector.tensor_tensor(out=X[b3][:], in0=xh[b3][:], in1=bcast(dtt[:, c, :], [128, 8, 64], 2), op=ALU.mult),
                     r=[("xh", b3), "dtt"], w=[("X", b3)])
                K.op("dve", lambda: nc.vector.tensor_tensor(out=Xd[b3][:], in0=xh[b3][:], in1=bcast(dtdec[:, c, :], [128, 8, 64], 2), op=ALU.mult),
                     r=[("xh", b3), "dtdec"], w=[("Xd", b3)])
                for g in range(2):
                    K.op("pe", lambda: nc.tensor.matmul(pG[:, g, :], lhsT=XB[:, 4 + g, tok], rhs=XB[:, 6 + g, tok], start=True, stop=True),
                         r=["XB"], w=["pG"])
                K.op("dve", lambda: nc.vector.tensor_tensor(out=GmT[b3][:], in0=pG[:], in1=bcast(self.mls_f, [128, 2, 128], 1), op=ALU.mult),
                     r=["pG", "cf"], w=[("GmT", b3)])
                for hh in range(2):
                    e = (c * 2 + hh) % 6
                    for h4 in range(4):
                        h = hh * 4 + h4
                        i = lic[0] % 4
                        lic[0] += 1
                        veng = "dve"
                        vh = nc.vector
                        K.op(veng, lambda: vh.tensor_scalar(out=lh[i][:], in0=self.SL_f, scalar1=A[:, c, h:h + 1], scalar2=None, op0=ALU.mult),
                             r=["A", "cf"], w=[("lh", i)])
                        K.op("pe", lambda: nc.tensor.matmul(pSeg[hh][:, h4, :], lhsT=lh[i][:], rhs=self.U_f, start=True, stop=True),
                             r=[("lh", i), "cf"], w=[("pSeg", hh)])
                    K.op("act", lambda: nc.scalar.activation(out=eL[e][:], in_=pSeg[hh][:], func=AF.Exp), r=[("pSeg", hh)], w=[("eL", e)])
                    K.op("pool", lambda: nc.gpsimd.tensor_tensor(out=scT[e][:], in0=eL[e][:], in1=bcast(GmT[b3][:, hh, :], [128, 4, 128], 1), op=ALU.mult),
                         r=[("eL", e), ("GmT", b3)], w=[("scT", e)])

            def stage_b(c):
                b = c % 2
                b3 = c % 3
                tok = slice(c * 128, (c + 1) * 128)
                for h in range(8):
                    e = (c * 2 + h // 4) % 6
                    K.op("pe", lambda: nc.tensor.matmul(pYd[:, h, :], lhsT=scT[e][:, h % 4, :], rhs=X[b3][:, h, :], start=True, stop=True),
                         r=[("scT", e), ("X", b3)], w=["pYd"])
                if c > 0:
                    for h in range(8):
                        K.op("pe", lambda: nc.tensor.matmul(pYo[:, h, :], lhsT=XB[:, 6 + h // 4, tok], rhs=hbf[b][:, h, :], start=True, stop=True),
                             r=["XB", ("hbf", b)], w=["pYo"])
                    K.op("dve", lambda: nc.vector.tensor_tensor(out=yo[b][:], in0=pYo[:], in1=bcast(ecum[:, c, :], [128, 8, 64], 2), op=ALU.mult),
                         r=["pYo", "ecum"], w=[("yo", b)])
                    K.op("dve", lambda: nc.vector.tensor_tensor(out=yy[b][:], in0=pYd[:], in1=yo[b][:], op=ALU.add),
                         r=["pYd", ("yo", b)], w=[("yy", b)])
                else:
                    K.op("dve", lambda: nc.vector.tensor_copy(out=yy[b][:], in_=pYd[:]), r=["pYd"], w=[("yy", b)])
                K.op("pool", lambda: nc.gpsimd.tensor_tensor(out=td[b][:], in0=xh[b3][:], in1=bcast(d_bc, [128, 8, 64], 2), op=ALU.mult),
                     r=[("xh", b3), "prm"], w=[("td", b)])
                K.op("pool", lambda: nc.gpsimd.tensor_tensor(out=yy[b][:], in0=yy[b][:], in1=td[b][:], op=ALU.add),
                     r=[("yy", b), ("td", b)], w=[("yy", b)])
                yf = yy[b][:].rearrange("p h c -> p (h c)")
                K.op("dve", lambda: nc.vector.tensor_tensor(out=yf, in0=yf, in1=zt[b3][:], op=ALU.mult),
                     r=[("yy", b), ("zt", b3)], w=[("yy", b)])
                for g in range(2):
                    K.op("act", lambda: nc.scalar.activation(out=junk[:], in_=yf[:, g * 256:(g + 1) * 256], func=AF.Square, accum_out=ss[b][:, g:g + 1]),
                         r=[("yy", b)], w=[("ss", b, g), "junk"])
                K.op("dve", lambda: nc.vector.tensor_scalar(out=ss[b][:], in0=ss[b][:], scalar1=1.0 / 256.0, scalar2=RMS_EPS, op0=ALU.mult, op1=ALU.add),
                     r=[("ss", b, 0), ("ss", b, 1)], w=[("ss", b, 0), ("ss", b, 1)])
                K.op("act", lambda: nc.scalar.activation(out=ss[b][:], in_=ss[b][:], func=AF.Ln),
                     r=[("ss", b, 0), ("ss", b, 1)], w=[("ss", b, 0), ("ss", b, 1)])
                K.op("act", lambda: nc.scalar.activation(out=ss[b][:], in_=ss[b][:], func=AF.Exp, scale=-0.5),
                     r=[("ss", b, 0), ("ss", b, 1)], w=[("ss", b, 0), ("ss", b, 1)])
                for g in range(2):
                    K.op("dve", lambda: nc.vector.scalar_tensor_tensor(out=yn[b][:, g * 256:(g + 1) * 256], in0=yf[:, g * 256:(g + 1) * 256],
                                                                        scalar=ss[b][:, g:g + 1], in1=ng_bc[:, g * 256:(g + 1) * 256],
                                                                        op0=ALU.mult, op1=ALU.mult),
                         r=[("yy", b), ("ss", b, 0), ("ss", b, 1), "prm"], w=[("yn", b)])

            def stage_t(c):
                b = c % 2
                b3 = c % 3
                for j in range(4):
                    K.op("pe", lambda: nc.tensor.transpose(tpy[:, j, :], yn[b][:, j * 128:(j + 1) * 128], self.ident), r=[("yn", b), "cbf"], w=["tpy"])
                g4 = (c // 4) % 2
                K.op("act", lambda: nc.scalar.copy(out=sdT[g4][:, :, (c % 4) * 128:(c % 4 + 1) * 128], in_=tpy[:]), r=["tpy"], w=[("sdT", g4)])
                if c % 4 == 3:
                    K.dma("act", self.mixT[4:8, :, (c // 4) * 512:(c // 4 + 1) * 512].rearrange("c p t -> p c t"), sdT[g4][:],
                          r=[("sdT", g4)], w=[("mixT", 4, c // 4)])

            def stage_s(c):
                b = c % 2
                b3 = c % 3
                if c < NT - 1:
                    for h in range(8):
                        K.op("pe", lambda: nc.tensor.matmul(pSt[:, h, :], lhsT=Bt[b3][:, h // 4, :], rhs=Xd[b3][:, h, :], start=True, stop=True),
                             r=[("Bt", b3), ("Xd", b3)], w=["pSt"])
                    if c == 0:
                        K.op("dve", lambda: nc.vector.tensor_copy(out=hf[:], in_=pSt[:]), r=["pSt"], w=["hf"])
                    else:
                        K.op("pool", lambda: nc.gpsimd.tensor_tensor(out=hf[:], in0=hf[:], in1=bcast(cdec[:, c, :], [128, 8, 64], 2), op=ALU.mult),
                             r=["hf", "cdec"], w=["hf"])
                        K.op("dve", lambda: nc.vector.tensor_tensor(out=hf[:], in0=hf[:], in1=pSt[:], op=ALU.add), r=["hf", "pSt"], w=["hf"])
                    K.op("act", lambda: nc.scalar.copy(out=hbf[1 - b][:], in_=hf[:]), r=["hf"], w=[("hbf", 1 - b)])

            stage_a(0)
            stage_a(1)
            for c in range(NT):
                stage_s(c)
                stage_b(c)
                if c + 2 < NT:
                    stage_a(c + 2)
                if c >= 1:
                    stage_t(c - 1)
            stage_t(NT - 1)
            K.barrier()
        self.ssd_es.close()

    def layernorm(self, es, name, beng="pool", nbuf=3):
        nc, K = self.nc, self.K
        st = [self.sb(es, name + "st", [128, 2, 6], F32) for _ in range(nbuf)]
        mv = [self.sb(es, name + "mv", [128, 4], F32) for _ in range(nbuf)]
        bh = nc.gpsimd if beng == "pool" else nc.vector

        def stats(i, h, hk):
            mk = (name + "mv", i)
            for c in range(2):
                K.op("dve", lambda: nc.vector.bn_stats(out=st[i][:, c, :], in_=h[:, c * 512:(c + 1) * 512]), r=[hk], w=[(name + "st", i, c)])
            K.op("dve", lambda: nc.vector.bn_aggr(out=mv[i][:, 0:2], in_=st[i][:].rearrange("p a b -> p (a b)")),
                 r=[(name + "st", i, 0), (name + "st", i, 1)], w=[mk])
            K.op("dve", lambda: nc.vector.tensor_scalar(out=mv[i][:, 1:2], in0=mv[i][:, 1:2], scalar1=LN_EPS, scalar2=None, op0=ALU.add), r=[mk], w=[mk])
            K.op("act", lambda: nc.scalar.activation(out=mv[i][:, 1:2], in_=mv[i][:, 1:2], func=AF.Ln), r=[mk], w=[mk])
            K.op("act", lambda: nc.scalar.activation(out=mv[i][:, 2:3], in_=mv[i][:, 1:2], func=AF.Exp, scale=-0.5), r=[mk], w=[mk])
            K.op("dve", lambda: nc.vector.tensor_scalar(out=mv[i][:, 3:4], in0=mv[i][:, 0:1], scalar1=mv[i][:, 2:3], scalar2=-1.0, op0=ALU.mult, op1=ALU.mult),
                 r=[mk], w=[mk])

        def apply(i, h, hk, out, ok, g_bc, b_bc, pk):
            mk = (name + "mv", i)
            K.op("act", lambda: nc.scalar.activation(out=h, in_=h, func=AF.Identity, bias=mv[i][:, 3:4], scale=mv[i][:, 2:3]), r=[hk, mk], w=[hk])
            K.op("dve", lambda: nc.vector.tensor_tensor(out=h, in0=h, in1=g_bc, op=ALU.mult), r=[hk, pk], w=[hk])
            K.op(beng, lambda: bh.tensor_tensor(out=out, in0=h, in1=b_bc, op=ALU.add), r=[hk, pk], w=[ok])
        return stats, apply

    def phase_outln(self, l, src, dst):
        nc, K = self.nc, self.K
        self.exp_es = contextlib.ExitStack()
        self.exp_pre = None
        if self.with_moe:
            ees = self.exp_es
            self.exp_pre = (self.sb(ees, "wgu0", [128, 8, 2 * D], BF16), self.sb(ees, "wdn0", [128, 8, D], BF16), self.sb(ees, "bdn0", [1, D], BF16))
        with contextlib.ExitStack() as es:
            wo = self.sb(es, "wo", [128, 12, D], BF16)
            for c in range(3):
                K.dma("pool", wo[:, c * 4:(c + 1) * 4, :], self.w_out[l, c * 512:(c + 1) * 512, :].rearrange("(c p) d -> p c d", p=128), w=["wo"])
            wr = self.sb(es, "wr", [128, 8, NE], BF16)
            K.dma("pool", wr[:], self.router_w[l].rearrange("(k p) e -> p k e", p=128), w=["wr"])
            if self.exp_pre is not None:
                w0, d0, b0 = self.exp_pre
                for c in range(2):
                    K.dma("pool", w0[:, :, c * D:(c + 1) * D], self.w_gu[l, 0, :, c * D:(c + 1) * D].rearrange("(k p) f -> p k f", p=128), w=[("wgu", 0)])
                K.dma("pool", d0[:], self.w_dn[l, 0].rearrange("(k p) f -> p k f", p=128), w=[("wdn", 0)])
                K.dma("pool", b0[:], self.b_dn[l, 0:1, :], w=[("bdn", 0)])
            lnp = self.sb(es, "lnp", [128, 2 * D + NE], F32)
            K.dma("sp", lnp[:, 0:D], self.bc[l, BC_L1G:BC_L1G + D].partition_broadcast(128), w=["lnp"])
            K.dma("sp", lnp[:, D:2 * D], self.bc[l, BC_L1B:BC_L1B + D].partition_broadcast(128), w=["lnp"])
            K.dma("sp", lnp[:, 2 * D:], self.bc[l, BC_RB:BC_RB + NE].partition_broadcast(128), w=["lnp"])
            g_bc, b_bc, rb_bc = lnp[:, 0:D], lnp[:, D:2 * D], lnp[:, 2 * D:]
            ecap = self.sb(es, "ecap", [128, NE], F32)
            K.op("dve", lambda: nc.vector.tensor_scalar(out=ecap[:], in0=self.iota_e, scalar1=float(CAP), scalar2=None, op0=ALU.mult), r=["cf"], w=["ecap"])
            cntb = self.sb(es, "cntb", [128, NE], F32)
            K.op("dve", lambda: nc.vector.memset(cntb[:], 0.0), w=["cntb"])
            ln_stats, ln_apply = self.layernorm(es, "ln1", beng="dve")
            NB = 3
            TB = 4
            NXB = 4 * TB
            mt = [self.sb(es, "mt", [128, 12, 512], BF16) for _ in range(2)]
            xr = [self.sb(es, "xr", [128, D], F32) for _ in range(NB)]
            hb = [self.sb(es, "hb", [128, D], F32) for _ in range(NB)]
            x1t = [self.sb(es, "x1t", [128, D], F32) for _ in range(NB)]
            x1b = [self.sb(es, "x1b", [128, D], BF16) for _ in range(NXB)]
            x1T = [self.sb(es, "x1T", [128, 8, 128], BF16) for _ in range(2)]
            pm = [self.ps(es, "pm", [128, 512], F32) for _ in range(4)]
            tpr = [self.ps(es, "tpr", [128, 8, 128], BF16) for _ in range(2)]
            plg = self.ps(es, "plg", [128, TB, NE], F32)
            ppos = self.ps(es, "ppos", [128, 2, TB, NE], F32)
            lg = self.sb(es, "lg", [128, TB, NE], F32)
            mx8 = self.sb(es, "mx8", [128, TB, 8], F32)
            ix8 = self.sb(es, "ix8", [128, TB, 8], U32)
            e4 = self.sb(es, "e4", [128, TB, 4], F32)
            rs = self.sb(es, "rs", [128, TB], F32)
            g4 = [self.sb(es, "g4", [128, TB, 4], F32) for _ in range(2)]
            idxf = self.sb(es, "idxf", [128, TB, 4], F32)
            oh = self.sb(es, "oh", [128, TB, 4, NE], F32)
            mskb = self.sb(es, "mskb", [128, TB, NE], BF16)
            cs = self.sb(es, "cs", [128, NE, TB], F32)
            inc = self.sb(es, "inc", [128, NE, TB], F32)
            slot = self.sb(es, "slot", [128, TB, NE], F32)
            tmp = self.sb(es, "tmp", [128, TB, 4, NE], F32)
            off = self.sb(es, "off", [128, TB, 4], F32)
            offi = [self.sb(es, "offi", [128, TB, 4], I32) for _ in range(2)]

            def stage_a(t):
                b = t % NB
                bx = t % NXB
                tok = slice(t * 128, (t + 1) * 128)
                g4i = (t // 4) % 2
                if t == 0:
                    K.dma("sp", mt[0][:], self.mixT[:, :, 0:512].rearrange("c p t -> p c t"), w=[("mt", 0)])
                    K.dma("sp", xr[0][:], src[0:128, :], w=[("xr", 0)])
                if t % 4 == 0 and t + 4 < NT:
                    gn = t // 4 + 1
                    K.dma("sp", mt[gn % 2][:], self.mixT[:, :, gn * 512:(gn + 1) * 512].rearrange("c p t -> p c t"), w=[("mt", gn % 2)])
                if t + 1 < NT:
                    K.dma("sp", xr[(t + 1) % NB][:], src[(t + 1) * 128:(t + 2) * 128, :], w=[("xr", (t + 1) % NB)])
                for half in range(2):
                    pi = (t % 2) * 2 + half
                    for mc in range(12):
                        K.op("pe", lambda: nc.tensor.matmul(pm[pi][:], lhsT=mt[g4i][:, mc, (t % 4) * 128:(t % 4 + 1) * 128],
                                                            rhs=wo[:, mc, half * 512:(half + 1) * 512], start=(mc == 0), stop=(mc == 11)),
                             r=[("mt", g4i), "wo"], w=[("pm", pi)])
                    K.op("dve", lambda: nc.vector.scalar_tensor_tensor(out=hb[b][:, half * 512:(half + 1) * 512], in0=xr[b][:, half * 512:(half + 1) * 512],
                                                                        scalar=ALPHA, in1=pm[pi][:], op0=ALU.mult, op1=ALU.add),
                         r=[("xr", b), ("pm", pi)], w=[("hb", b)])
                ln_stats(b, hb[b][:], ("hb", b))

            def stage_a2(t):
                b = t % NB
                bx = t % NXB
                tok = slice(t * 128, (t + 1) * 128)
                ln_apply(b, hb[b][:], ("hb", b), x1t[b][:], ("x1t", b), g_bc, b_bc, "lnp")
                K.dma("sp", self.x1[tok, :], x1t[b][:], r=[("x1t", b)], w=[("x1", t)])
                K.op("act", lambda: nc.scalar.copy(out=x1b[bx][:], in_=x1t[b][:]), r=[("x1t", b)], w=[("x1b", bx)])

            def stage_r(t):
                b = t % 2
                bx = t % NXB
                for kc in range(8):
                    K.op("pe", lambda: nc.tensor.transpose(tpr[b][:, kc, :], x1b[bx][:, kc * 128:(kc + 1) * 128], self.ident), r=[("x1b", bx), "cbf"], w=[("tpr", b)])
                K.op("act", lambda: nc.scalar.copy(out=x1T[b][:], in_=tpr[b][:]), r=[("tpr", b)], w=[("x1T", b)])
                for kc in range(8):
                    K.op("pe", lambda: nc.tensor.matmul(plg[:, t % TB, :], lhsT=x1T[b][:, kc, :], rhs=wr[:, kc, :], start=(kc == 0), stop=(kc == 7)),
                         r=[("x1T", b), "wr"], w=["plg"])

            def stage_b(tb):
                gb = tb % 2
                bc4 = lambda ap: ap.unsqueeze(3).broadcast_to([128, TB, 4, NE])
                K.op("dve", lambda: nc.vector.tensor_tensor(out=lg[:], in0=plg[:], in1=rb_bc.unsqueeze(1).broadcast_to([128, TB, NE]), op=ALU.add),
                     r=["plg", "lnp"], w=["lg"])
                for i in range(TB):
                    K.op("dve", lambda: nc.vector.max(out=mx8[:, i, :], in_=lg[:, i, :]), r=["lg"], w=[("mx8", i)])
                yield
                for i in range(TB):
                    K.op("dve", lambda: nc.vector.max_index(out=ix8[:, i, :], in_max=mx8[:, i, :], in_values=lg[:, i, :]), r=["lg", ("mx8", i)], w=[("ix8", i)])
                yield
                allmx = [("mx8", i) for i in range(TB)]
                allix = [("ix8", i) for i in range(TB)]
                K.op("dve", lambda: nc.vector.tensor_tensor(out=e4[:], in0=mx8[:, :, 0:4], in1=mx8[:, :, 0:1].broadcast_to([128, TB, 4]), op=ALU.subtract),
                     r=allmx, w=["e4"])
                K.op("act", lambda: nc.scalar.activation(out=e4[:], in_=e4[:], func=AF.Exp), r=["e4"], w=["e4"])
                K.op("dve", lambda: nc.vector.tensor_reduce(out=rs[:], in_=e4[:], axis=AX.X, op=ALU.add), r=["e4"], w=["rs"])
                K.op("dve", lambda: nc.vector.reciprocal(out=rs[:], in_=rs[:]), r=["rs"], w=["rs"])
                K.op("dve", lambda: nc.vector.tensor_tensor(out=g4[gb][:], in0=e4[:], in1=rs[:].unsqueeze(2).broadcast_to([128, TB, 4]), op=ALU.mult),
                     r=["e4", "rs"], w=[("g4", gb)])
                K.dma("act", self.gate_s[tb * TB * 128:(tb + 1) * TB * 128, :].rearrange("(t p) k -> p t k", p=128), g4[gb][:], r=[("g4", gb)], w=[("gate_s", tb)])
                yield
                K.op("dve", lambda: nc.vector.tensor_copy(out=idxf[:], in_=ix8[:, :, 0:4]), r=allix, w=["idxf"])
                K.op("dve", lambda: nc.vector.tensor_tensor(out=oh[:], in0=self.iota_e.unsqueeze(1).unsqueeze(1).broadcast_to([128, TB, 4, NE]),
                                                            in1=bc4(idxf[:]), op=ALU.is_equal), r=["idxf", "cf"], w=["oh"])
                with nc.allow_low_precision(reason="0/1 one-hot sums are exact in bf16"):
                    K.op("dve", lambda: nc.vector.tensor_reduce(out=mskb[:], in_=oh[:].rearrange("p t k e -> p t e k"), axis=AX.X, op=ALU.add), r=["oh"], w=["mskb"])
                yield
                mflat = mskb[:].rearrange("p t e -> p (t e)")
                K.op("pe", lambda: nc.tensor.matmul(ppos[:, 0, :, :].rearrange("p t e -> p (t e)"), lhsT=self.sl_bf, rhs=mflat, start=True, stop=True),
                     r=["mskb", "cbf"], w=["ppos"])
                K.op("pe", lambda: nc.tensor.matmul(ppos[:, 1, :, :].rearrange("p t e -> p (t e)"), lhsT=self.ones_bf, rhs=mflat, start=True, stop=True),
                     r=["mskb", "cbf"], w=["ppos"])
                K.op("dve", lambda: nc.vector.tensor_copy(out=cs[:], in_=ppos[:, 1, :, :].rearrange("p t e -> p e t")), r=["ppos"], w=["cs"])
                K.op("dve", lambda: nc.vector.tensor_tensor_scan(out=inc[:].rearrange("p e t -> p (e t)"), data0=self.zr_f,
                                                                  data1=cs[:].rearrange("p e t -> p (e t)"), initial=0.0, op0=ALU.mult, op1=ALU.add),
                     r=["cs", "cf"], w=["inc"])
                yield
                K.op("dve", lambda: nc.vector.tensor_tensor(out=slot[:], in0=ppos[:, 0, :, :], in1=inc[:].rearrange("p e t -> p t e"), op=ALU.add),
                     r=["ppos", "inc"], w=["slot"])
                K.op("dve", lambda: nc.vector.tensor_tensor(out=slot[:], in0=slot[:], in1=cs[:].rearrange("p e t -> p t e"), op=ALU.subtract),
                     r=["slot", "cs"], w=["slot"])
                K.op("dve", lambda: nc.vector.tensor_tensor(out=slot[:], in0=slot[:], in1=cntb[:].unsqueeze(1).broadcast_to([128, TB, NE]), op=ALU.add),
                     r=["slot", "cntb"], w=["slot"])
                K.op("dve", lambda: nc.vector.tensor_scalar(out=slot[:], in0=slot[:], scalar1=float(CAP - 1), scalar2=None, op0=ALU.min),
                     r=["slot"], w=["slot"])
                K.op("dve", lambda: nc.vector.tensor_tensor(out=slot[:], in0=slot[:], in1=ecap[:].unsqueeze(1).broadcast_to([128, TB, NE]), op=ALU.add),
                     r=["slot", "ecap"], w=["slot"])
                K.op("dve", lambda: nc.vector.tensor_tensor(out=cntb[:], in0=cntb[:], in1=inc[:, :, TB - 1], op=ALU.add), r=["cntb", "inc", "slot"], w=["cntb"])
                yield
                K.op("dve", lambda: nc.vector.tensor_tensor(out=tmp[:], in0=oh[:], in1=slot[:].unsqueeze(2).broadcast_to([128, TB, 4, NE]), op=ALU.mult),
                     r=["oh", "slot"], w=["tmp"])
                K.op("dve", lambda: nc.vector.tensor_reduce(out=off[:], in_=tmp[:], axis=AX.X, op=ALU.add), r=["tmp"], w=["off"])
                K.op("dve", lambda: nc.vector.tensor_copy(out=offi[gb][:], in_=off[:]), r=["off"], w=[("offi", gb)])
                K.dma("act", self.offs[tb * TB * 128:(tb + 1) * TB * 128, :].rearrange("(t p) k -> p t k", p=128), offi[gb][:], r=[("offi", gb)], w=[("offs", tb)])
                yield
                for i in range(TB):
                    bx = (tb * TB + i) % NXB
                    for k in range(4):
                        K.dma("pool", self.xg[:, :], x1b[bx][:], r=[("x1b", bx), ("offi", gb)], w=["xg"],
                              indirect=dict(out_offset=bass.IndirectOffsetOnAxis(offi[gb][:, i, k:k + 1], 0), in_offset=None))

            pending = []
            for t in range(NT + 2):
                if 2 <= t <= NT + 1:
                    stage_r(t - 2)
                    if (t - 1) % TB == 0:
                        for _ in stage_b((t - 1) // TB - 1):
                            pass
                if 1 <= t <= NT:
                    stage_a2(t - 1)
                for g in list(pending):
                    try:
                        next(g)
                    except StopIteration:
                        pending.remove(g)
                if t < NT:
                    stage_a(t)
            for g in pending:
                for _ in g:
                    pass
            K.barrier()

    def phase_route(self, l, src, dst):
        pass

    def phase_experts(self, l, src, dst):
        nc, K = self.nc, self.K
        NCG, CGW = 2, CAP // 2
        NST = CAP // 128
        with contextlib.ExitStack() as es:
            bgu = self.sb(es, "bgu", [128, NE * 16], F32)
            K.dma("sp", bgu[:], self.pp[l, :, PP_BGU:PP_BGU + NE * 16], w=["bgu"])
            wgu = [self.exp_pre[0], self.sb(es, "wgu", [128, 8, 2 * D], BF16)]
            wdn = [self.exp_pre[1], self.sb(es, "wdn", [128, 8, D], BF16)]
            bdn = [self.exp_pre[2], self.sb(es, "bdn", [1, D], BF16)]
            xgt = [self.sb(es, "xgt", [128, XGW], BF16) for _ in range(CAP // 128)]
            xgT = [self.sb(es, "xgT", [128, 8, CAP], BF16) for _ in range(2)]
            hT = [self.sb(es, "hT", [128, 8, CAP], BF16) for _ in range(2)]
            g1 = [self.sb(es, "g1", [128, CGW], F32) for _ in range(2)]
            sg = [self.sb(es, "sg", [128, CGW], F32) for _ in range(2)]
            u1 = [self.sb(es, "u1", [128, CGW], F32) for _ in range(2)]
            yt = [self.sb(es, "yt", [128, D], F32) for _ in range(2)]
            tpx = [self.ps(es, "tpx", [128, 8, 128], BF16) for _ in range(2)]
            pg = [self.ps(es, "pg", [128, 512], F32) for _ in range(2)]
            pu = [self.ps(es, "pu", [128, 512], F32) for _ in range(2)]
            py = [self.ps(es, "py", [128, 512], F32) for _ in range(2)]
            cnt = {"xi": 0, "ei": 0, "yi": 0, "ti": 0}

            def prep_loads(e):
                wb = e % 2
                if e > 0:
                    for c in range(2):
                        K.dma("pool", wgu[wb][:, :, c * D:(c + 1) * D], self.w_gu[l, e, :, c * D:(c + 1) * D].rearrange("(k p) f -> p k f", p=128), w=[("wgu", wb)])
                    K.dma("pool", wdn[wb][:], self.w_dn[l, e].rearrange("(k p) f -> p k f", p=128), w=[("wdn", wb)])
                    K.dma("pool", bdn[wb][:], self.b_dn[l, e:e + 1, :], w=[("bdn", wb)])
                for i in range(NST):
                    r0 = e * CAP + i * 128
                    K.dma("sp", xgt[i][:], self.xg[r0:r0 + 128, :], w=[("xgt", i)])

            def prep_T(e):
                wb = e % 2
                for i in range(NST):
                    x_ = xgt[i]
                    xk = ("xgt", i)
                    ti = cnt["ti"] % 2
                    cnt["ti"] += 1
                    for kc in range(8):
                        K.op("pe", lambda: nc.tensor.transpose(tpx[ti][:, kc, :], x_[:, kc * 128:(kc + 1) * 128], self.ident), r=[xk, "cbf"], w=[("tpx", ti)])
                    K.op("act", lambda: nc.scalar.copy(out=xgT[wb][:, :, i * 128:(i + 1) * 128], in_=tpx[ti][:]), r=[("tpx", ti)], w=[("xgT", wb)])

            def gateup(e):
                wb = e % 2
                for cg in range(NCG):
                    cs = slice(cg * CGW, (cg + 1) * CGW)
                    for j in range(8):
                        p = cnt["ei"] % 2
                        cnt["ei"] += 1
                        for kc in range(8):
                            K.op("pe", lambda: nc.tensor.matmul(pg[p][:, 0:CGW], lhsT=wgu[wb][:, kc, j * 128:(j + 1) * 128], rhs=xgT[wb][:, kc, cs],
                                                                start=(kc == 0), stop=(kc == 7)), r=[("wgu", wb), ("xgT", wb)], w=[("pg", p)])
                        for kc in range(8):
                            K.op("pe", lambda: nc.tensor.matmul(pu[p][:, 0:CGW], lhsT=wgu[wb][:, kc, D + j * 128:D + (j + 1) * 128], rhs=xgT[wb][:, kc, cs],
                                                                start=(kc == 0), stop=(kc == 7)), r=[("wgu", wb), ("xgT", wb)], w=[("pu", p)])
                        bg = bgu[:, e * 16 + j:e * 16 + j + 1]
                        bu = bgu[:, e * 16 + 8 + j:e * 16 + 8 + j + 1]
                        K.op("dve", lambda: nc.vector.tensor_scalar(out=g1[p][:], in0=pg[p][:, 0:CGW], scalar1=bg, scalar2=7.0, op0=ALU.add, op1=ALU.min),
                             r=[("pg", p), "bgu"], w=[("g1", p)])
                        K.op("act", lambda: nc.scalar.activation(out=sg[p][:], in_=g1[p][:], func=AF.Sigmoid, scale=1.702), r=[("g1", p)], w=[("sg", p)])
                        K.op("dve", lambda: nc.vector.tensor_scalar(out=u1[p][:], in0=pu[p][:, 0:CGW], scalar1=bu, scalar2=7.0, op0=ALU.add, op1=ALU.min),
                             r=[("pu", p), "bgu"], w=[("u1", p)])
                        K.op("dve", lambda: nc.vector.tensor_scalar(out=u1[p][:], in0=u1[p][:], scalar1=-7.0, scalar2=1.0, op0=ALU.max, op1=ALU.add),
                             r=[("u1", p)], w=[("u1", p)])
                        K.op("dve", lambda: nc.vector.tensor_tensor(out=g1[p][:], in0=g1[p][:], in1=sg[p][:], op=ALU.mult), r=[("g1", p), ("sg", p)], w=[("g1", p)])
                        K.op("dve", lambda: nc.vector.tensor_tensor(out=hT[wb][:, j, cs], in0=g1[p][:], in1=u1[p][:], op=ALU.mult),
                             r=[("g1", p), ("u1", p)], w=[("hT", wb)])

            def down(e):
                wb = e % 2
                for i in range(NST):
                    yi = cnt["yi"]
                    cnt["yi"] += 1
                    yb_ = yt[yi % 2]
                    yk = ("yt", yi % 2)
                    for half in range(2):
                        hs = slice(half * 512, (half + 1) * 512)
                        K.op("pe", lambda: nc.tensor.matmul(py[half][:], lhsT=self.ones_bf[0:1, :], rhs=bdn[wb][0:1, hs], start=True, stop=False),
                             r=[("bdn", wb), "cbf"], w=[("py", half)])
                        for fc in range(8):
                            K.op("pe", lambda: nc.tensor.matmul(py[half][:], lhsT=hT[wb][:, fc, i * 128:(i + 1) * 128], rhs=wdn[wb][:, fc, hs],
                                                                start=False, stop=(fc == 7)), r=[("hT", wb), ("wdn", wb)], w=[("py", half)])
                        if half == 0:
                            K.op("act", lambda: nc.scalar.copy(out=yb_[:, hs], in_=py[half][:]), r=[("py", half)], w=[yk])
                        else:
                            K.op("dve", lambda: nc.vector.tensor_copy(out=yb_[:, hs], in_=py[half][:]), r=[("py", half)], w=[yk])
                    r0 = e * CAP + i * 128
                    K.dma("sp", self.yb[r0:r0 + 128, :], yb_[:], r=[yk], w=[("yb", e, i)])

            prep_loads(0)
            prep_T(0)
            for e in range(NE):
                if e + 1 < NE:
                    prep_loads(e + 1)
                gateup(e)
                if e + 1 < NE:
                    prep_T(e + 1)
                down(e)
            K.barrier()
        self.exp_es.close()

    def phase_combine(self, l, src, dst):
        nc, K = self.nc, self.K
        with contextlib.ExitStack() as es:
            lnp = self.sb(es, "lnp2", [128, 2 * D], F32)
            K.dma("sp", lnp[:, 0:D], self.bc[l, BC_L2G:BC_L2G + D].partition_broadcast(128), w=["lnp2"])
            K.dma("sp", lnp[:, D:2 * D], self.bc[l, BC_L2B:BC_L2B + D].partition_broadcast(128), w=["lnp2"])
            ln_stats, ln_apply = self.layernorm(es, "ln2", beng="dve")
            NB = 3
            offi = [self.sb(es, "offi2", [128, 4], I32) for _ in range(NB)]
            gt = [self.sb(es, "gt", [128, 4], F32) for _ in range(NB)]
            yg = [self.sb(es, "yg", [128, 4, D], F32) for _ in range(NB)]
            x1t = [self.sb(es, "x1c", [128, D], F32) for _ in range(NB)]
            hb = [self.sb(es, "hb2", [128, D], F32) for _ in range(3)]
            ot = [self.sb(es, "ot", [128, D], F32) for _ in range(2)]
            def loads(t):
                b = t % NB
                tok = slice(t * 128, (t + 1) * 128)
                K.dma("sp", offi[b][:], self.offs[tok, :], w=[("offi2", b)])
                K.dma("sp", gt[b][:], self.gate_s[tok, :], w=[("gt", b)])
                K.dma("sp", x1t[b][:], self.x1[tok, :], w=[("x1c", b)])

            def gathers(t):
                b = t % NB
                for k in range(4):
                    K.dma("pool", yg[b][:, k, :], self.yb[:, :], r=[("offi2", b)], w=[("yg", b, k)],
                          indirect=dict(out_offset=None, in_offset=bass.IndirectOffsetOnAxis(offi[b][:, k:k + 1], 0)))

            def c1(t):
                b = t % NB
                K.op("act", lambda: nc.scalar.activation(out=hb[b][:], in_=yg[b][:, 0, :], func=AF.Copy, scale=gt[b][:, 0:1]),
                     r=[("yg", b, 0), ("gt", b)], w=[("hb2", b)])
                for k in range(1, 4):
                    K.op("dve", lambda: nc.vector.scalar_tensor_tensor(out=hb[b][:], in0=yg[b][:, k, :], scalar=gt[b][:, k:k + 1], in1=hb[b][:],
                                                                        op0=ALU.mult, op1=ALU.add),
                         r=[("yg", b, k), ("gt", b), ("hb2", b)], w=[("hb2", b)])
                K.op("dve", lambda: nc.vector.scalar_tensor_tensor(out=hb[b][:], in0=x1t[b][:], scalar=ALPHA, in1=hb[b][:], op0=ALU.mult, op1=ALU.add),
                     r=[("x1c", b), ("hb2", b)], w=[("hb2", b)])
                ln_stats(b, hb[b][:], ("hb2", b))

            def c2(t):
                b = t % NB
                b2 = t % 2
                tok = slice(t * 128, (t + 1) * 128)
                ln_apply(b, hb[b][:], ("hb2", b), ot[b2][:], ("ot", b2), lnp[:, 0:D], lnp[:, D:2 * D], "lnp2")
                K.dma("act", dst[tok, :], ot[b2][:], r=[("ot", b2)], w=[("dst", t)])

            loads(0)
            loads(1)
            gathers(0)
            for t in range(NT + 1):
                if t + 2 < NT:
                    loads(t + 2)
                if t + 1 < NT:
                    gathers(t + 1)
                if t < NT:
                    c1(t)
                if t >= 1:
                    c2(t - 1)
            K.barrier()


def host_consts():
    bf = ml_dtypes.bfloat16
    k = np.arange(128)[:, None]
    q = np.arange(128)[None, :]
    cb = np.zeros((128, 1024), np.float32)
    cb[:, 0:128] = np.eye(128)
    cb[:, 128:256] = (k >= q)
    cb[:, 256:384] = (k <= q)
    cb[:, 384:512] = 1.0
    cb[:, 512:640] = (k < q)
    cb[:, 640:768] = (k <= q)
    cb[:, 768:896] = (k >= q)
    cf = np.zeros((128, 1024), np.float32)
    cf[:, 0:128] = (k <= q)
    cf[:, 128:256] = (k > q)
    cf[:, 256:384] = 1.0
    cf[:, 384:512] = (q >= k)
    cf[:, 512:544] = np.arange(32)[None, :]
    zr = np.ones((32, 8), np.float32)
    zr[:, 0] = 0.0
    cf[:, 544:800] = zr.reshape(1, 256)
    zr4 = np.ones((32, 4), np.float32)
    zr4[:, 0] = 0.0
    cf[:, 800:928] = zr4.reshape(1, 128)
    half = 32
    inv = (10000.0 ** (-np.arange(half, dtype=np.float32) / half)).astype(np.float32)
    ang = np.arange(S, dtype=np.float32)[None, :] * inv[:, None]
    cos = np.cos(ang).astype(np.float32)
    sin = np.sin(ang).astype(np.float32)
    cos_t = np.concatenate([cos, cos, cos, cos], 0)
    sin_t = np.concatenate([-sin, sin, -sin, sin], 0)
    return cb.astype(bf), cf, np.ascontiguousarray(cos_t), np.ascontiguousarray(sin_t)


def host_layout(inp):
    f = lambda a: np.ascontiguousarray(np.asarray(a, dtype=np.float32))
    w_in = f(inp["w_in"])
    qk = w_in[:, :, 0:1024].reshape(L, D, 16, 2, 32)
    w_qkp = np.ascontiguousarray(qk[:, :, :, ::-1, :].reshape(L, D, 1024))
    pp = np.zeros((L, 128, PPW), np.float32)
    cw = f(inp["ssd_conv_w"])
    pp[:, :, PP_CW:PP_CW + 32] = cw.reshape(L, 4, 8, 128).transpose(0, 3, 2, 1).reshape(L, 128, 32)
    pp[:, :, PP_CB:PP_CB + 8] = f(inp["ssd_conv_b"]).reshape(L, 8, 128).transpose(0, 2, 1)
    scw = f(inp["sc_conv_w"])
    pp[:, :, PP_SCW:PP_SCW + 12] = scw.reshape(L, 3, 4, 128).transpose(0, 3, 2, 1).reshape(L, 128, 12)
    bgu = f(inp["exp_b_gu"])
    pp[:, :, PP_BGU:] = bgu.reshape(L, NE, 16, 128).transpose(0, 3, 1, 2).reshape(L, 128, NE * 16)
    bc = np.concatenate([f(inp["ssd_dt_bias"]), f(inp["ssd_a_log"]), f(inp["ssd_d"]), f(inp["ssd_norm_g"]),
                         f(inp["router_b"]), f(inp["ln1_g"]), f(inp["ln1_b"]), f(inp["ln2_g"]), f(inp["ln2_b"])], axis=1)
    assert bc.shape == (L, BCW)
    cb, cf, cos_t, sin_t = host_consts()
    shared = {
        "w_in": w_in, "w_qkp": w_qkp, "w_out": f(inp["w_out"]), "router_w": f(inp["router_w"]),
        "exp_w_gu": f(inp["exp_w_gu"]), "exp_w_down": f(inp["exp_w_down"]), "exp_b_down": f(inp["exp_b_down"]),
        "pp": pp, "bc": np.ascontiguousarray(bc), "cos_t": cos_t, "sin_t": sin_t, "cst_bf": cb, "cst_f": cf,
    }
    return shared


def kernel(**inputs):
    x = np.ascontiguousarray(np.asarray(inputs["x"], dtype=np.float32))
    shared = host_layout(inputs)
    nc = Builder().build()
    in_maps = [dict(shared, x=x[b]) for b in range(8)]
    res = run_bass_kernel_spmd(nc, in_maps, core_ids=list(range(8)))
    return np.stack([np.asarray(r["y"], dtype=np.float32) for r in res.results], axis=0)
```

```python
import contextlib
import numpy as np
import ml_dtypes
import concourse.bass as bass
import concourse.mybir as mybir
from concourse.bass_utils import run_bass_kernel_spmd

F32 = mybir.dt.float32
BF16 = mybir.dt.bfloat16
I32 = mybir.dt.int32
U32 = mybir.dt.uint32
AF = mybir.ActivationFunctionType
ALU = mybir.AluOpType
AX = mybir.AxisListType

S = 4096
D = 1024
L = 2
NT = S // 128
INW = 4616
NE = 32
CAP = 768
NSLOT = NE * CAP
XGW = 1024
ALPHA = (2.0 * L) ** 0.25
LN_EPS = 1e-5
RMS_EPS = 1e-5
PATTERNS = (1, 4, 16)

C_Q, C_K, C_V, C_Z, C_XS, C_B, C_C, C_DT, C_SB, C_SC, C_SH = (
    0, 512, 1024, 1536, 2048, 2560, 2816, 3072, 3080, 3592, 4104)

PP_CW, PP_CB, PP_SCW, PP_BGU = 0, 32, 40, 52
PPW = 52 + NE * 16
BC_DTB, BC_ALOG, BC_D, BC_NG, BC_RB, BC_L1G, BC_L1B, BC_L2G, BC_L2B = (
    0, 8, 16, 24, 536, 568, 1592, 2616, 3640)
BCW = 4664


class Sched:
    def __init__(self, nc, n_dma_sems=40):
        self.nc = nc
        self.h = {"pe": nc.tensor, "act": nc.scalar, "dve": nc.vector, "pool": nc.gpsimd, "sp": nc.sync}
        self.sem = {k: nc.alloc_semaphore("prog_" + k) for k in self.h}
        self.cnt = {k: 0 for k in self.h}
        self.seen = {k: {} for k in self.h}
        self.dsem = [nc.alloc_semaphore("dma%d" % i) for i in range(n_dma_sems)]
        self.dval = [0] * n_dma_sems
        self.dpool = {"sp": list(range(0, 16)), "pool": list(range(16, 32)), "act": list(range(32, n_dma_sems))}
        self.drr = {"sp": 0, "pool": 0, "act": 0}
        self.res = {}

    def _wait(self, eng, tok):
        kind, key, val = tok
        if kind == "e" and key == eng:
            return
        sk = (kind, key)
        if self.seen[eng].get(sk, 0) >= val:
            return
        sem = self.sem[key] if kind == "e" else self.dsem[key]
        self.h[eng].wait_ge(sem, val)
        self.seen[eng][sk] = val

    def _deps(self, eng, r, w):
        for k in r:
            st = self.res.get(k)
            if st and st[0] is not None:
                tok = st[0]
                if tok[0] == "e" and tok[1] == eng:
                    if tok[2] > self.cnt[eng] - 2 and eng != "pe":
                        sk = ("e", eng)
                        if self.seen[eng].get(sk, 0) < tok[2]:
                            self.h[eng].wait_ge(self.sem[eng], tok[2])
                            self.seen[eng][sk] = tok[2]
                else:
                    self._wait(eng, tok)
        for k in w:
            st = self.res.get(k)
            if st:
                if st[0] is not None:
                    self._wait(eng, st[0])
                for tok in st[1]:
                    self._wait(eng, tok)

    def _commit(self, tok, r, w):
        for k in r:
            st = self.res.setdefault(k, [None, []])
            st[1] = [t for t in st[1] if (t[0], t[1]) != (tok[0], tok[1])] + [tok]
        for k in w:
            self.res[k] = [tok, []]

    def op(self, eng, fn, r=(), w=()):
        self._deps(eng, r, w)
        ins = fn()
        self.cnt[eng] += 1
        ins.then_inc(self.sem[eng], 1)
        self._commit(("e", eng, self.cnt[eng]), r, w)
        return ins

    def dma(self, q, out, in_, r=(), w=(), indirect=None):
        pl = self.dpool[q]
        i = pl[self.drr[q] % len(pl)]
        self.drr[q] += 1
        if self.dval[i] > 0:
            self._wait(q, ("d", i, self.dval[i]))
        self._deps(q, r, w)
        if indirect is None:
            ins = self.h[q].dma_start(out=out, in_=in_)
        else:
            ins = self.nc.gpsimd.indirect_dma_start(out=out, in_=in_, **indirect)
        self.dval[i] += 16
        ins.then_inc(self.dsem[i], 16)
        self._commit(("d", i, self.dval[i]), r, w)
        return ins

    def barrier(self):
        for e in self.h:
            for o in self.h:
                if o != e and self.cnt[o] > 0:
                    self._wait(e, ("e", o, self.cnt[o]))
            for i, v in enumerate(self.dval):
                if v > 0:
                    self._wait(e, ("d", i, v))
        self.res = {}


class Builder:
    def __init__(self, debug=(), nlayers=L, stop_after=None, with_moe=True):
        self.debug = set(debug)
        self.with_moe = with_moe
        self.nlayers = nlayers
        self.stop_after = stop_after
        self.nc = nc = bass.Bass("TRN2", target_bir_lowering=False)
        self.K = Sched(nc)
        self.uid = 0
        ein = lambda n, s, d: nc.dram_tensor(n, s, d, kind="ExternalInput").ap()
        self.x = ein("x", [S, D], F32)
        self.w_in = ein("w_in", [L, D, INW], F32)
        self.w_qkp = ein("w_qkp", [L, D, 1024], F32)
        self.w_out = ein("w_out", [L, 1536, D], F32)
        self.router_w = ein("router_w", [L, D, NE], F32)
        if with_moe:
            self.w_gu = ein("exp_w_gu", [L, NE, D, 2 * D], F32)
            self.w_dn = ein("exp_w_down", [L, NE, D, D], F32)
            self.b_dn = ein("exp_b_down", [L, NE, D], F32)
        self.pp = ein("pp", [L, 128, PPW], F32)
        self.bc = ein("bc", [L, BCW], F32)
        self.cos_t = ein("cos_t", [128, S], F32)
        self.sin_t = ein("sin_t", [128, S], F32)
        self.cst_bf = ein("cst_bf", [128, 1024], BF16)
        self.cst_f = ein("cst_f", [128, 1024], F32)
        self.y = nc.dram_tensor("y", [S, D], F32, kind="ExternalOutput").ap()
        self.qT = self.scr("qT", [4, 128, S], BF16)
        self.kT = self.scr("kT", [4, 128, S], BF16)
        self.v_s = self.scr("v_s", [S, 8 * 66], BF16)
        self.z_s = self.scr("z_s", [S, 512], F32)
        self.dt_s = self.scr("dt_s", [S, 8], F32)
        self.xbc = self.scr("xbc", [8, 128, S], BF16)
        self.mixT = self.scr("mixT", [12, 128, S], BF16)
        self.o_s = self.scr("o_s", [3, S, 8 * 65], F32)
        self.x1 = self.scr("x1", [S, D], F32)
        self.xg = self.scr("xg", [NSLOT, XGW], BF16)
        self.yb = self.scr("yb", [NSLOT, D], F32)
        self.offs = self.scr("offs", [S, 4], I32)
        self.gate_s = self.scr("gate_s", [S, 4], F32)
        self.xres = self.scr("xres", [S, D], F32)

    def scr(self, name, shape, dt):
        kind = "ExternalOutput" if name in self.debug else "Internal"
        return self.nc.dram_tensor(name, shape, dt, kind=kind).ap()

    def sb(self, es, name, shape, dt):
        self.uid += 1
        return es.enter_context(self.nc.sbuf_tensor("%s_%d" % (name, self.uid), shape, dt))

    def ps(self, es, name, shape, dt):
        self.uid += 1
        return es.enter_context(self.nc.psum_tensor("%s_%d" % (name, self.uid), shape, dt))

    def build(self):
        nc, K = self.nc, self.K
        with contextlib.ExitStack() as es:
            self.cbf = self.sb(es, "cbf", [128, 1024], BF16)
            self.cf = self.sb(es, "cf", [128, 1024], F32)
            K.dma("sp", self.cbf[:], self.cst_bf[:, :], w=["cbf"])
            K.dma("sp", self.cf[:], self.cst_f[:, :], w=["cf"])
            self.ident = self.cbf[:, 0:128]
            self.maskpc = self.cbf[:, 128:384]
            self.ones_bf = self.cbf[:, 384:512]
            self.sl_bf = self.cbf[:, 512:640]
            self.maskcp = self.cbf[:, 640:896]
            self.U_f = self.cf[:, 0:128]
            self.SL_f = self.cf[:, 128:256]
            self.ones_f = self.cf[:, 256:384]
            self.mls_f = self.cf[:, 384:512]
            self.iota_e = self.cf[:, 512:544]
            self.zr_f = self.cf[:, 800:928]
            for l in range(self.nlayers):
                src = self.x if l == 0 else self.xres
                dst = self.y if l == self.nlayers - 1 else self.xres
                self.layer(l, src, dst)
                if self.stop_after is not None:
                    break
            K.barrier()
        return nc

    def layer(self, l, src, dst):
        K = self.K
        phases = [self.phase_proj, self.phase_attn, self.phase_ssd, self.phase_outln,
                  self.phase_route, self.phase_experts, self.phase_combine]
        for i, ph in enumerate(phases):
            ph(l, src, dst)
            K.barrier()
            if self.stop_after is not None and i >= self.stop_after:
                if getattr(self, "exp_es", None) is not None and 3 <= i < 5:
                    self.exp_es.close()
                if i == 1:
                    self.ssd_es.close()
                return

    def phase_proj(self, l, src, dst):
        nc, K = self.nc, self.K
        with contextlib.ExitStack() as es:
            xT = self.sb(es, "xT", [128, 8, S], BF16)
            ppt = self.sb(es, "ppt", [128, 52], F32)
            bct = self.sb(es, "bct", [128, 8], F32)
            K.dma("sp", ppt[:], self.pp[l, :, 0:52], w=["ppt"])
            K.dma("sp", bct[:], self.bc[l, BC_DTB:BC_DTB + 8].partition_broadcast(128), w=["bct"])
            sup_src = [("q", self.w_in[l, :, C_Q:C_Q + 512]), ("qp", self.w_qkp[l, :, 0:512]),
                       ("k", self.w_in[l, :, C_K:C_K + 512]), ("kp", self.w_qkp[l, :, 512:1024]),
                       ("xs0", self.w_in[l, :, C_XS:C_XS + 512]), ("xs1", self.w_in[l, :, C_XS + 512:C_XS + 1024]),
                       ("sc", self.w_in[l, :, C_SC:C_SC + 512]), ("sh", self.w_in[l, :, C_SH:C_SH + 512]),
                       ("sb", self.w_in[l, :, C_SB:C_SB + 512])]
            sup = {}
            for nm, _ in sup_src:
                sup[nm] = self.sb(es, "sup_" + nm, [128, 8, 512], BF16)
            sup_when = {4: ["q", "qp"]}
            sup_ap = dict(sup_src)

            def load_sup(nm):
                K.dma("pool", sup[nm][:], sup_ap[nm].rearrange("(k p) c -> p k c", p=128), w=[("sup", nm)])

            with contextlib.ExitStack() as es1:
                xtb = [self.sb(es1, "xtb", [128, D], BF16) for _ in range(3)]
                tp = [self.ps(es1, "tp", [128, 8, 128], BF16) for _ in range(2)]
                for t in range(NT):
                    xb = xtb[t % 3]
                    K.dma("pool", xb[:], src[t * 128:(t + 1) * 128, :], w=[("xtb", t % 3)])
                    for nm in sup_when.get(t, []):
                        load_sup(nm)
                    p = tp[t % 2]
                    for kc in range(8):
                        K.op("pe", lambda: nc.tensor.transpose(p[:, kc, :], xb[:, kc * 128:(kc + 1) * 128], self.ident),
                             r=[("xtb", t % 3), "cbf"], w=[("tp", t % 2)])
                    eng = "act" if t % 2 == 0 else "dve"
                    if eng == "act":
                        K.op("act", lambda: nc.scalar.copy(out=xT[:, :, t * 128:(t + 1) * 128], in_=p[:]),
                             r=[("tp", t % 2)], w=[("xT", t // 4)])
                    else:
                        K.op("dve", lambda: nc.vector.tensor_copy(out=xT[:, :, t * 128:(t + 1) * 128], in_=p[:]),
                             r=[("tp", t % 2)], w=[("xT", t // 4)])
                K.barrier()

            def proj_fm(es2, wsrc_list, evac, nacc=1, tag="fm"):
                acc = [[self.ps(es2, "acc", [128, 512], F32) for _ in range(nacc)] for _ in range(2 if nacc > 1 else 4)]
                nb = len(acc)
                it = 0
                for j, grp in enumerate(wsrc_list):
                    for n in range(8):
                        b = it % nb
                        it += 1
                        for a in range(nacc):
                            nm, c0 = grp[a]
                            for kc in range(8):
                                K.op("pe", lambda: nc.tensor.matmul(
                                    acc[b][a][:], lhsT=sup[nm][:, kc, c0:c0 + 128],
                                    rhs=xT[:, kc, n * 512:(n + 1) * 512], start=(kc == 0), stop=(kc == 7)),
                                    r=[("sup", nm), ("xT", n)], w=[(tag + "acc", b, a)])
                        evac(j, n, acc[b], [(tag + "acc", b, a) for a in range(nacc)])

            for nm in ("k", "kp", "xs0", "xs1", "sc", "sh", "sb"):
                load_sup(nm)
            with contextlib.ExitStack() as es2:
                cos = self.sb(es2, "cos", [128, S], F32)
                sin = self.sb(es2, "sin", [128, S], F32)
                K.dma("sp", cos[:], self.cos_t[:, :], w=["cos"])
                K.dma("sp", sin[:], self.sin_t[:, :], w=["sin"])
                t1 = [self.sb(es2, "t1", [128, 512], F32) for _ in range(2)]
                t2 = [self.sb(es2, "t2", [128, 512], F32) for _ in range(2)]
                ob = [self.sb(es2, "ob", [128, 512], BF16) for _ in range(3)]
                groups = []
                for a_, b_ in (("q", "qp"), ("k", "kp")):
                    for jp in range(4):
                        groups.append([(a_, jp * 128), (b_, jp * 128)])
                cnt = [0]

                def evac_qk(j, n, ps, psr):
                    i = cnt[0]
                    cnt[0] += 1
                    a, b, o = t1[i % 2], t2[i % 2], ob[i % 3]
                    K.op("dve", lambda: nc.vector.tensor_tensor(out=a[:], in0=ps[0][:], in1=cos[:, n * 512:(n + 1) * 512], op=ALU.mult),
                         r=[psr[0], "cos"], w=[("t1", i % 2)])
                    K.op("dve", lambda: nc.vector.tensor_tensor(out=b[:], in0=ps[1][:], in1=sin[:, n * 512:(n + 1) * 512], op=ALU.mult),
                         r=[psr[1], "sin"], w=[("t2", i % 2)])
                    K.op("pool", lambda: nc.gpsimd.tensor_tensor(out=o[:], in0=a[:], in1=b[:], op=ALU.add),
                         r=[("t1", i % 2), ("t2", i % 2)], w=[("ob", i % 3)])
                    dstT = self.qT if j < 4 else self.kT
                    K.dma("sp", dstT[j % 4, :, n * 512:(n + 1) * 512], o[:], r=[("ob", i % 3)], w=[("qk", j, n)])

                proj_fm(es2, groups, evac_qk, nacc=2, tag="qk")
                K.barrier()

            with contextlib.ExitStack() as es2:
                R = [self.sb(es2, "R", [128, S + 4], F32) for _ in range(2)]
                accb = [self.sb(es2, "accb", [128, 1024], F32) for _ in range(3)]
                outb = [self.sb(es2, "outb", [128, 1024], BF16) for _ in range(2)]
                stg = [self.sb(es2, "stg", [128, 512], BF16) for _ in range(2)]
                for i in range(2):
                    K.op("dve", lambda: nc.vector.memset(R[i][:, 0:4], 0.0), w=[("R", i)])
                segc = [0]

                def conv_row(Rb, rkey, wcols, nk, seg_out):
                    for sgi in range(4):
                        i = segc[0]
                        segc[0] += 1
                        ai = i % 2
                        a = accb[ai]
                        t0 = sgi * 1024
                        ce = "dve"
                        ch = nc.vector
                        for k in range(nk):
                            sh = 4 - (nk - 1) + k
                            src_ap = Rb[:, t0 + sh: t0 + sh + 1024]
                            if k == 0:
                                K.op(ce, lambda: ch.tensor_scalar(out=a[:], in0=src_ap, scalar1=wcols[k], scalar2=None, op0=ALU.mult),
                                     r=[rkey, "ppt"], w=[("accb", ai)])
                            else:
                                K.op(ce, lambda: ch.scalar_tensor_tensor(out=a[:], in0=src_ap, scalar=wcols[k], in1=a[:], op0=ALU.mult, op1=ALU.add),
                                     r=[rkey, "ppt", ("accb", ai)], w=[("accb", ai)])
                        seg_out(sgi, a, ("accb", ai), i)

                def evac_xbc(j, n, ps, psr):
                    K.op("act", lambda: nc.scalar.copy(out=R[j % 2][:, 4 + n * 512: 4 + (n + 1) * 512], in_=ps[0][:]),
                         r=[psr[0]], w=[("R", j % 2)])
                    if n == 7:
                        def seg_out(sgi, a, akey, i):
                            o = outb[i % 2]
                            K.op("act", lambda: nc.scalar.activation(out=o[:], in_=a[:], func=AF.Silu, bias=ppt[:, PP_CB + j: PP_CB + j + 1], scale=1.0),
                                 r=[akey, "ppt"], w=[("outb", i % 2)])
                            K.dma("sp", self.xbc[j, :, sgi * 1024:(sgi + 1) * 1024], o[:], r=[("outb", i % 2)], w=[("xbc", j, sgi)])
                        conv_row(R[j % 2], ("R", j % 2), [ppt[:, PP_CW + j * 4 + k: PP_CW + j * 4 + k + 1] for k in range(4)], 4, seg_out)

                groups = [[("xs%d" % (j // 4), (j % 4) * 128)] for j in range(8)]
                proj_fm(es2, groups, evac_xbc, nacc=1, tag="xbc")

                def evac_sconv(jj, n, ps, psr):
                    j, which = jj // 3, jj % 3
                    cols = slice(4 + n * 512, 4 + (n + 1) * 512)
                    if which == 0:
                        K.op("act", lambda: nc.scalar.copy(out=R[0][:, cols], in_=ps[0][:]), r=[psr[0]], w=[("R", 0)])
                    elif which == 1:
                        K.op("dve", lambda: nc.vector.tensor_tensor(out=R[1][:, cols], in0=ps[0][:], in1=R[0][:, cols], op=ALU.mult),
                             r=[psr[0], ("R", 0)], w=[("R", 1)])
                        if n == 7:
                            def seg_out(sgi, a, akey, i):
                                K.op("pool", lambda: nc.gpsimd.tensor_copy(out=R[0][:, 4 + sgi * 1024: 4 + (sgi + 1) * 1024], in_=a[:]),
                                     r=[akey], w=[("R", 0)])
                            conv_row(R[1], ("R", 1), [ppt[:, PP_SCW + j * 3 + k: PP_SCW + j * 3 + k + 1] for k in range(3)], 3, seg_out)
                    else:
                        i = segc[0]
                        segc[0] += 1
                        o = stg[i % 2]
                        K.op("dve", lambda: nc.vector.tensor_tensor(out=o[:], in0=ps[0][:], in1=R[0][:, cols], op=ALU.mult),
                             r=[psr[0], ("R", 0)], w=[("stg", i % 2)])
                        K.dma("sp", self.mixT[8 + j, :, n * 512:(n + 1) * 512], o[:], r=[("stg", i % 2)], w=[("mixT", 8 + j, n)])

                groups = []
                for j in range(4):
                    for nm in ("sc", "sh", "sb"):
                        groups.append([(nm, j * 128)])
                proj_fm(es2, groups, evac_sconv, nacc=1, tag="sc")
                K.barrier()

            with contextlib.ExitStack() as es2:
                wv = self.sb(es2, "wv", [128, 8, 512], BF16)
                wz = self.sb(es2, "wz", [128, 8, 512], BF16)
                wd = self.sb(es2, "wd", [128, 8, 8], BF16)
                K.dma("pool", wv[:], self.w_in[l, :, C_V:C_V + 512].rearrange("(k p) c -> p k c", p=128), w=["wv"])
                K.dma("pool", wz[:], self.w_in[l, :, C_Z:C_Z + 512].rearrange("(k p) c -> p k c", p=128), w=["wz"])
                K.dma("pool", wd[:], self.w_in[l, :, C_DT:C_DT + 8].rearrange("(k p) c -> p k c", p=128), w=["wd"])
                pv = [self.ps(es2, "pv", [128, 512], F32) for _ in range(2)]
                pz = [self.ps(es2, "pz", [128, 512], F32) for _ in range(2)]
                pd = [self.ps(es2, "pd", [128, 8], F32) for _ in range(2)]
                vst = [self.sb(es2, "vst", [128, 8, 66], BF16) for _ in range(2)]
                zst = [self.sb(es2, "zst", [128, 512], F32) for _ in range(2)]
                dtall = self.sb(es2, "dtall", [128, NT, 8], F32)
                for i in range(2):
                    K.op("dve", lambda: nc.vector.memset(vst[i][:], 1.0), w=[("vst", i)])
                for t in range(NT):
                    b = t % 2
                    tok = slice(t * 128, (t + 1) * 128)
                    for (wt_, wk, pt, pk, ncol) in ((wv, "wv", pv, "pv", 512), (wz, "wz", pz, "pz", 512), (wd, "wd", pd, "pd", 8)):
                        for kc in range(8):
                            K.op("pe", lambda: nc.tensor.matmul(pt[b][:], lhsT=xT[:, kc, tok], rhs=wt_[:, kc, :], start=(kc == 0), stop=(kc == 7)),
                                 r=[wk, ("xT", t // 4)], w=[(pk, b)])
                    K.op("act", lambda: nc.scalar.copy(out=vst[b][:, :, 0:64], in_=pv[b][:].rearrange("p (h c) -> p h c", h=8)),
                         r=[("pv", b)], w=[("vst", b)])
                    K.dma("sp", self.v_s[tok, :], vst[b][:].rearrange("p h c -> p (h c)"), r=[("vst", b)], w=[("v_s", t)])
                    K.op("act", lambda: nc.scalar.activation(out=zst[b][:], in_=pz[b][:], func=AF.Silu),
                         r=[("pz", b)], w=[("zst", b)])
                    K.dma("sp", self.z_s[tok, :], zst[b][:], r=[("zst", b)], w=[("z_s", t)])
                    K.op("dve", lambda: nc.vector.tensor_tensor(out=dtall[:, t, :], in0=pd[b][:], in1=bct[:], op=ALU.add),
                         r=[("pd", b), "bct"], w=["dtall"])
                dtf = dtall[:].rearrange("p t h -> p (t h)")
                K.op("act", lambda: nc.scalar.activation(out=dtf, in_=dtf, func=AF.Exp), r=["dtall"], w=["dtall"])
                K.op("act", lambda: nc.scalar.activation(out=dtf, in_=dtf, func=AF.Ln, bias=1.0, scale=1.0), r=["dtall"], w=["dtall"])
                K.dma("sp", self.dt_s.rearrange("(t p) h -> p t h", p=128), dtall[:], r=["dtall"], w=["dt_s"])
                K.barrier()

    def phase_attn(self, l, src, dst):
        nc, K = self.nc, self.K
        self.ssd_es = contextlib.ExitStack()
        self.XB = self.sb(self.ssd_es, "XB", [128, 8, S], BF16)
        with contextlib.ExitStack() as es:
            QT = self.sb(es, "QT", [128, 4, S], BF16)
            KT = self.sb(es, "KT", [128, 4, S], BF16)
            for p in range(4):
                K.dma("sp", QT[:, p, :], self.qT[p, :, :], w=["QT"])
                K.dma("sp", KT[:, p, :], self.kT[p, :, :], w=["KT"])
            for j in range(8):
                K.dma("act", self.XB[:, j, :], self.xbc[j, :, :], w=["XBpre"])
            Vd = self.sb(es, "Vd", [128, 32, 8 * 66], BF16)
            pS = [self.ps(es, "pS", [128, 256], F32) for _ in range(4)]
            pO = [self.ps(es, "pO", [128, 4 * 65], F32) for _ in range(4)]
            pt = [self.sb(es, "pt", [128, 256], BF16) for _ in range(6)]
            pmk = [self.sb(es, "pmk", [128, 256], BF16) for _ in range(24)]
            ost = [self.sb(es, "ost", [128, 8, 65], F32) for _ in range(2)]
            LAG = 3
            NBUF = 6
            for di, d in enumerate(PATTERNS):
                nb = 32 // d
                vsrc = self.v_s.rearrange("(n j dd) c -> dd j n c", j=128, dd=d)
                for r in range(d):
                    K.dma("sp", Vd[:, r * nb:(r + 1) * nb, :], vsrc[r], w=["Vd"])
                items = [(r, n, h) for r in range(d) for n in range(nb) for h in range(8)]
                N = len(items)

                def geom(r, n):
                    b = r * nb + n
                    base = r + d * 128 * n
                    cols = slice(base, base + d * 127 + 1, d)
                    pcols = slice(base - d * 128, base - d * 128 + d * 127 + 1, d)
                    return b, cols, pcols

                for s_ in range(N + LAG):
                    if s_ < N:
                        r, n, h = items[s_]
                        b, cols, pcols = geom(r, n)
                        i = s_ % NBUF
                        sl_ = (n % 3) * 8 + h
                        width = 256 if n + 1 < nb else 128
                        base = r + d * 128 * n
                        qcols = slice(base, base + d * (width - 1) + 1, d)
                        pair, pb = h // 2, 64 * (h % 2)
                        K.op("pe", lambda: nc.tensor.matmul(pS[i % 4][:, 0:width], lhsT=KT[pb:pb + 64, pair, cols],
                                                            rhs=QT[pb:pb + 64, pair, qcols], start=True, stop=True),
                             r=["QT", "KT"], w=[("pS", i % 4)])
                        K.op("act", lambda: nc.scalar.activation(out=pt[i][:, 0:width], in_=pS[i % 4][:, 0:width], func=AF.Exp, scale=0.125),
                             r=[("pS", i % 4)], w=[("pt", i)])
                        K.op("dve", lambda: nc.vector.tensor_tensor(out=pmk[sl_][:, 0:width], in0=pt[i][:, 0:width], in1=self.maskcp[:, 0:width], op=ALU.mult),
                             r=[("pt", i), "cbf"], w=[("pmk", sl_)])
                    if s_ >= LAG:
                        r, n, h = items[s_ - LAG]
                        b, cols, pcols = geom(r, n)
                        bi = b
                        oi = (bi % 2) * 2 + h // 4
                        O = pO[oi][:, (h % 4) * 65:(h % 4 + 1) * 65]
                        slc = (n % 3) * 8 + h
                        slp = ((n - 1) % 3) * 8 + h
                        if n > 0:
                            K.op("pe", lambda: nc.tensor.matmul(O, lhsT=pmk[slp][:, 128:256], rhs=Vd[:, b - 1, h * 66:h * 66 + 65], start=True, stop=False),
                                 r=[("pmk", slp), "Vd"], w=[("pO", oi)])
                        K.op("pe", lambda: nc.tensor.matmul(O, lhsT=pmk[slc][:, 0:128], rhs=Vd[:, b, h * 66:h * 66 + 65], start=(n == 0), stop=True),
                             r=[("pmk", slc), "Vd"], w=[("pO", oi)])
                        if h % 4 == 3:
                            hh = h // 4
                            dst_ap = ost[bi % 2][:, hh * 4:(hh + 1) * 4, :]
                            src_ap = pO[oi][:].rearrange("p (h c) -> p h c", h=4)
                            K.op("dve", lambda: nc.vector.tensor_copy(out=dst_ap, in_=src_ap), r=[("pO", oi)], w=[("ost", bi % 2, hh)])
                        if h == 7:
                            K.dma("sp", self.o_s[di, cols, :], ost[bi % 2][:].rearrange("p h c -> p (h c)"),
                                  r=[("ost", bi % 2, 0), ("ost", bi % 2, 1)], w=[("o_s", di, b)])
            K.barrier()
        with contextlib.ExitStack() as es:
            o3 = [self.sb(es, "o3", [128, 3, 520], F32) for _ in range(4)]
            nm = [self.sb(es, "nm", [128, 8, 65], F32) for _ in range(2)]
            rd = [self.sb(es, "rd", [128, 8], F32) for _ in range(2)]
            at = [self.sb(es, "at", [128, 8, 64], BF16) for _ in range(2)]
            tpo = [self.ps(es, "tpo", [128, 4, 128], BF16) for _ in range(2)]
            atT = [self.sb(es, "atT", [128, 4, 512], BF16) for _ in range(2)]
            for t in range(NT):
                b = t % 2
                b4 = t % 4
                tok = slice(t * 128, (t + 1) * 128)
                K.dma("sp", o3[b4][:], self.o_s[:, tok, :].rearrange("d t c -> t d c"), w=[("o3", b4)])
                nmf = nm[b][:].rearrange("p h c -> p (h c)")
                K.op("dve", lambda: nc.vector.tensor_tensor(out=nmf, in0=o3[b4][:, 0, :], in1=o3[b4][:, 1, :], op=ALU.add),
                     r=[("o3", b4)], w=[("nm", b)])
                K.op("dve", lambda: nc.vector.tensor_tensor(out=nmf, in0=nmf, in1=o3[b4][:, 2, :], op=ALU.add),
                     r=[("o3", b4), ("nm", b)], w=[("nm", b)])
                K.op("dve", lambda: nc.vector.reciprocal(out=rd[b][:], in_=nm[b][:, :, 64]), r=[("nm", b)], w=[("rd", b)])
                K.op("dve", lambda: nc.vector.tensor_tensor(out=at[b][:], in0=nm[b][:, :, 0:64],
                                                            in1=rd[b][:].unsqueeze(2).broadcast_to([128, 8, 64]), op=ALU.mult),
                     r=[("nm", b), ("rd", b)], w=[("at", b)])
                atf = at[b][:].rearrange("p h c -> p (h c)")
                for c in range(4):
                    K.op("pe", lambda: nc.tensor.transpose(tpo[b][:, c, :], atf[:, c * 128:(c + 1) * 128], self.ident),
                         r=[("at", b), "cbf"], w=[("tpo", b)])
                g = (t // 4) % 2
                K.op("act", lambda: nc.scalar.copy(out=atT[g][:, :, (t % 4) * 128:(t % 4 + 1) * 128], in_=tpo[b][:]),
                     r=[("tpo", b)], w=[("atT", g)])
                if t % 4 == 3:
                    K.dma("act", self.mixT[0:4, :, (t // 4) * 512:(t // 4 + 1) * 512].rearrange("c p t -> p c t"), atT[g][:],
                          r=[("atT", g)], w=[("mixT", 0, t // 4)])
            K.barrier()

    def phase_ssd(self, l, src, dst):
        nc, K = self.nc, self.K
        bcast = lambda ap, shape, ax: ap.unsqueeze(ax).broadcast_to(shape)
        with contextlib.ExitStack() as es:
            XB = self.XB
            dtt = self.sb(es, "dtt", [128, 32, 8], F32)
            K.dma("sp", dtt[:], self.dt_s.rearrange("(c p) h -> p c h", p=128), w=["dtt"])
            prm = self.sb(es, "prm", [128, 24 + 512], F32)
            K.dma("sp", prm[:, 0:8], self.bc[l, BC_ALOG:BC_ALOG + 8].partition_broadcast(128), w=["prm"])
            K.dma("sp", prm[:, 16:24], self.bc[l, BC_D:BC_D + 8].partition_broadcast(128), w=["prm"])
            K.dma("sp", prm[:, 24:536], self.bc[l, BC_NG:BC_NG + 512].partition_broadcast(128), w=["prm"])
            a_bc, d_bc, ng_bc = prm[:, 8:16], prm[:, 16:24], prm[:, 24:536]
            A = self.sb(es, "A", [128, 32, 8], F32)
            cum = self.sb(es, "cum", [128, 32, 8], F32)
            clast = self.sb(es, "clast", [128, 32, 8], F32)
            decst = self.sb(es, "decst", [128, 32, 8], F32)
            dtdec = self.sb(es, "dtdec", [128, 32, 8], F32)
            ecum = self.sb(es, "ecum", [128, 32, 8], F32)
            cdec = self.sb(es, "cdec", [128, 32, 8], F32)
            fl = lambda t: t[:].rearrange("p c h -> p (c h)")
            K.op("act", lambda: nc.scalar.activation(out=prm[:, 8:16], in_=prm[:, 0:8], func=AF.Exp), r=["prm"], w=["prm2"])
            K.op("dve", lambda: nc.vector.tensor_scalar(out=prm[:, 8:16], in0=prm[:, 8:16], scalar1=-1.0, scalar2=None, op0=ALU.mult),
                 r=["prm2"], w=["prm2"])
            K.op("dve", lambda: nc.vector.tensor_tensor(out=A[:], in0=dtt[:], in1=bcast(a_bc, [128, 32, 8], 1), op=ALU.mult),
                 r=["prm2", "dtt"], w=["A"])
            with contextlib.ExitStack() as es1:
                pc = self.ps(es1, "pc", [128, 256], F32)
                pl = self.ps(es1, "pl", [128, 256], F32)
                K.op("pe", lambda: nc.tensor.matmul(pc[:], lhsT=self.U_f, rhs=fl(A), start=True, stop=True), r=["A", "cf"], w=["pc"])
                K.op("pe", lambda: nc.tensor.matmul(pl[:], lhsT=self.ones_f, rhs=fl(A), start=True, stop=True), r=["A", "cf"], w=["pl"])
                K.op("dve", lambda: nc.vector.tensor_copy(out=fl(cum), in_=pc[:]), r=["pc"], w=["cum"])
                K.op("dve", lambda: nc.vector.tensor_copy(out=fl(clast), in_=pl[:]), r=["pl"], w=["clast"])
                K.op("dve", lambda: nc.vector.tensor_tensor(out=fl(decst), in0=fl(clast), in1=fl(cum), op=ALU.subtract),
                     r=["cum", "clast"], w=["decst"])
                K.op("act", lambda: nc.scalar.activation(out=fl(decst), in_=fl(decst), func=AF.Exp), r=["decst"], w=["decst"])
                K.op("act", lambda: nc.scalar.activation(out=fl(ecum), in_=fl(cum), func=AF.Exp), r=["cum"], w=["ecum"])
                K.op("act", lambda: nc.scalar.activation(out=fl(cdec), in_=fl(clast), func=AF.Exp), r=["clast"], w=["cdec"])
                K.op("dve", lambda: nc.vector.tensor_tensor(out=fl(dtdec), in0=fl(decst), in1=fl(dtt), op=ALU.mult),
                     r=["decst", "dtt"], w=["dtdec"])
                K.barrier()
            tpx = self.ps(es, "tpx", [128, 6, 128], BF16)
            pG = self.ps(es, "pG", [128, 2, 128], F32)
            pSeg = [self.ps(es, "pSeg", [128, 4, 128], F32) for _ in range(2)]
            pYd = self.ps(es, "pYd", [128, 8, 64], F32)
            pYo = self.ps(es, "pYo", [128, 8, 64], F32)
            pSt = self.ps(es, "pSt", [128, 8, 64], F32)
            tpy = self.ps(es, "tpy", [128, 4, 128], BF16)
            X = [self.sb(es, "X", [128, 8, 64], BF16) for _ in range(3)]
            Xd = [self.sb(es, "Xd", [128, 8, 64], BF16) for _ in range(3)]
            xh = [self.sb(es, "xh", [128, 8, 64], BF16) for _ in range(3)]
            Bt = [self.sb(es, "Bt", [128, 2, 128], BF16) for _ in range(3)]
            GmT = [self.sb(es, "GmT", [128, 2, 128], F32) for _ in range(3)]
            lh = [self.sb(es, "lh", [128, 128], F32) for _ in range(4)]
            eL = [self.sb(es, "eL", [128, 4, 128], F32) for _ in range(6)]
            scT = [self.sb(es, "scT", [128, 4, 128], BF16) for _ in range(6)]
            yo = [self.sb(es, "yo", [128, 8, 64], F32) for _ in range(2)]
            yy = [self.sb(es, "yy", [128, 8, 64], F32) for _ in range(2)]
            td = [self.sb(es, "td", [128, 8, 64], F32) for _ in range(2)]
            zt = [self.sb(es, "zt", [128, 512], F32) for _ in range(3)]
            junk = self.sb(es, "junk", [128, 256], F32)
            ss = [self.sb(es, "ss", [128, 2], F32) for _ in range(2)]
            yn = [self.sb(es, "yn", [128, 512], BF16) for _ in range(2)]
            sdT = [self.sb(es, "sdT", [128, 4, 512], BF16) for _ in range(2)]
            hf = self.sb(es, "hf", [128, 8, 64], F32)
            hbf = [self.sb(es, "hbf", [128, 8, 64], BF16) for _ in range(2)]
            lic = [0]

            def stage_a(c):
                b = c % 2
                b3 = c % 3
                tok = slice(c * 128, (c + 1) * 128)
                K.dma("sp", zt[b3][:], self.z_s[tok, :], w=[("zt", b3)])
                for j in range(6):
                    K.op("pe", lambda: nc.tensor.transpose(tpx[:, j, :], XB[:, j, tok], self.ident), r=["XB", "cbf"], w=["tpx"])
                tx = tpx[:, 0:4, :].rearrange("p a (b c) -> p (a b) c", b=2)
                K.op("act", lambda: nc.scalar.copy(out=xh[b3][:], in_=tx), r=["tpx"], w=[("xh", b3)])
                K.op("act", lambda: nc.scalar.copy(out=Bt[b3][:], in_=tpx[:, 4:6, :]), r=["tpx"], w=[("Bt", b3)])
                K.op("dve", lambda: nc.vector.tensor_tensor(out=X[b3][:], in0=xh[b3][:], in1=bcast(dtt[:, c, :], [128, 8, 64], 2), op=ALU.mult),
                     r=[("xh", b3), "dtt"], w=[("X", b3)])
                K.op("dve", lambda: nc.vector.tensor_tensor(out=Xd[b3][:], in0=xh[b3][:], in1=bcast(dtdec[:, c, :], [128, 8, 64], 2), op=ALU.mult),
                     r=[("xh", b3), "dtdec"], w=[("Xd", b3)])
                for g in range(2):
                    K.op("pe", lambda: nc.tensor.matmul(pG[:, g, :], lhsT=XB[:, 4 + g, tok], rhs=XB[:, 6 + g, tok], start=True, stop=True),
                         r=["XB"], w=["pG"])
                K.op("dve", lambda: nc.vector.tensor_tensor(out=GmT[b3][:], in0=pG[:], in1=bcast(self.mls_f, [128, 2, 128], 1), op=ALU.mult),
                     r=["pG", "cf"], w=[("GmT", b3)])
                for hh in range(2):
                    e = (c * 2 + hh) % 6
                    for h4 in range(4):
                        h = hh * 4 + h4
                        i = lic[0] % 4
                        lic[0] += 1
                        veng = "dve"
                        vh = nc.vector
                        K.op(veng, lambda: vh.tensor_scalar(out=lh[i][:], in0=self.SL_f, scalar1=A[:, c, h:h + 1], scalar2=None, op0=ALU.mult),
                             r=["A", "cf"], w=[("lh", i)])
                        K.op("pe", lambda: nc.tensor.matmul(pSeg[hh][:, h4, :], lhsT=lh[i][:], rhs=self.U_f, start=True, stop=True),
                             r=[("lh", i), "cf"], w=[("pSeg", hh)])
                    K.op("act", lambda: nc.scalar.activation(out=eL[e][:], in_=pSeg[hh][:], func=AF.Exp), r=[("pSeg", hh)], w=[("eL", e)])
                    K.op("pool", lambda: nc.gpsimd.tensor_tensor(out=scT[e][:], in0=eL[e][:], in1=bcast(GmT[b3][:, hh, :], [128, 4, 128], 1), op=ALU.mult),
                         r=[("eL", e), ("GmT", b3)], w=[("scT", e)])

            def stage_b(c):
                b = c % 2
                b3 = c % 3
                tok = slice(c * 128, (c + 1) * 128)
                for h in range(8):
                    e = (c * 2 + h // 4) % 6
                    K.op("pe", lambda: nc.tensor.matmul(pYd[:, h, :], lhsT=scT[e][:, h % 4, :], rhs=X[b3][:, h, :], start=True, stop=True),
                         r=[("scT", e), ("X", b3)], w=["pYd"])
                if c > 0:
                    for h in range(8):
                        K.op("pe", lambda: nc.tensor.matmul(pYo[:, h, :], lhsT=XB[:, 6 + h // 4, tok], rhs=hbf[b][:, h, :], start=True, stop=True),
                             r=["XB", ("hbf", b)], w=["pYo"])
                    K.op("dve", lambda: nc.vector.tensor_tensor(out=yo[b][:], in0=pYo[:], in1=bcast(ecum[:, c, :], [128, 8, 64], 2), op=ALU.mult),
                         r=["pYo", "ecum"], w=[("yo", b)])
                    K.op("dve", lambda: nc.vector.tensor_tensor(out=yy[b][:], in0=pYd[:], in1=yo[b][:], op=ALU.add),
                         r=["pYd", ("yo", b)], w=[("yy", b)])
                else:
                    K.op("dve", lambda: nc.vector.tensor_copy(out=yy[b][:], in_=pYd[:]), r=["pYd"], w=[("yy", b)])
                K.op("pool", lambda: nc.gpsimd.tensor_tensor(out=td[b][:], in0=xh[b3][:], in1=bcast(d_bc, [128, 8, 64], 2), op=ALU.mult),
                     r=[("xh", b3), "prm"], w=[("td", b)])
                K.op("pool", lambda: nc.gpsimd.tensor_tensor(out=yy[b][:], in0=yy[b][:], in1=td[b][:], op=ALU.add),
                     r=[("yy", b), ("td", b)], w=[("yy", b)])
                yf = yy[b][:].rearrange("p h c -> p (h c)")
                K.op("dve", lambda: nc.vector.tensor_tensor(out=yf, in0=yf, in1=zt[b3][:], op=ALU.mult),
                     r=[("yy", b), ("zt", b3)], w=[("yy", b)])
                for g in range(2):
                    K.op("act", lambda: nc.scalar.activation(out=junk[:], in_=yf[:, g * 256:(g + 1) * 256], func=AF.Square, accum_out=ss[b][:, g:g + 1]),
                         r=[("yy", b)], w=[("ss", b, g), "junk"])
                K.op("dve", lambda: nc.vector.tensor_scalar(out=ss[b][:], in0=ss[b][:], scalar1=1.0 / 256.0, scalar2=RMS_EPS, op0=ALU.mult, op1=ALU.add),
                     r=[("ss", b, 0), ("ss", b, 1)], w=[("ss", b, 0), ("ss", b, 1)])
                K.op("act", lambda: nc.scalar.activation(out=ss[b][:], in_=ss[b][:], func=AF.Ln),
                     r=[("ss", b, 0), ("ss", b, 1)], w=[("ss", b, 0), ("ss", b, 1)])
                K.op("act", lambda: nc.scalar.activation(out=ss[b][:], in_=ss[b][:], func=AF.Exp, scale=-0.5),
                     r=[("ss", b, 0), ("ss", b, 1)], w=[("ss", b, 0), ("ss", b, 1)])
                for g in range(2):
                    K.op("dve", lambda: nc.vector.scalar_tensor_tensor(out=yn[b][:, g * 256:(g + 1) * 256], in0=yf[:, g * 256:(g + 1) * 256],
                                                                        scalar=ss[b][:, g:g + 1], in1=ng_bc[:, g * 256:(g + 1) * 256],
                                                                        op0=ALU.mult, op1=ALU.mult),
                         r=[("yy", b), ("ss", b, 0), ("ss", b, 1), "prm"], w=[("yn", b)])

            def stage_t(c):
                b = c % 2
                b3 = c % 3
                for j in range(4):
                    K.op("pe", lambda: nc.tensor.transpose(tpy[:, j, :], yn[b][:, j * 128:(j + 1) * 128], self.ident), r=[("yn", b), "cbf"], w=["tpy"])
                g4 = (c // 4) % 2
                K.op("act", lambda: nc.scalar.copy(out=sdT[g4][:, :, (c % 4) * 128:(c % 4 + 1) * 128], in_=tpy[:]), r=["tpy"], w=[("sdT", g4)])
                if c % 4 == 3:
                    K.dma("act", self.mixT[4:8, :, (c // 4) * 512:(c // 4 + 1) * 512].rearrange("c p t -> p c t"), sdT[g4][:],
                          r=[("sdT", g4)], w=[("mixT", 4, c // 4)])

            def stage_s(c):
                b = c % 2
                b3 = c % 3
                if c < NT - 1:
                    for h in range(8):
                        K.op("pe", lambda: nc.tensor.matmul(pSt[:, h, :], lhsT=Bt[b3][:, h // 4, :], rhs=Xd[b3][:, h, :], start=True, stop=True),
                             r=[("Bt", b3), ("Xd", b3)], w=["pSt"])
                    if c == 0:
                        K.op("dve", lambda: nc.vector.tensor_copy(out=hf[:], in_=pSt[:]), r=["pSt"], w=["hf"])
                    else:
                        K.op("pool", lambda: nc.gpsimd.tensor_tensor(out=hf[:], in0=hf[:], in1=bcast(cdec[:, c, :], [128, 8, 64], 2), op=ALU.mult),
                             r=["hf", "cdec"], w=["hf"])
                        K.op("dve", lambda: nc.vector.tensor_tensor(out=hf[:], in0=hf[:], in1=pSt[:], op=ALU.add), r=["hf", "pSt"], w=["hf"])
                    K.op("act", lambda: nc.scalar.copy(out=hbf[1 - b][:], in_=hf[:]), r=["hf"], w=[("hbf", 1 - b)])

            stage_a(0)
            stage_a(1)
            for c in range(NT):
                stage_s(c)
                stage_b(c)
                if c + 2 < NT:
                    stage_a(c + 2)
                if c >= 1:
                    stage_t(c - 1)
            stage_t(NT - 1)
            K.barrier()
        self.ssd_es.close()

    def layernorm(self, es, name, beng="pool", nbuf=3):
        nc, K = self.nc, self.K
        st = [self.sb(es, name + "st", [128, 2, 6], F32) for _ in range(nbuf)]
        mv = [self.sb(es, name + "mv", [128, 4], F32) for _ in range(nbuf)]
        bh = nc.gpsimd if beng == "pool" else nc.vector

        def stats(i, h, hk):
            mk = (name + "mv", i)
            for c in range(2):
                K.op("dve", lambda: nc.vector.bn_stats(out=st[i][:, c, :], in_=h[:, c * 512:(c + 1) * 512]), r=[hk], w=[(name + "st", i, c)])
            K.op("dve", lambda: nc.vector.bn_aggr(out=mv[i][:, 0:2], in_=st[i][:].rearrange("p a b -> p (a b)")),
                 r=[(name + "st", i, 0), (name + "st", i, 1)], w=[mk])
            K.op("dve", lambda: nc.vector.tensor_scalar(out=mv[i][:, 1:2], in0=mv[i][:, 1:2], scalar1=LN_EPS, scalar2=None, op0=ALU.add), r=[mk], w=[mk])
            K.op("act", lambda: nc.scalar.activation(out=mv[i][:, 1:2], in_=mv[i][:, 1:2], func=AF.Ln), r=[mk], w=[mk])
            K.op("act", lambda: nc.scalar.activation(out=mv[i][:, 2:3], in_=mv[i][:, 1:2], func=AF.Exp, scale=-0.5), r=[mk], w=[mk])
            K.op("dve", lambda: nc.vector.tensor_scalar(out=mv[i][:, 3:4], in0=mv[i][:, 0:1], scalar1=mv[i][:, 2:3], scalar2=-1.0, op0=ALU.mult, op1=ALU.mult),
                 r=[mk], w=[mk])

        def apply(i, h, hk, out, ok, g_bc, b_bc, pk):
            mk = (name + "mv", i)
            K.op("act", lambda: nc.scalar.activation(out=h, in_=h, func=AF.Identity, bias=mv[i][:, 3:4], scale=mv[i][:, 2:3]), r=[hk, mk], w=[hk])
            K.op("dve", lambda: nc.vector.tensor_tensor(out=h, in0=h, in1=g_bc, op=ALU.mult), r=[hk, pk], w=[hk])
            K.op(beng, lambda: bh.tensor_tensor(out=out, in0=h, in1=b_bc, op=ALU.add), r=[hk, pk], w=[ok])
        return stats, apply

    def phase_outln(self, l, src, dst):
        nc, K = self.nc, self.K
        self.exp_es = contextlib.ExitStack()
        self.exp_pre = None
        if self.with_moe:
            ees = self.exp_es
            self.exp_pre = (self.sb(ees, "wgu0", [128, 8, 2 * D], BF16), self.sb(ees, "wdn0", [128, 8, D], BF16), self.sb(ees, "bdn0", [1, D], BF16))
        with contextlib.ExitStack() as es:
            wo = self.sb(es, "wo", [128, 12, D], BF16)
            for c in range(3):
                K.dma("pool", wo[:, c * 4:(c + 1) * 4, :], self.w_out[l, c * 512:(c + 1) * 512, :].rearrange("(c p) d -> p c d", p=128), w=["wo"])
            wr = self.sb(es, "wr", [128, 8, NE], BF16)
            K.dma("pool", wr[:], self.router_w[l].rearrange("(k p) e -> p k e", p=128), w=["wr"])
            if self.exp_pre is not None:
                w0, d0, b0 = self.exp_pre
                for c in range(2):
                    K.dma("pool", w0[:, :, c * D:(c + 1) * D], self.w_gu[l, 0, :, c * D:(c + 1) * D].rearrange("(k p) f -> p k f", p=128), w=[("wgu", 0)])
                K.dma("pool", d0[:], self.w_dn[l, 0].rearrange("(k p) f -> p k f", p=128), w=[("wdn", 0)])
                K.dma("pool", b0[:], self.b_dn[l, 0:1, :], w=[("bdn", 0)])
            lnp = self.sb(es, "lnp", [128, 2 * D + NE], F32)
            K.dma("sp", lnp[:, 0:D], self.bc[l, BC_L1G:BC_L1G + D].partition_broadcast(128), w=["lnp"])
            K.dma("sp", lnp[:, D:2 * D], self.bc[l, BC_L1B:BC_L1B + D].partition_broadcast(128), w=["lnp"])
            K.dma("sp", lnp[:, 2 * D:], self.bc[l, BC_RB:BC_RB + NE].partition_broadcast(128), w=["lnp"])
            g_bc, b_bc, rb_bc = lnp[:, 0:D], lnp[:, D:2 * D], lnp[:, 2 * D:]
            ecap = self.sb(es, "ecap", [128, NE], F32)
            K.op("dve", lambda: nc.vector.tensor_scalar(out=ecap[:], in0=self.iota_e, scalar1=float(CAP), scalar2=None, op0=ALU.mult), r=["cf"], w=["ecap"])
            cntb = self.sb(es, "cntb", [128, NE], F32)
            K.op("dve", lambda: nc.vector.memset(cntb[:], 0.0), w=["cntb"])
            ln_stats, ln_apply = self.layernorm(es, "ln1", beng="dve")
            NB = 3
            TB = 4
            NXB = 4 * TB
            mt = [self.sb(es, "mt", [128, 12, 512], BF16) for _ in range(2)]
            xr = [self.sb(es, "xr", [128, D], F32) for _ in range(NB)]
            hb = [self.sb(es, "hb", [128, D], F32) for _ in range(NB)]
            x1t = [self.sb(es, "x1t", [128, D], F32) for _ in range(NB)]
            x1b = [self.sb(es, "x1b", [128, D], BF16) for _ in range(NXB)]
            x1T = [self.sb(es, "x1T", [128, 8, 128], BF16) for _ in range(2)]
            pm = [self.ps(es, "pm", [128, 512], F32) for _ in range(4)]
            tpr = [self.ps(es, "tpr", [128, 8, 128], BF16) for _ in range(2)]
            plg = self.ps(es, "plg", [128, TB, NE], F32)
            ppos = self.ps(es, "ppos", [128, 2, TB, NE], F32)
            lg = self.sb(es, "lg", [128, TB, NE], F32)
            mx8 = self.sb(es, "mx8", [128, TB, 8], F32)
            ix8 = self.sb(es, "ix8", [128, TB, 8], U32)
            e4 = self.sb(es, "e4", [128, TB, 4], F32)
            rs = self.sb(es, "rs", [128, TB], F32)
            g4 = [self.sb(es, "g4", [128, TB, 4], F32) for _ in range(2)]
            idxf = self.sb(es, "idxf", [128, TB, 4], F32)
            oh = self.sb(es, "oh", [128, TB, 4, NE], F32)
            mskb = self.sb(es, "mskb", [128, TB, NE], BF16)
            cs = self.sb(es, "cs", [128, NE, TB], F32)
            inc = self.sb(es, "inc", [128, NE, TB], F32)
            slot = self.sb(es, "slot", [128, TB, NE], F32)
            tmp = self.sb(es, "tmp", [128, TB, 4, NE], F32)
            off = self.sb(es, "off", [128, TB, 4], F32)
            offi = [self.sb(es, "offi", [128, TB, 4], I32) for _ in range(2)]

            def stage_a(t):
                b = t % NB
                bx = t % NXB
                tok = slice(t * 128, (t + 1) * 128)
                g4i = (t // 4) % 2
                if t == 0:
                    K.dma("sp", mt[0][:], self.mixT[:, :, 0:512].rearrange("c p t -> p c t"), w=[("mt", 0)])
                    K.dma("sp", xr[0][:], src[0:128, :], w=[("xr", 0)])
                if t % 4 == 0 and t + 4 < NT:
                    gn = t // 4 + 1
                    K.dma("sp", mt[gn % 2][:], self.mixT[:, :, gn * 512:(gn + 1) * 512].rearrange("c p t -> p c t"), w=[("mt", gn % 2)])
                if t + 1 < NT:
                    K.dma("sp", xr[(t + 1) % NB][:], src[(t + 1) * 128:(t + 2) * 128, :], w=[("xr", (t + 1) % NB)])
                for half in range(2):
                    pi = (t % 2) * 2 + half
                    for mc in range(12):
                        K.op("pe", lambda: nc.tensor.matmul(pm[pi][:], lhsT=mt[g4i][:, mc, (t % 4) * 128:(t % 4 + 1) * 128],
                                                            rhs=wo[:, mc, half * 512:(half + 1) * 512], start=(mc == 0), stop=(mc == 11)),
                             r=[("mt", g4i), "wo"], w=[("pm", pi)])
                    K.op("dve", lambda: nc.vector.scalar_tensor_tensor(out=hb[b][:, half * 512:(half + 1) * 512], in0=xr[b][:, half * 512:(half + 1) * 512],
                                                                        scalar=ALPHA, in1=pm[pi][:], op0=ALU.mult, op1=ALU.add),
                         r=[("xr", b), ("pm", pi)], w=[("hb", b)])
                ln_stats(b, hb[b][:], ("hb", b))

            def stage_a2(t):
                b = t % NB
                bx = t % NXB
                tok = slice(t * 128, (t + 1) * 128)
                ln_apply(b, hb[b][:], ("hb", b), x1t[b][:], ("x1t", b), g_bc, b_bc, "lnp")
                K.dma("sp", self.x1[tok, :], x1t[b][:], r=[("x1t", b)], w=[("x1", t)])
                K.op("act", lambda: nc.scalar.copy(out=x1b[bx][:], in_=x1t[b][:]), r=[("x1t", b)], w=[("x1b", bx)])

            def stage_r(t):
                b = t % 2
                bx = t % NXB
                for kc in range(8):
                    K.op("pe", lambda: nc.tensor.transpose(tpr[b][:, kc, :], x1b[bx][:, kc * 128:(kc + 1) * 128], self.ident), r=[("x1b", bx), "cbf"], w=[("tpr", b)])
                K.op("act", lambda: nc.scalar.copy(out=x1T[b][:], in_=tpr[b][:]), r=[("tpr", b)], w=[("x1T", b)])
                for kc in range(8):
                    K.op("pe", lambda: nc.tensor.matmul(plg[:, t % TB, :], lhsT=x1T[b][:, kc, :], rhs=wr[:, kc, :], start=(kc == 0), stop=(kc == 7)),
                         r=[("x1T", b), "wr"], w=["plg"])

            def stage_b(tb):
                gb = tb % 2
                bc4 = lambda ap: ap.unsqueeze(3).broadcast_to([128, TB, 4, NE])
                K.op("dve", lambda: nc.vector.tensor_tensor(out=lg[:], in0=plg[:], in1=rb_bc.unsqueeze(1).broadcast_to([128, TB, NE]), op=ALU.add),
                     r=["plg", "lnp"], w=["lg"])
                for i in range(TB):
                    K.op("dve", lambda: nc.vector.max(out=mx8[:, i, :], in_=lg[:, i, :]), r=["lg"], w=[("mx8", i)])
                yield
                for i in range(TB):
                    K.op("dve", lambda: nc.vector.max_index(out=ix8[:, i, :], in_max=mx8[:, i, :], in_values=lg[:, i, :]), r=["lg", ("mx8", i)], w=[("ix8", i)])
                yield
                allmx = [("mx8", i) for i in range(TB)]
                allix = [("ix8", i) for i in range(TB)]
                K.op("dve", lambda: nc.vector.tensor_tensor(out=e4[:], in0=mx8[:, :, 0:4], in1=mx8[:, :, 0:1].broadcast_to([128, TB, 4]), op=ALU.subtract),
                     r=allmx, w=["e4"])
                K.op("act", lambda: nc.scalar.activation(out=e4[:], in_=e4[:], func=AF.Exp), r=["e4"], w=["e4"])
                K.op("dve", lambda: nc.vector.tensor_reduce(out=rs[:], in_=e4[:], axis=AX.X, op=ALU.add), r=["e4"], w=["rs"])
                K.op("dve", lambda: nc.vector.reciprocal(out=rs[:], in_=rs[:]), r=["rs"], w=["rs"])
                K.op("dve", lambda: nc.vector.tensor_tensor(out=g4[gb][:], in0=e4[:], in1=rs[:].unsqueeze(2).broadcast_to([128, TB, 4]), op=ALU.mult),
                     r=["e4", "rs"], w=[("g4", gb)])
                K.dma("act", self.gate_s[tb * TB * 128:(tb + 1) * TB * 128, :].rearrange("(t p) k -> p t k", p=128), g4[gb][:], r=[("g4", gb)], w=[("gate_s", tb)])
                yield
                K.op("dve", lambda: nc.vector.tensor_copy(out=idxf[:], in_=ix8[:, :, 0:4]), r=allix, w=["idxf"])
                K.op("dve", lambda: nc.vector.tensor_tensor(out=oh[:], in0=self.iota_e.unsqueeze(1).unsqueeze(1).broadcast_to([128, TB, 4, NE]),
                                                            in1=bc4(idxf[:]), op=ALU.is_equal), r=["idxf", "cf"], w=["oh"])
                with nc.allow_low_precision(reason="0/1 one-hot sums are exact in bf16"):
                    K.op("dve", lambda: nc.vector.tensor_reduce(out=mskb[:], in_=oh[:].rearrange("p t k e -> p t e k"), axis=AX.X, op=ALU.add), r=["oh"], w=["mskb"])
                yield
                mflat = mskb[:].rearrange("p t e -> p (t e)")
                K.op("pe", lambda: nc.tensor.matmul(ppos[:, 0, :, :].rearrange("p t e -> p (t e)"), lhsT=self.sl_bf, rhs=mflat, start=True, stop=True),
                     r=["mskb", "cbf"], w=["ppos"])
                K.op("pe", lambda: nc.tensor.matmul(ppos[:, 1, :, :].rearrange("p t e -> p (t e)"), lhsT=self.ones_bf, rhs=mflat, start=True, stop=True),
                     r=["mskb", "cbf"], w=["ppos"])
                K.op("dve", lambda: nc.vector.tensor_copy(out=cs[:], in_=ppos[:, 1, :, :].rearrange("p t e -> p e t")), r=["ppos"], w=["cs"])
                K.op("dve", lambda: nc.vector.tensor_tensor_scan(out=inc[:].rearrange("p e t -> p (e t)"), data0=self.zr_f,
                                                                  data1=cs[:].rearrange("p e t -> p (e t)"), initial=0.0, op0=ALU.mult, op1=ALU.add),
                     r=["cs", "cf"], w=["inc"])
                yield
                K.op("dve", lambda: nc.vector.tensor_tensor(out=slot[:], in0=ppos[:, 0, :, :], in1=inc[:].rearrange("p e t -> p t e"), op=ALU.add),
                     r=["ppos", "inc"], w=["slot"])
                K.op("dve", lambda: nc.vector.tensor_tensor(out=slot[:], in0=slot[:], in1=cs[:].rearrange("p e t -> p t e"), op=ALU.subtract),
                     r=["slot", "cs"], w=["slot"])
                K.op("dve", lambda: nc.vector.tensor_tensor(out=slot[:], in0=slot[:], in1=cntb[:].unsqueeze(1).broadcast_to([128, TB, NE]), op=ALU.add),
                     r=["slot", "cntb"], w=["slot"])
                K.op("dve", lambda: nc.vector.tensor_scalar(out=slot[:], in0=slot[:], scalar1=float(CAP - 1), scalar2=None, op0=ALU.min),
                     r=["slot"], w=["slot"])
                K.op("dve", lambda: nc.vector.tensor_tensor(out=slot[:], in0=slot[:], in1=ecap[:].unsqueeze(1).broadcast_to([128, TB, NE]), op=ALU.add),
                     r=["slot", "ecap"], w=["slot"])
                K.op("dve", lambda: nc.vector.tensor_tensor(out=cntb[:], in0=cntb[:], in1=inc[:, :, TB - 1], op=ALU.add), r=["cntb", "inc", "slot"], w=["cntb"])
                yield
                K.op("dve", lambda: nc.vector.tensor_tensor(out=tmp[:], in0=oh[:], in1=slot[:].unsqueeze(2).broadcast_to([128, TB, 4, NE]), op=ALU.mult),
                     r=["oh", "slot"], w=["tmp"])
                K.op("dve", lambda: nc.vector.tensor_reduce(out=off[:], in_=tmp[:], axis=AX.X, op=ALU.add), r=["tmp"], w=["off"])
                K.op("dve", lambda: nc.vector.tensor_copy(out=offi[gb][:], in_=off[:]), r=["off"], w=[("offi", gb)])
                K.dma("act", self.offs[tb * TB * 128:(tb + 1) * TB * 128, :].rearrange("(t p) k -> p t k", p=128), offi[gb][:], r=[("offi", gb)], w=[("offs", tb)])
                yield
                for i in range(TB):
                    bx = (tb * TB + i) % NXB
                    for k in range(4):
                        K.dma("pool", self.xg[:, :], x1b[bx][:], r=[("x1b", bx), ("offi", gb)], w=["xg"],
                              indirect=dict(out_offset=bass.IndirectOffsetOnAxis(offi[gb][:, i, k:k + 1], 0), in_offset=None))

            pending = []
            for t in range(NT + 2):
                if 2 <= t <= NT + 1:
                    stage_r(t - 2)
                    if (t - 1) % TB == 0:
                        for _ in stage_b((t - 1) // TB - 1):
                            pass
                if 1 <= t <= NT:
                    stage_a2(t - 1)
                for g in list(pending):
                    try:
                        next(g)
                    except StopIteration:
                        pending.remove(g)
                if t < NT:
                    stage_a(t)
            for g in pending:
                for _ in g:
                    pass
            K.barrier()

    def phase_route(self, l, src, dst):
        pass

    def phase_experts(self, l, src, dst):
        nc, K = self.nc, self.K
        NCG, CGW = 2, CAP // 2
        NST = CAP // 128
        with contextlib.ExitStack() as es:
            bgu = self.sb(es, "bgu", [128, NE * 16], F32)
            K.dma("sp", bgu[:], self.pp[l, :, PP_BGU:PP_BGU + NE * 16], w=["bgu"])
            wgu = [self.exp_pre[0], self.sb(es, "wgu", [128, 8, 2 * D], BF16)]
            wdn = [self.exp_pre[1], self.sb(es, "wdn", [128, 8, D], BF16)]
            bdn = [self.exp_pre[2], self.sb(es, "bdn", [1, D], BF16)]
            xgt = [self.sb(es, "xgt", [128, XGW], BF16) for _ in range(CAP // 128)]
            xgT = [self.sb(es, "xgT", [128, 8, CAP], BF16) for _ in range(2)]
            hT = [self.sb(es, "hT", [128, 8, CAP], BF16) for _ in range(2)]
            g1 = [self.sb(es, "g1", [128, CGW], F32) for _ in range(2)]
            sg = [self.sb(es, "sg", [128, CGW], F32) for _ in range(2)]
            u1 = [self.sb(es, "u1", [128, CGW], F32) for _ in range(2)]
            yt = [self.sb(es, "yt", [128, D], F32) for _ in range(2)]
            tpx = [self.ps(es, "tpx", [128, 8, 128], BF16) for _ in range(2)]
            pg = [self.ps(es, "pg", [128, 512], F32) for _ in range(2)]
            pu = [self.ps(es, "pu", [128, 512], F32) for _ in range(2)]
            py = [self.ps(es, "py", [128, 512], F32) for _ in range(2)]
            cnt = {"xi": 0, "ei": 0, "yi": 0, "ti": 0}

            def prep_loads(e):
                wb = e % 2
                if e > 0:
                    for c in range(2):
                        K.dma("pool", wgu[wb][:, :, c * D:(c + 1) * D], self.w_gu[l, e, :, c * D:(c + 1) * D].rearrange("(k p) f -> p k f", p=128), w=[("wgu", wb)])
                    K.dma("pool", wdn[wb][:], self.w_dn[l, e].rearrange("(k p) f -> p k f", p=128), w=[("wdn", wb)])
                    K.dma("pool", bdn[wb][:], self.b_dn[l, e:e + 1, :], w=[("bdn", wb)])
                for i in range(NST):
                    r0 = e * CAP + i * 128
                    K.dma("sp", xgt[i][:], self.xg[r0:r0 + 128, :], w=[("xgt", i)])

            def prep_T(e):
                wb = e % 2
                for i in range(NST):
                    x_ = xgt[i]
                    xk = ("xgt", i)
                    ti = cnt["ti"] % 2
                    cnt["ti"] += 1
                    for kc in range(8):
                        K.op("pe", lambda: nc.tensor.transpose(tpx[ti][:, kc, :], x_[:, kc * 128:(kc + 1) * 128], self.ident), r=[xk, "cbf"], w=[("tpx", ti)])
                    K.op("act", lambda: nc.scalar.copy(out=xgT[wb][:, :, i * 128:(i + 1) * 128], in_=tpx[ti][:]), r=[("tpx", ti)], w=[("xgT", wb)])

            def gateup(e):
                wb = e % 2
                for cg in range(NCG):
                    cs = slice(cg * CGW, (cg + 1) * CGW)
                    for j in range(8):
                        p = cnt["ei"] % 2
                        cnt["ei"] += 1
                        for kc in range(8):
                            K.op("pe", lambda: nc.tensor.matmul(pg[p][:, 0:CGW], lhsT=wgu[wb][:, kc, j * 128:(j + 1) * 128], rhs=xgT[wb][:, kc, cs],
                                                                start=(kc == 0), stop=(kc == 7)), r=[("wgu", wb), ("xgT", wb)], w=[("pg", p)])
                        for kc in range(8):
                            K.op("pe", lambda: nc.tensor.matmul(pu[p][:, 0:CGW], lhsT=wgu[wb][:, kc, D + j * 128:D + (j + 1) * 128], rhs=xgT[wb][:, kc, cs],
                                                                start=(kc == 0), stop=(kc == 7)), r=[("wgu", wb), ("xgT", wb)], w=[("pu", p)])
                        bg = bgu[:, e * 16 + j:e * 16 + j + 1]
                        bu = bgu[:, e * 16 + 8 + j:e * 16 + 8 + j + 1]
                        K.op("dve", lambda: nc.vector.tensor_scalar(out=g1[p][:], in0=pg[p][:, 0:CGW], scalar1=bg, scalar2=7.0, op0=ALU.add, op1=ALU.min),
                             r=[("pg", p), "bgu"], w=[("g1", p)])
                        K.op("act", lambda: nc.scalar.activation(out=sg[p][:], in_=g1[p][:], func=AF.Sigmoid, scale=1.702), r=[("g1", p)], w=[("sg", p)])
                        K.op("dve", lambda: nc.vector.tensor_scalar(out=u1[p][:], in0=pu[p][:, 0:CGW], scalar1=bu, scalar2=7.0, op0=ALU.add, op1=ALU.min),
                             r=[("pu", p), "bgu"], w=[("u1", p)])
                        K.op("dve", lambda: nc.vector.tensor_scalar(out=u1[p][:], in0=u1[p][:], scalar1=-7.0, scalar2=1.0, op0=ALU.max, op1=ALU.add),
                             r=[("u1", p)], w=[("u1", p)])
                        K.op("dve", lambda: nc.vector.tensor_tensor(out=g1[p][:], in0=g1[p][:], in1=sg[p][:], op=ALU.mult), r=[("g1", p), ("sg", p)], w=[("g1", p)])
                        K.op("dve", lambda: nc.vector.tensor_tensor(out=hT[wb][:, j, cs], in0=g1[p][:], in1=u1[p][:], op=ALU.mult),
                             r=[("g1", p), ("u1", p)], w=[("hT", wb)])

            def down(e):
                wb = e % 2
                for i in range(NST):
                    yi = cnt["yi"]
                    cnt["yi"] += 1
                    yb_ = yt[yi % 2]
                    yk = ("yt", yi % 2)
                    for half in range(2):
                        hs = slice(half * 512, (half + 1) * 512)
                        K.op("pe", lambda: nc.tensor.matmul(py[half][:], lhsT=self.ones_bf[0:1, :], rhs=bdn[wb][0:1, hs], start=True, stop=False),
                             r=[("bdn", wb), "cbf"], w=[("py", half)])
                        for fc in range(8):
                            K.op("pe", lambda: nc.tensor.matmul(py[half][:], lhsT=hT[wb][:, fc, i * 128:(i + 1) * 128], rhs=wdn[wb][:, fc, hs],
                                                                start=False, stop=(fc == 7)), r=[("hT", wb), ("wdn", wb)], w=[("py", half)])
                        if half == 0:
                            K.op("act", lambda: nc.scalar.copy(out=yb_[:, hs], in_=py[half][:]), r=[("py", half)], w=[yk])
                        else:
                            K.op("dve", lambda: nc.vector.tensor_copy(out=yb_[:, hs], in_=py[half][:]), r=[("py", half)], w=[yk])
                    r0 = e * CAP + i * 128
                    K.dma("sp", self.yb[r0:r0 + 128, :], yb_[:], r=[yk], w=[("yb", e, i)])

            prep_loads(0)
            prep_T(0)
            for e in range(NE):
                if e + 1 < NE:
                    prep_loads(e + 1)
                gateup(e)
                if e + 1 < NE:
                    prep_T(e + 1)
                down(e)
            K.barrier()
        self.exp_es.close()

    def phase_combine(self, l, src, dst):
        nc, K = self.nc, self.K
        with contextlib.ExitStack() as es:
            lnp = self.sb(es, "lnp2", [128, 2 * D], F32)
            K.dma("sp", lnp[:, 0:D], self.bc[l, BC_L2G:BC_L2G + D].partition_broadcast(128), w=["lnp2"])
            K.dma("sp", lnp[:, D:2 * D], self.bc[l, BC_L2B:BC_L2B + D].partition_broadcast(128), w=["lnp2"])
            ln_stats, ln_apply = self.layernorm(es, "ln2", beng="dve")
            NB = 3
            offi = [self.sb(es, "offi2", [128, 4], I32) for _ in range(NB)]
            gt = [self.sb(es, "gt", [128, 4], F32) for _ in range(NB)]
            yg = [self.sb(es, "yg", [128, 4, D], F32) for _ in range(NB)]
            x1t = [self.sb(es, "x1c", [128, D], F32) for _ in range(NB)]
            hb = [self.sb(es, "hb2", [128, D], F32) for _ in range(3)]
            ot = [self.sb(es, "ot", [128, D], F32) for _ in range(2)]
            def loads(t):
                b = t % NB
                tok = slice(t * 128, (t + 1) * 128)
                K.dma("sp", offi[b][:], self.offs[tok, :], w=[("offi2", b)])
                K.dma("sp", gt[b][:], self.gate_s[tok, :], w=[("gt", b)])
                K.dma("sp", x1t[b][:], self.x1[tok, :], w=[("x1c", b)])

            def gathers(t):
                b = t % NB
                for k in range(4):
                    K.dma("pool", yg[b][:, k, :], self.yb[:, :], r=[("offi2", b)], w=[("yg", b, k)],
                          indirect=dict(out_offset=None, in_offset=bass.IndirectOffsetOnAxis(offi[b][:, k:k + 1], 0)))

            def c1(t):
                b = t % NB
                K.op("act", lambda: nc.scalar.activation(out=hb[b][:], in_=yg[b][:, 0, :], func=AF.Copy, scale=gt[b][:, 0:1]),
                     r=[("yg", b, 0), ("gt", b)], w=[("hb2", b)])
                for k in range(1, 4):
                    K.op("dve", lambda: nc.vector.scalar_tensor_tensor(out=hb[b][:], in0=yg[b][:, k, :], scalar=gt[b][:, k:k + 1], in1=hb[b][:],
                                                                        op0=ALU.mult, op1=ALU.add),
                         r=[("yg", b, k), ("gt", b), ("hb2", b)], w=[("hb2", b)])
                K.op("dve", lambda: nc.vector.scalar_tensor_tensor(out=hb[b][:], in0=x1t[b][:], scalar=ALPHA, in1=hb[b][:], op0=ALU.mult, op1=ALU.add),
                     r=[("x1c", b), ("hb2", b)], w=[("hb2", b)])
                ln_stats(b, hb[b][:], ("hb2", b))

            def c2(t):
                b = t % NB
                b2 = t % 2
                tok = slice(t * 128, (t + 1) * 128)
                ln_apply(b, hb[b][:], ("hb2", b), ot[b2][:], ("ot", b2), lnp[:, 0:D], lnp[:, D:2 * D], "lnp2")
                K.dma("act", dst[tok, :], ot[b2][:], r=[("ot", b2)], w=[("dst", t)])

            loads(0)
            loads(1)
            gathers(0)
            for t in range(NT + 1):
                if t + 2 < NT:
                    loads(t + 2)
                if t + 1 < NT:
                    gathers(t + 1)
                if t < NT:
                    c1(t)
                if t >= 1:
                    c2(t - 1)
            K.barrier()


def host_consts():
    bf = ml_dtypes.bfloat16
    k = np.arange(128)[:, None]
    q = np.arange(128)[None, :]
    cb = np.zeros((128, 1024), np.float32)
    cb[:, 0:128] = np.eye(128)
    cb[:, 128:256] = (k >= q)
    cb[:, 256:384] = (k <= q)
    cb[:, 384:512] = 1.0
    cb[:, 512:640] = (k < q)
    cb[:, 640:768] = (k <= q)
    cb[:, 768:896] = (k >= q)
    cf = np.zeros((128, 1024), np.float32)
    cf[:, 0:128] = (k <= q)
    cf[:, 128:256] = (k > q)
    cf[:, 256:384] = 1.0
    cf[:, 384:512] = (q >= k)
    cf[:, 512:544] = np.arange(32)[None, :]
    zr = np.ones((32, 8), np.float32)
    zr[:, 0] = 0.0
    cf[:, 544:800] = zr.reshape(1, 256)
    zr4 = np.ones((32, 4), np.float32)
    zr4[:, 0] = 0.0
    cf[:, 800:928] = zr4.reshape(1, 128)
    half = 32
    inv = (10000.0 ** (-np.arange(half, dtype=np.float32) / half)).astype(np.float32)
    ang = np.arange(S, dtype=np.float32)[None, :] * inv[:, None]
    cos = np.cos(ang).astype(np.float32)
    sin = np.sin(ang).astype(np.float32)
    cos_t = np.concatenate([cos, cos, cos, cos], 0)
    sin_t = np.concatenate([-sin, sin, -sin, sin], 0)
    return cb.astype(bf), cf, np.ascontiguousarray(cos_t), np.ascontiguousarray(sin_t)


def host_layout(inp):
    f = lambda a: np.ascontiguousarray(np.asarray(a, dtype=np.float32))
    w_in = f(inp["w_in"])
    qk = w_in[:, :, 0:1024].reshape(L, D, 16, 2, 32)
    w_qkp = np.ascontiguousarray(qk[:, :, :, ::-1, :].reshape(L, D, 1024))
    pp = np.zeros((L, 128, PPW), np.float32)
    cw = f(inp["ssd_conv_w"])
    pp[:, :, PP_CW:PP_CW + 32] = cw.reshape(L, 4, 8, 128).transpose(0, 3, 2, 1).reshape(L, 128, 32)
    pp[:, :, PP_CB:PP_CB + 8] = f(inp["ssd_conv_b"]).reshape(L, 8, 128).transpose(0, 2, 1)
    scw = f(inp["sc_conv_w"])
    pp[:, :, PP_SCW:PP_SCW + 12] = scw.reshape(L, 3, 4, 128).transpose(0, 3, 2, 1).reshape(L, 128, 12)
    bgu = f(inp["exp_b_gu"])
    pp[:, :, PP_BGU:] = bgu.reshape(L, NE, 16, 128).transpose(0, 3, 1, 2).reshape(L, 128, NE * 16)
    bc = np.concatenate([f(inp["ssd_dt_bias"]), f(inp["ssd_a_log"]), f(inp["ssd_d"]), f(inp["ssd_norm_g"]),
                         f(inp["router_b"]), f(inp["ln1_g"]), f(inp["ln1_b"]), f(inp["ln2_g"]), f(inp["ln2_b"])], axis=1)
    assert bc.shape == (L, BCW)
    cb, cf, cos_t, sin_t = host_consts()
    shared = {
        "w_in": w_in, "w_qkp": w_qkp, "w_out": f(inp["w_out"]), "router_w": f(inp["router_w"]),
        "exp_w_gu": f(inp["exp_w_gu"]), "exp_w_down": f(inp["exp_w_down"]), "exp_b_down": f(inp["exp_b_down"]),
        "pp": pp, "bc": np.ascontiguousarray(bc), "cos_t": cos_t, "sin_t": sin_t, "cst_bf": cb, "cst_f": cf,
    }
    return shared


def kernel(**inputs):
    x = np.ascontiguousarray(np.asarray(inputs["x"], dtype=np.float32))
    shared = host_layout(inputs)
    nc = Builder().build()
    in_maps = [dict(shared, x=x[b]) for b in range(8)]
    res = run_bass_kernel_spmd(nc, in_maps, core_ids=list(range(8)))
    return np.stack([np.asarray(r["y"], dtype=np.float32) for r in res.results], axis=0)
```

```python
import contextlib
import numpy as np
import ml_dtypes
import concourse.bass as bass
import concourse.mybir as mybir
from concourse.bass_utils import run_bass_kernel_spmd

F32 = mybir.dt.float32
BF16 = mybir.dt.bfloat16
I32 = mybir.dt.int32
U32 = mybir.dt.uint32
AF = mybir.ActivationFunctionType
ALU = mybir.AluOpType
AX = mybir.AxisListType

S = 4096
D = 1024
L = 2
NT = S // 128
INW = 4616
NE = 32
CAP = 768
NSLOT = NE * CAP
XGW = 1024
ALPHA = (2.0 * L) ** 0.25
LN_EPS = 1e-5
RMS_EPS = 1e-5
PATTERNS = (1, 4, 16)

C_Q, C_K, C_V, C_Z, C_XS, C_B, C_C, C_DT, C_SB, C_SC, C_SH = (
    0, 512, 1024, 1536, 2048, 2560, 2816, 3072, 3080, 3592, 4104)

PP_CW, PP_CB, PP_SCW, PP_BGU = 0, 32, 40, 52
PPW = 52 + NE * 16
BC_DTB, BC_ALOG, BC_D, BC_NG, BC_RB, BC_L1G, BC_L1B, BC_L2G, BC_L2B = (
    0, 8, 16, 24, 536, 568, 1592, 2616, 3640)
BCW = 4664


class Sched:
    def __init__(self, nc, n_dma_sems=40):
        self.nc = nc
        self.h = {"pe": nc.tensor, "act": nc.scalar, "dve": nc.vector, "pool": nc.gpsimd, "sp": nc.sync}
        self.sem = {k: nc.alloc_semaphore("prog_" + k) for k in self.h}
        self.cnt = {k: 0 for k in self.h}
        self.seen = {k: {} for k in self.h}
        self.dsem = [nc.alloc_semaphore("dma%d" % i) for i in range(n_dma_sems)]
        self.dval = [0] * n_dma_sems
        self.dpool = {"sp": list(range(0, 16)), "pool": list(range(16, 32)), "act": list(range(32, n_dma_sems))}
        self.drr = {"sp": 0, "pool": 0, "act": 0}
        self.res = {}

    def _wait(self, eng, tok):
        kind, key, val = tok
        if kind == "e" and key == eng:
            return
        sk = (kind, key)
        if self.seen[eng].get(sk, 0) >= val:
            return
        sem = self.sem[key] if kind == "e" else self.dsem[key]
        self.h[eng].wait_ge(sem, val)
        self.seen[eng][sk] = val

    def _deps(self, eng, r, w):
        for k in r:
            st = self.res.get(k)
            if st and st[0] is not None:
                tok = st[0]
                if tok[0] == "e" and tok[1] == eng:
                    if tok[2] > self.cnt[eng] - 2 and eng != "pe":
                        sk = ("e", eng)
                        if self.seen[eng].get(sk, 0) < tok[2]:
                            self.h[eng].wait_ge(self.sem[eng], tok[2])
                            self.seen[eng][sk] = tok[2]
                else:
                    self._wait(eng, tok)
        for k in w:
            st = self.res.get(k)
            if st:
                if st[0] is not None:
                    self._wait(eng, st[0])
                for tok in st[1]:
                    self._wait(eng, tok)

    def _commit(self, tok, r, w):
        for k in r:
            st = self.res.setdefault(k, [None, []])
            st[1] = [t for t in st[1] if (t[0], t[1]) != (tok[0], tok[1])] + [tok]
        for k in w:
            self.res[k] = [tok, []]

    def op(self, eng, fn, r=(), w=()):
        self._deps(eng, r, w)
        ins = fn()
        self.cnt[eng] += 1
        ins.then_inc(self.sem[eng], 1)
        self._commit(("e", eng, self.cnt[eng]), r, w)
        return ins

    def dma(self, q, out, in_, r=(), w=(), indirect=None):
        pl = self.dpool[q]
        i = pl[self.drr[q] % len(pl)]
        self.drr[q] += 1
        if self.dval[i] > 0:
            self._wait(q, ("d", i, self.dval[i]))
        self._deps(q, r, w)
        if indirect is None:
            ins = self.h[q].dma_start(out=out, in_=in_)
        else:
            ins = self.nc.gpsimd.indirect_dma_start(out=out, in_=in_, **indirect)
        self.dval[i] += 16
        ins.then_inc(self.dsem[i], 16)
        self._commit(("d", i, self.dval[i]), r, w)
        return ins

    def barrier(self):
        for e in self.h:
            for o in self.h:
                if o != e and self.cnt[o] > 0:
                    self._wait(e, ("e", o, self.cnt[o]))
            for i, v in enumerate(self.dval):
                if v > 0:
                    self._wait(e, ("d", i, v))
        self.res = {}


class Builder:
    def __init__(self, debug=(), nlayers=L, stop_after=None, with_moe=True):
        self.debug = set(debug)
        self.with_moe = with_moe
        self.nlayers = nlayers
        self.stop_after = stop_after
        self.nc = nc = bass.Bass("TRN2", target_bir_lowering=False)
        self.K = Sched(nc)
        self.uid = 0
        ein = lambda n, s, d: nc.dram_tensor(n, s, d, kind="ExternalInput").ap()
        self.x = ein("x", [S, D], F32)
        self.w_in = ein("w_in", [L, D, INW], F32)
        self.w_qkp = ein("w_qkp", [L, D, 1024], F32)
        self.w_out = ein("w_out", [L, 1536, D], F32)
        self.router_w = ein("router_w", [L, D, NE], F32)
        if with_moe:
            self.w_gu = ein("exp_w_gu", [L, NE, D, 2 * D], F32)
            self.w_dn = ein("exp_w_down", [L, NE, D, D], F32)
            self.b_dn = ein("exp_b_down", [L, NE, D], F32)
        self.pp = ein("pp", [L, 128, PPW], F32)
        self.bc = ein("bc", [L, BCW], F32)
        self.cos_t = ein("cos_t", [128, S], F32)
        self.sin_t = ein("sin_t", [128, S], F32)
        self.cst_bf = ein("cst_bf", [128, 1024], BF16)
        self.cst_f = ein("cst_f", [128, 1024], F32)
        self.y = nc.dram_tensor("y", [S, D], F32, kind="ExternalOutput").ap()
        self.qT = self.scr("qT", [4, 128, S], BF16)
        self.kT = self.scr("kT", [4, 128, S], BF16)
        self.v_s = self.scr("v_s", [S, 8 * 66], BF16)
        self.z_s = self.scr("z_s", [S, 512], F32)
        self.dt_s = self.scr("dt_s", [S, 8], F32)
        self.xbc = self.scr("xbc", [8, 128, S], BF16)
        self.mixT = self.scr("mixT", [12, 128, S], BF16)
        self.o_s = self.scr("o_s", [3, S, 8 * 65], F32)
        self.x1 = self.scr("x1", [S, D], F32)
        self.xg = self.scr("xg", [NSLOT, XGW], BF16)
        self.yb = self.scr("yb", [NSLOT, D], F32)
        self.offs = self.scr("offs", [S, 4], I32)
        self.gate_s = self.scr("gate_s", [S, 4], F32)
        self.xres = self.scr("xres", [S, D], F32)

    def scr(self, name, shape, dt):
        kind = "ExternalOutput" if name in self.debug else "Internal"
        return self.nc.dram_tensor(name, shape, dt, kind=kind).ap()

    def sb(self, es, name, shape, dt):
        self.uid += 1
        return es.enter_context(self.nc.sbuf_tensor("%s_%d" % (name, self.uid), shape, dt))

    def ps(self, es, name, shape, dt):
        self.uid += 1
        return es.enter_context(self.nc.psum_tensor("%s_%d" % (name, self.uid), shape, dt))

    def build(self):
        nc, K = self.nc, self.K
        with contextlib.ExitStack() as es:
            self.cbf = self.sb(es, "cbf", [128, 1024], BF16)
            self.cf = self.sb(es, "cf", [128, 1024], F32)
            K.dma("sp", self.cbf[:], self.cst_bf[:, :], w=["cbf"])
            K.dma("sp", self.cf[:], self.cst_f[:, :], w=["cf"])
            self.ident = self.cbf[:, 0:128]
            self.maskpc = self.cbf[:, 128:384]
            self.ones_bf = self.cbf[:, 384:512]
            self.sl_bf = self.cbf[:, 512:640]
            self.maskcp = self.cbf[:, 640:896]
            self.U_f = self.cf[:, 0:128]
            self.SL_f = self.cf[:, 128:256]
            self.ones_f = self.cf[:, 256:384]
            self.mls_f = self.cf[:, 384:512]
            self.iota_e = self.cf[:, 512:544]
            self.zr_f = self.cf[:, 800:928]
            for l in range(self.nlayers):
                src = self.x if l == 0 else self.xres
                dst = self.y if l == self.nlayers - 1 else self.xres
                self.layer(l, src, dst)
                if self.stop_after is not None:
                    break
            K.barrier()
        return nc

    def layer(self, l, src, dst):
        K = self.K
        phases = [self.phase_proj, self.phase_attn, self.phase_ssd, self.phase_outln,
                  self.phase_route, self.phase_experts, self.phase_combine]
        for i, ph in enumerate(phases):
            ph(l, src, dst)
            K.barrier()
            if self.stop_after is not None and i >= self.stop_after:
                if getattr(self, "exp_es", None) is not None and 3 <= i < 5:
                    self.exp_es.close()
                if i == 1:
                    self.ssd_es.close()
                return

    def phase_proj(self, l, src, dst):
        nc, K = self.nc, self.K
        with contextlib.ExitStack() as es:
            xT = self.sb(es, "xT", [128, 8, S], BF16)
            ppt = self.sb(es, "ppt", [128, 52], F32)
            bct = self.sb(es, "bct", [128, 8], F32)
            K.dma("sp", ppt[:], self.pp[l, :, 0:52], w=["ppt"])
            K.dma("sp", bct[:], self.bc[l, BC_DTB:BC_DTB + 8].partition_broadcast(128), w=["bct"])
            sup_src = [("q", self.w_in[l, :, C_Q:C_Q + 512]), ("qp", self.w_qkp[l, :, 0:512]),
                       ("k", self.w_in[l, :, C_K:C_K + 512]), ("kp", self.w_qkp[l, :, 512:1024]),
                       ("xs0", self.w_in[l, :, C_XS:C_XS + 512]), ("xs1", self.w_in[l, :, C_XS + 512:C_XS + 1024]),
                       ("sc", self.w_in[l, :, C_SC:C_SC + 512]), ("sh", self.w_in[l, :, C_SH:C_SH + 512]),
                       ("sb", self.w_in[l, :, C_SB:C_SB + 512])]
            sup = {}
            for nm, _ in sup_src:
                sup[nm] = self.sb(es, "sup_" + nm, [128, 8, 512], BF16)
            sup_when = {4: ["q", "qp"]}
            sup_ap = dict(sup_src)

            def load_sup(nm):
                K.dma("pool", sup[nm][:], sup_ap[nm].rearrange("(k p) c -> p k c", p=128), w=[("sup", nm)])

            with contextlib.ExitStack() as es1:
                xtb = [self.sb(es1, "xtb", [128, D], BF16) for _ in range(3)]
                tp = [self.ps(es1, "tp", [128, 8, 128], BF16) for _ in range(2)]
                for t in range(NT):
                    xb = xtb[t % 3]
                    K.dma("pool", xb[:], src[t * 128:(t + 1) * 128, :], w=[("xtb", t % 3)])
                    for nm in sup_when.get(t, []):
                        load_sup(nm)
                    p = tp[t % 2]
                    for kc in range(8):
                        K.op("pe", lambda: nc.tensor.transpose(p[:, kc, :], xb[:, kc * 128:(kc + 1) * 128], self.ident),
                             r=[("xtb", t % 3), "cbf"], w=[("tp", t % 2)])
                    eng = "act" if t % 2 == 0 else "dve"
                    if eng == "act":
                        K.op("act", lambda: nc.scalar.copy(out=xT[:, :, t * 128:(t + 1) * 128], in_=p[:]),
                             r=[("tp", t % 2)], w=[("xT", t // 4)])
                    else:
                        K.op("dve", lambda: nc.vector.tensor_copy(out=xT[:, :, t * 128:(t + 1) * 128], in_=p[:]),
                             r=[("tp", t % 2)], w=[("xT", t // 4)])
                K.barrier()

            def proj_fm(es2, wsrc_list, evac, nacc=1, tag="fm"):
                acc = [[self.ps(es2, "acc", [128, 512], F32) for _ in range(nacc)] for _ in range(2 if nacc > 1 else 4)]
                nb = len(acc)
                it = 0
                for j, grp in enumerate(wsrc_list):
                    for n in range(8):
                        b = it % nb
                        it += 1
                        for a in range(nacc):
                            nm, c0 = grp[a]
                            for kc in range(8):
                                K.op("pe", lambda: nc.tensor.matmul(
                                    acc[b][a][:], lhsT=sup[nm][:, kc, c0:c0 + 128],
                                    rhs=xT[:, kc, n * 512:(n + 1) * 512], start=(kc == 0), stop=(kc == 7)),
                                    r=[("sup", nm), ("xT", n)], w=[(tag + "acc", b, a)])
                        evac(j, n, acc[b], [(tag + "acc", b, a) for a in range(nacc)])

            for nm in ("k", "kp", "xs0", "xs1", "sc", "sh", "sb"):
                load_sup(nm)
            with contextlib.ExitStack() as es2:
                cos = self.sb(es2, "cos", [128, S], F32)
                sin = self.sb(es2, "sin", [128, S], F32)
                K.dma("sp", cos[:], self.cos_t[:, :], w=["cos"])
                K.dma("sp", sin[:], self.sin_t[:, :], w=["sin"])
                t1 = [self.sb(es2, "t1", [128, 512], F32) for _ in range(2)]
                t2 = [self.sb(es2, "t2", [128, 512], F32) for _ in range(2)]
                ob = [self.sb(es2, "ob", [128, 512], BF16) for _ in range(3)]
                groups = []
                for a_, b_ in (("q", "qp"), ("k", "kp")):
                    for jp in range(4):
                        groups.append([(a_, jp * 128), (b_, jp * 128)])
                cnt = [0]

                def evac_qk(j, n, ps, psr):
                    i = cnt[0]
                    cnt[0] += 1
                    a, b, o = t1[i % 2], t2[i % 2], ob[i % 3]
                    K.op("dve", lambda: nc.vector.tensor_tensor(out=a[:], in0=ps[0][:], in1=cos[:, n * 512:(n + 1) * 512], op=ALU.mult),
                         r=[psr[0], "cos"], w=[("t1", i % 2)])
                    K.op("dve", lambda: nc.vector.tensor_tensor(out=b[:], in0=ps[1][:], in1=sin[:, n * 512:(n + 1) * 512], op=ALU.mult),
                         r=[psr[1], "sin"], w=[("t2", i % 2)])
                    K.op("pool", lambda: nc.gpsimd.tensor_tensor(out=o[:], in0=a[:], in1=b[:], op=ALU.add),
                         r=[("t1", i % 2), ("t2", i % 2)], w=[("ob", i % 3)])
                    dstT = self.qT if j < 4 else self.kT
                    K.dma("sp", dstT[j % 4, :, n * 512:(n + 1) * 512], o[:], r=[("ob", i % 3)], w=[("qk", j, n)])

                proj_fm(es2, groups, evac_qk, nacc=2, tag="qk")
                K.barrier()

            with contextlib.ExitStack() as es2:
                R = [self.sb(es2, "R", [128, S + 4], F32) for _ in range(2)]
                accb = [self.sb(es2, "accb", [128, 1024], F32) for _ in range(3)]
                outb = [self.sb(es2, "outb", [128, 1024], BF16) for _ in range(2)]
                stg = [self.sb(es2, "stg", [128, 512], BF16) for _ in range(2)]
                for i in range(2):
                    K.op("dve", lambda: nc.vector.memset(R[i][:, 0:4], 0.0), w=[("R", i)])
                segc = [0]

                def conv_row(Rb, rkey, wcols, nk, seg_out):
                    for sgi in range(4):
                        i = segc[0]
                        segc[0] += 1
                        ai = i % 2
                        a = accb[ai]
                        t0 = sgi * 1024
                        ce = "dve"
                        ch = nc.vector
                        for k in range(nk):
                            sh = 4 - (nk - 1) + k
                            src_ap = Rb[:, t0 + sh: t0 + sh + 1024]
                            if k == 0:
                                K.op(ce, lambda: ch.tensor_scalar(out=a[:], in0=src_ap, scalar1=wcols[k], scalar2=None, op0=ALU.mult),
                                     r=[rkey, "ppt"], w=[("accb", ai)])
                            else:
                                K.op(ce, lambda: ch.scalar_tensor_tensor(out=a[:], in0=src_ap, scalar=wcols[k], in1=a[:], op0=ALU.mult, op1=ALU.add),
                                     r=[rkey, "ppt", ("accb", ai)], w=[("accb", ai)])
                        seg_out(sgi, a, ("accb", ai), i)

                def evac_xbc(j, n, ps, psr):
                    K.op("act", lambda: nc.scalar.copy(out=R[j % 2][:, 4 + n * 512: 4 + (n + 1) * 512], in_=ps[0][:]),
                         r=[psr[0]], w=[("R", j % 2)])
                    if n == 7:
                        def seg_out(sgi, a, akey, i):
                            o = outb[i % 2]
                            K.op("act", lambda: nc.scalar.activation(out=o[:], in_=a[:], func=AF.Silu, bias=ppt[:, PP_CB + j: PP_CB + j + 1], scale=1.0),
                                 r=[akey, "ppt"], w=[("outb", i % 2)])
                            K.dma("sp", self.xbc[j, :, sgi * 1024:(sgi + 1) * 1024], o[:], r=[("outb", i % 2)], w=[("xbc", j, sgi)])
                        conv_row(R[j % 2], ("R", j % 2), [ppt[:, PP_CW + j * 4 + k: PP_CW + j * 4 + k + 1] for k in range(4)], 4, seg_out)

                groups = [[("xs%d" % (j // 4), (j % 4) * 128)] for j in range(8)]
                proj_fm(es2, groups, evac_xbc, nacc=1, tag="xbc")

                def evac_sconv(jj, n, ps, psr):
                    j, which = jj // 3, jj % 3
                    cols = slice(4 + n * 512, 4 + (n + 1) * 512)
                    if which == 0:
                        K.op("act", lambda: nc.scalar.copy(out=R[0][:, cols], in_=ps[0][:]), r=[psr[0]], w=[("R", 0)])
                    elif which == 1:
                        K.op("dve", lambda: nc.vector.tensor_tensor(out=R[1][:, cols], in0=ps[0][:], in1=R[0][:, cols], op=ALU.mult),
                             r=[psr[0], ("R", 0)], w=[("R", 1)])
                        if n == 7:
                            def seg_out(sgi, a, akey, i):
                                K.op("pool", lambda: nc.gpsimd.tensor_copy(out=R[0][:, 4 + sgi * 1024: 4 + (sgi + 1) * 1024], in_=a[:]),
                                     r=[akey], w=[("R", 0)])
                            conv_row(R[1], ("R", 1), [ppt[:, PP_SCW + j * 3 + k: PP_SCW + j * 3 + k + 1] for k in range(3)], 3, seg_out)
                    else:
                        i = segc[0]
                        segc[0] += 1
                        o = stg[i % 2]
                        K.op("dve", lambda: nc.vector.tensor_tensor(out=o[:], in0=ps[0][:], in1=R[0][:, cols], op=ALU.mult),
                             r=[psr[0], ("R", 0)], w=[("stg", i % 2)])
                        K.dma("sp", self.mixT[8 + j, :, n * 512:(n + 1) * 512], o[:], r=[("stg", i % 2)], w=[("mixT", 8 + j, n)])

                groups = []
                for j in range(4):
                    for nm in ("sc", "sh", "sb"):
                        groups.append([(nm, j * 128)])
                proj_fm(es2, groups, evac_sconv, nacc=1, tag="sc")
                K.barrier()

            with contextlib.ExitStack() as es2:
                wv = self.sb(es2, "wv", [128, 8, 512], BF16)
                wz = self.sb(es2, "wz", [128, 8, 512], BF16)
                wd = self.sb(es2, "wd", [128, 8, 8], BF16)
                K.dma("pool", wv[:], self.w_in[l, :, C_V:C_V + 512].rearrange("(k p) c -> p k c", p=128), w=["wv"])
                K.dma("pool", wz[:], self.w_in[l, :, C_Z:C_Z + 512].rearrange("(k p) c -> p k c", p=128), w=["wz"])
                K.dma("pool", wd[:], self.w_in[l, :, C_DT:C_DT + 8].rearrange("(k p) c -> p k c", p=128), w=["wd"])
                pv = [self.ps(es2, "pv", [128, 512], F32) for _ in range(2)]
                pz = [self.ps(es2, "pz", [128, 512], F32) for _ in range(2)]
                pd = [self.ps(es2, "pd", [128, 8], F32) for _ in range(2)]
                vst = [self.sb(es2, "vst", [128, 8, 66], BF16) for _ in range(2)]
                zst = [self.sb(es2, "zst", [128, 512], F32) for _ in range(2)]
                dtall = self.sb(es2, "dtall", [128, NT, 8], F32)
                for i in range(2):
                    K.op("dve", lambda: nc.vector.memset(vst[i][:], 1.0), w=[("vst", i)])
                for t in range(NT):
                    b = t % 2
                    tok = slice(t * 128, (t + 1) * 128)
                    for (wt_, wk, pt, pk, ncol) in ((wv, "wv", pv, "pv", 512), (wz, "wz", pz, "pz", 512), (wd, "wd", pd, "pd", 8)):
                        for kc in range(8):
                            K.op("pe", lambda: nc.tensor.matmul(pt[b][:], lhsT=xT[:, kc, tok], rhs=wt_[:, kc, :], start=(kc == 0), stop=(kc == 7)),
                                 r=[wk, ("xT", t // 4)], w=[(pk, b)])
                    K.op("act", lambda: nc.scalar.copy(out=vst[b][:, :, 0:64], in_=pv[b][:].rearrange("p (h c) -> p h c", h=8)),
                         r=[("pv", b)], w=[("vst", b)])
                    K.dma("sp", self.v_s[tok, :], vst[b][:].rearrange("p h c -> p (h c)"), r=[("vst", b)], w=[("v_s", t)])
                    K.op("act", lambda: nc.scalar.activation(out=zst[b][:], in_=pz[b][:], func=AF.Silu),
                         r=[("pz", b)], w=[("zst", b)])
                    K.dma("sp", self.z_s[tok, :], zst[b][:], r=[("zst", b)], w=[("z_s", t)])
                    K.op("dve", lambda: nc.vector.tensor_tensor(out=dtall[:, t, :], in0=pd[b][:], in1=bct[:], op=ALU.add),
                         r=[("pd", b), "bct"], w=["dtall"])
                dtf = dtall[:].rearrange("p t h -> p (t h)")
                K.op("act", lambda: nc.scalar.activation(out=dtf, in_=dtf, func=AF.Exp), r=["dtall"], w=["dtall"])
                K.op("act", lambda: nc.scalar.activation(out=dtf, in_=dtf, func=AF.Ln, bias=1.0, scale=1.0), r=["dtall"], w=["dtall"])
                K.dma("sp", self.dt_s.rearrange("(t p) h -> p t h", p=128), dtall[:], r=["dtall"], w=["dt_s"])
                K.barrier()

    def phase_attn(self, l, src, dst):
        nc, K = self.nc, self.K
        self.ssd_es = contextlib.ExitStack()
        self.XB = self.sb(self.ssd_es, "XB", [128, 8, S], BF16)
        with contextlib.ExitStack() as es:
            QT = self.sb(es, "QT", [128, 4, S], BF16)
            KT = self.sb(es, "KT", [128, 4, S], BF16)
            for p in range(4):
                K.dma("sp", QT[:, p, :], self.qT[p, :, :], w=["QT"])
                K.dma("sp", KT[:, p, :], self.kT[p, :, :], w=["KT"])
            for j in range(8):
                K.dma("act", self.XB[:, j, :], self.xbc[j, :, :], w=["XBpre"])
            Vd = self.sb(es, "Vd", [128, 32, 8 * 66], BF16)
            pS = [self.ps(es, "pS", [128, 256], F32) for _ in range(4)]
            pO = [self.ps(es, "pO", [128, 4 * 65], F32) for _ in range(4)]
            pt = [self.sb(es, "pt", [128, 256], BF16) for _ in range(6)]
            pmk = [self.sb(es, "pmk", [128, 256], BF16) for _ in range(24)]
            ost = [self.sb(es, "ost", [128, 8, 65], F32) for _ in range(2)]
            LAG = 3
            NBUF = 6
            for di, d in enumerate(PATTERNS):
                nb = 32 // d
                vsrc = self.v_s.rearrange("(n j dd) c -> dd j n c", j=128, dd=d)
                for r in range(d):
                    K.dma("sp", Vd[:, r * nb:(r + 1) * nb, :], vsrc[r], w=["Vd"])
                items = [(r, n, h) for r in range(d) for n in range(nb) for h in range(8)]
                N = len(items)

                def geom(r, n):
                    b = r * nb + n
                    base = r + d * 128 * n
                    cols = slice(base, base + d * 127 + 1, d)
                    pcols = slice(base - d * 128, base - d * 128 + d * 127 + 1, d)
                    return b, cols, pcols

                for s_ in range(N + LAG):
                    if s_ < N:
                        r, n, h = items[s_]
                        b, cols, pcols = geom(r, n)
                        i = s_ % NBUF
                        sl_ = (n % 3) * 8 + h
                        width = 256 if n + 1 < nb else 128
                        base = r + d * 128 * n
                        qcols = slice(base, base + d * (width - 1) + 1, d)
                        pair, pb = h // 2, 64 * (h % 2)
                        K.op("pe", lambda: nc.tensor.matmul(pS[i % 4][:, 0:width], lhsT=KT[pb:pb + 64, pair, cols],
                                                            rhs=QT[pb:pb + 64, pair, qcols], start=True, stop=True),
                             r=["QT", "KT"], w=[("pS", i % 4)])
                        K.op("act", lambda: nc.scalar.activation(out=pt[i][:, 0:width], in_=pS[i % 4][:, 0:width], func=AF.Exp, scale=0.125),
                             r=[("pS", i % 4)], w=[("pt", i)])
                        K.op("dve", lambda: nc.vector.tensor_tensor(out=pmk[sl_][:, 0:width], in0=pt[i][:, 0:width], in1=self.maskcp[:, 0:width], op=ALU.mult),
                             r=[("pt", i), "cbf"], w=[("pmk", sl_)])
                    if s_ >= LAG:
                        r, n, h = items[s_ - LAG]
                        b, cols, pcols = geom(r, n)
                        bi = b
                        oi = (bi % 2) * 2 + h // 4
                        O = pO[oi][:, (h % 4) * 65:(h % 4 + 1) * 65]
                        slc = (n % 3) * 8 + h
                        slp = ((n - 1) % 3) * 8 + h
                        if n > 0:
                            K.op("pe", lambda: nc.tensor.matmul(O, lhsT=pmk[slp][:, 128:256], rhs=Vd[:, b - 1, h * 66:h * 66 + 65], start=True, stop=False),
                                 r=[("pmk", slp), "Vd"], w=[("pO", oi)])
                        K.op("pe", lambda: nc.tensor.matmul(O, lhsT=pmk[slc][:, 0:128], rhs=Vd[:, b, h * 66:h * 66 + 65], start=(n == 0), stop=True),
                             r=[("pmk", slc), "Vd"], w=[("pO", oi)])
                        if h % 4 == 3:
                            hh = h // 4
                            dst_ap = ost[bi % 2][:, hh * 4:(hh + 1) * 4, :]
                            src_ap = pO[oi][:].rearrange("p (h c) -> p h c", h=4)
                            K.op("dve", lambda: nc.vector.tensor_copy(out=dst_ap, in_=src_ap), r=[("pO", oi)], w=[("ost", bi % 2, hh)])
                        if h == 7:
                            K.dma("sp", self.o_s[di, cols, :], ost[bi % 2][:].rearrange("p h c -> p (h c)"),
                                  r=[("ost", bi % 2, 0), ("ost", bi % 2, 1)], w=[("o_s", di, b)])
            K.barrier()
        with contextlib.ExitStack() as es:
            o3 = [self.sb(es, "o3", [128, 3, 520], F32) for _ in range(4)]
            nm = [self.sb(es, "nm", [128, 8, 65], F32) for _ in range(2)]
            rd = [self.sb(es, "rd", [128, 8], F32) for _ in range(2)]
            at = [self.sb(es, "at", [128, 8, 64], BF16) for _ in range(2)]
            tpo = [self.ps(es, "tpo", [128, 4, 128], BF16) for _ in range(2)]
            atT = [self.sb(es, "atT", [128, 4, 512], BF16) for _ in range(2)]
            for t in range(NT):
                b = t % 2
                b4 = t % 4
                tok = slice(t * 128, (t + 1) * 128)
                K.dma("sp", o3[b4][:], self.o_s[:, tok, :].rearrange("d t c -> t d c"), w=[("o3", b4)])
                nmf = nm[b][:].rearrange("p h c -> p (h c)")
                K.op("dve", lambda: nc.vector.tensor_tensor(out=nmf, in0=o3[b4][:, 0, :], in1=o3[b4][:, 1, :], op=ALU.add),
                     r=[("o3", b4)], w=[("nm", b)])
                K.op("dve", lambda: nc.vector.tensor_tensor(out=nmf, in0=nmf, in1=o3[b4][:, 2, :], op=ALU.add),
                     r=[("o3", b4), ("nm", b)], w=[("nm", b)])
                K.op("dve", lambda: nc.vector.reciprocal(out=rd[b][:], in_=nm[b][:, :, 64]), r=[("nm", b)], w=[("rd", b)])
                K.op("dve", lambda: nc.vector.tensor_tensor(out=at[b][:], in0=nm[b][:, :, 0:64],
                                                            in1=rd[b][:].unsqueeze(2).broadcast_to([128, 8, 64]), op=ALU.mult),
                     r=[("nm", b), ("rd", b)], w=[("at", b)])
                atf = at[b][:].rearrange("p h c -> p (h c)")
                for c in range(4):
                    K.op("pe", lambda: nc.tensor.transpose(tpo[b][:, c, :], atf[:, c * 128:(c + 1) * 128], self.ident),
                         r=[("at", b), "cbf"], w=[("tpo", b)])
                g = (t // 4) % 2
                K.op("act", lambda: nc.scalar.copy(out=atT[g][:, :, (t % 4) * 128:(t % 4 + 1) * 128], in_=tpo[b][:]),
                     r=[("tpo", b)], w=[("atT", g)])
                if t % 4 == 3:
                    K.dma("act", self.mixT[0:4, :, (t // 4) * 512:(t // 4 + 1) * 512].rearrange("c p t -> p c t"), atT[g][:],
                          r=[("atT", g)], w=[("mixT", 0, t // 4)])
            K.barrier()

    def phase_ssd(self, l, src, dst):
        nc, K = self.nc, self.K
        bcast = lambda ap, shape, ax: ap.unsqueeze(ax).broadcast_to(shape)
        with contextlib.ExitStack() as es:
            XB = self.XB
            dtt = self.sb(es, "dtt", [128, 32, 8], F32)
            K.dma("sp", dtt[:], self.dt_s.rearrange("(c p) h -> p c h", p=128), w=["dtt"])
            prm = self.sb(es, "prm", [128, 24 + 512], F32)
            K.dma("sp", prm[:, 0:8], self.bc[l, BC_ALOG:BC_ALOG + 8].partition_broadcast(128), w=["prm"])
            K.dma("sp", prm[:, 16:24], self.bc[l, BC_D:BC_D + 8].partition_broadcast(128), w=["prm"])
            K.dma("sp", prm[:, 24:536], self.bc[l, BC_NG:BC_NG + 512].partition_broadcast(128), w=["prm"])
            a_bc, d_bc, ng_bc = prm[:, 8:16], prm[:, 16:24], prm[:, 24:536]
            A = self.sb(es, "A", [128, 32, 8], F32)
            cum = self.sb(es, "cum", [128, 32, 8], F32)
            clast = self.sb(es, "clast", [128, 32, 8], F32)
            decst = self.sb(es, "decst", [128, 32, 8], F32)
            dtdec = self.sb(es, "dtdec", [128, 32, 8], F32)
            ecum = self.sb(es, "ecum", [128, 32, 8], F32)
            cdec = self.sb(es, "cdec", [128, 32, 8], F32)
            fl = lambda t: t[:].rearrange("p c h -> p (c h)")
            K.op("act", lambda: nc.scalar.activation(out=prm[:, 8:16], in_=prm[:, 0:8], func=AF.Exp), r=["prm"], w=["prm2"])
            K.op("dve", lambda: nc.vector.tensor_scalar(out=prm[:, 8:16], in0=prm[:, 8:16], scalar1=-1.0, scalar2=None, op0=ALU.mult),
                 r=["prm2"], w=["prm2"])
            K.op("dve", lambda: nc.vector.tensor_tensor(out=A[:], in0=dtt[:], in1=bcast(a_bc, [128, 32, 8], 1), op=ALU.mult),
                 r=["prm2", "dtt"], w=["A"])
            with contextlib.ExitStack() as es1:
                pc = self.ps(es1, "pc", [128, 256], F32)
                pl = self.ps(es1, "pl", [128, 256], F32)
                K.op("pe", lambda: nc.tensor.matmul(pc[:], lhsT=self.U_f, rhs=fl(A), start=True, stop=True), r=["A", "cf"], w=["pc"])
                K.op("pe", lambda: nc.tensor.matmul(pl[:], lhsT=self.ones_f, rhs=fl(A), start=True, stop=True), r=["A", "cf"], w=["pl"])
                K.op("dve", lambda: nc.vector.tensor_copy(out=fl(cum), in_=pc[:]), r=["pc"], w=["cum"])
                K.op("dve", lambda: nc.vector.tensor_copy(out=fl(clast), in_=pl[:]), r=["pl"], w=["clast"])
                K.op("dve", lambda: nc.vector.tensor_tensor(out=fl(decst), in0=fl(clast), in1=fl(cum), op=ALU.subtract),
                     r=["cum", "clast"], w=["decst"])
                K.op("act", lambda: nc.scalar.activation(out=fl(decst), in_=fl(decst), func=AF.Exp), r=["decst"], w=["decst"])
                K.op("act", lambda: nc.scalar.activation(out=fl(ecum), in_=fl(cum), func=AF.Exp), r=["cum"], w=["ecum"])
                K.op("act", lambda: nc.scalar.activation(out=fl(cdec), in_=fl(clast), func=AF.Exp), r=["clast"], w=["cdec"])
                K.op("dve", lambda: nc.vector.tensor_tensor(out=fl(dtdec), in0=fl(decst), in1=fl(dtt), op=ALU.mult),
                     r=["decst", "dtt"], w=["dtdec"])
                K.barrier()
            tpx = self.ps(es, "tpx", [128, 6, 128], BF16)
            pG = self.ps(es, "pG", [128, 2, 128], F32)
            pSeg = [self.ps(es, "pSeg", [128, 4, 128], F32) for _ in range(2)]
            pYd = self.ps(es, "pYd", [128, 8, 64], F32)
            pYo = self.ps(es, "pYo", [128, 8, 64], F32)
            pSt = self.ps(es, "pSt", [128, 8, 64], F32)
            tpy = self.ps(es, "tpy", [128, 4, 128], BF16)
            X = [self.sb(es, "X", [128, 8, 64], BF16) for _ in range(3)]
            Xd = [self.sb(es, "Xd", [128, 8, 64], BF16) for _ in range(3)]
            xh = [self.sb(es, "xh", [128, 8, 64], BF16) for _ in range(3)]
            Bt = [self.sb(es, "Bt", [128, 2, 128], BF16) for _ in range(3)]
            GmT = [self.sb(es, "GmT", [128, 2, 128], F32) for _ in range(3)]
            lh = [self.sb(es, "lh", [128, 128], F32) for _ in range(16)]
            eL = [self.sb(es, "eL", [128, 4, 128], F32) for _ in range(6)]
            scT = [self.sb(es, "scT", [128, 4, 128], BF16) for _ in range(6)]
            yo = [self.sb(es, "yo", [128, 8, 64], F32) for _ in range(2)]
            yy = [self.sb(es, "yy", [128, 8, 64], F32) for _ in range(2)]
            td = [self.sb(es, "td", [128, 8, 64], F32) for _ in range(2)]
            zt = [self.sb(es, "zt", [128, 512], F32) for _ in range(3)]
            junk = self.sb(es, "junk", [128, 256], F32)
            ss = [self.sb(es, "ss", [128, 2], F32) for _ in range(2)]
            yn = [self.sb(es, "yn", [128, 512], BF16) for _ in range(2)]
            sdT = [self.sb(es, "sdT", [128, 4, 512], BF16) for _ in range(2)]
            hf = self.sb(es, "hf", [128, 8, 64], F32)
            hbf = [self.sb(es, "hbf", [128, 8, 64], BF16) for _ in range(2)]
            lic = [0]

            def stage_a(c):
                b = c % 2
                b3 = c % 3
                tok = slice(c * 128, (c + 1) * 128)
                K.dma("sp", zt[b3][:], self.z_s[tok, :], w=[("zt", b3)])
                for j in range(6):
                    K.op("pe", lambda: nc.tensor.transpose(tpx[:, j, :], XB[:, j, tok], self.ident), r=["XB", "cbf"], w=["tpx"])
                tx = tpx[:, 0:4, :].rearrange("p a (b c) -> p (a b) c", b=2)
                K.op("act", lambda: nc.scalar.copy(out=xh[b3][:], in_=tx), r=["tpx"], w=[("xh", b3)])
                K.op("act", lambda: nc.scalar.copy(out=Bt[b3][:], in_=tpx[:, 4:6, :]), r=["tpx"], w=[("Bt", b3)])
                K.op("dve", lambda: nc.vector.tensor_tensor(out=X[b3][:], in0=xh[b3][:], in1=bcast(dtt[:, c, :], [128, 8, 64], 2), op=ALU.mult),
                     r=[("xh", b3), "dtt"], w=[("X", b3)])
                K.op("dve", lambda: nc.vector.tensor_tensor(out=Xd[b3][:], in0=xh[b3][:], in1=bcast(dtdec[:, c, :], [128, 8, 64], 2), op=ALU.mult),
                     r=[("xh", b3), "dtdec"], w=[("Xd", b3)])
                for g in range(2):
                    K.op("pe", lambda: nc.tensor.matmul(pG[:, g, :], lhsT=XB[:, 4 + g, tok], rhs=XB[:, 6 + g, tok], start=True, stop=True),
                         r=["XB"], w=["pG"])
                K.op("dve", lambda: nc.vector.tensor_tensor(out=GmT[b3][:], in0=pG[:], in1=bcast(self.mls_f, [128, 2, 128], 1), op=ALU.mult),
                     r=["pG", "cf"], w=[("GmT", b3)])
                for hh in range(2):
                    e = (c * 2 + hh) % 6
                    for h4 in range(4):
                        h = hh * 4 + h4
                        i = (c % 2) * 8 + h
                        K.op("pe", lambda: nc.tensor.matmul(pSeg[hh][:, h4, :], lhsT=lh[i][:], rhs=self.U_f, start=True, stop=True),
                             r=[("lh", i), "cf"], w=[("pSeg", hh)])
                    K.op("act", lambda: nc.scalar.activation(out=eL[e][:], in_=pSeg[hh][:], func=AF.Exp), r=[("pSeg", hh)], w=[("eL", e)])
                    K.op("pool", lambda: nc.gpsimd.tensor_tensor(out=scT[e][:], in0=eL[e][:], in1=bcast(GmT[b3][:, hh, :], [128, 4, 128], 1), op=ALU.mult),
                         r=[("eL", e), ("GmT", b3)], w=[("scT", e)])

            def stage_lh(c):
                for h in range(8):
                    i = (c % 2) * 8 + h
                    K.op("dve", lambda: nc.vector.tensor_scalar(out=lh[i][:], in0=self.SL_f, scalar1=A[:, c, h:h + 1], scalar2=None, op0=ALU.mult),
                         r=["A", "cf"], w=[("lh", i)])

            def stage_b(c):
                b = c % 2
                b3 = c % 3
                tok = slice(c * 128, (c + 1) * 128)
                for h in range(8):
                    e = (c * 2 + h // 4) % 6
                    K.op("pe", lambda: nc.tensor.matmul(pYd[:, h, :], lhsT=scT[e][:, h % 4, :], rhs=X[b3][:, h, :], start=True, stop=True),
                         r=[("scT", e), ("X", b3)], w=["pYd"])
                if c > 0:
                    for h in range(8):
                        K.op("pe", lambda: nc.tensor.matmul(pYo[:, h, :], lhsT=XB[:, 6 + h // 4, tok], rhs=hbf[b][:, h, :], start=True, stop=True),
                             r=["XB", ("hbf", b)], w=["pYo"])
                    K.op("dve", lambda: nc.vector.tensor_tensor(out=yo[b][:], in0=pYo[:], in1=bcast(ecum[:, c, :], [128, 8, 64], 2), op=ALU.mult),
                         r=["pYo", "ecum"], w=[("yo", b)])
                    K.op("dve", lambda: nc.vector.tensor_tensor(out=yy[b][:], in0=pYd[:], in1=yo[b][:], op=ALU.add),
                         r=["pYd", ("yo", b)], w=[("yy", b)])
                else:
                    K.op("dve", lambda: nc.vector.tensor_copy(out=yy[b][:], in_=pYd[:]), r=["pYd"], w=[("yy", b)])
                K.op("pool", lambda: nc.gpsimd.tensor_tensor(out=td[b][:], in0=xh[b3][:], in1=bcast(d_bc, [128, 8, 64], 2), op=ALU.mult),
                     r=[("xh", b3), "prm"], w=[("td", b)])
                K.op("pool", lambda: nc.gpsimd.tensor_tensor(out=yy[b][:], in0=yy[b][:], in1=td[b][:], op=ALU.add),
                     r=[("yy", b), ("td", b)], w=[("yy", b)])
                yf = yy[b][:].rearrange("p h c -> p (h c)")
                K.op("dve", lambda: nc.vector.tensor_tensor(out=yf, in0=yf, in1=zt[b3][:], op=ALU.mult),
                     r=[("yy", b), ("zt", b3)], w=[("yy", b)])
                for g in range(2):
                    K.op("act", lambda: nc.scalar.activation(out=junk[:], in_=yf[:, g * 256:(g + 1) * 256], func=AF.Square, accum_out=ss[b][:, g:g + 1]),
                         r=[("yy", b)], w=[("ss", b, g), "junk"])
                K.op("dve", lambda: nc.vector.tensor_scalar(out=ss[b][:], in0=ss[b][:], scalar1=1.0 / 256.0, scalar2=RMS_EPS, op0=ALU.mult, op1=ALU.add),
                     r=[("ss", b, 0), ("ss", b, 1)], w=[("ss", b, 0), ("ss", b, 1)])
                K.op("act", lambda: nc.scalar.activation(out=ss[b][:], in_=ss[b][:], func=AF.Ln),
                     r=[("ss", b, 0), ("ss", b, 1)], w=[("ss", b, 0), ("ss", b, 1)])
                K.op("act", lambda: nc.scalar.activation(out=ss[b][:], in_=ss[b][:], func=AF.Exp, scale=-0.5),
                     r=[("ss", b, 0), ("ss", b, 1)], w=[("ss", b, 0), ("ss", b, 1)])
                for g in range(2):
                    K.op("dve", lambda: nc.vector.scalar_tensor_tensor(out=yn[b][:, g * 256:(g + 1) * 256], in0=yf[:, g * 256:(g + 1) * 256],
                                                                        scalar=ss[b][:, g:g + 1], in1=ng_bc[:, g * 256:(g + 1) * 256],
                                                                        op0=ALU.mult, op1=ALU.mult),
                         r=[("yy", b), ("ss", b, 0), ("ss", b, 1), "prm"], w=[("yn", b)])

            def stage_t(c):
                b = c % 2
                b3 = c % 3
                for j in range(4):
                    K.op("pe", lambda: nc.tensor.transpose(tpy[:, j, :], yn[b][:, j * 128:(j + 1) * 128], self.ident), r=[("yn", b), "cbf"], w=["tpy"])
                g4 = (c // 4) % 2
                K.op("act", lambda: nc.scalar.copy(out=sdT[g4][:, :, (c % 4) * 128:(c % 4 + 1) * 128], in_=tpy[:]), r=["tpy"], w=[("sdT", g4)])
                if c % 4 == 3:
                    K.dma("act", self.mixT[4:8, :, (c // 4) * 512:(c // 4 + 1) * 512].rearrange("c p t -> p c t"), sdT[g4][:],
                          r=[("sdT", g4)], w=[("mixT", 4, c // 4)])

            def stage_s(c):
                b = c % 2
                b3 = c % 3
                if c < NT - 1:
                    for h in range(8):
                        K.op("pe", lambda: nc.tensor.matmul(pSt[:, h, :], lhsT=Bt[b3][:, h // 4, :], rhs=Xd[b3][:, h, :], start=True, stop=True),
                             r=[("Bt", b3), ("Xd", b3)], w=["pSt"])
                    if c == 0:
                        K.op("dve", lambda: nc.vector.tensor_copy(out=hf[:], in_=pSt[:]), r=["pSt"], w=["hf"])
                    else:
                        K.op("pool", lambda: nc.gpsimd.tensor_tensor(out=hf[:], in0=hf[:], in1=bcast(cdec[:, c, :], [128, 8, 64], 2), op=ALU.mult),
                             r=["hf", "cdec"], w=["hf"])
                        K.op("dve", lambda: nc.vector.tensor_tensor(out=hf[:], in0=hf[:], in1=pSt[:], op=ALU.add), r=["hf", "pSt"], w=["hf"])
                    K.op("act", lambda: nc.scalar.copy(out=hbf[1 - b][:], in_=hf[:]), r=["hf"], w=[("hbf", 1 - b)])

            stage_lh(0)
            stage_lh(1)
            stage_a(0)
            stage_a(1)
            for c in range(NT):
                if c + 2 < NT:
                    stage_lh(c + 2)
                stage_s(c)
                stage_b(c)
                if c + 2 < NT:
                    stage_a(c + 2)
                if c >= 1:
                    stage_t(c - 1)
            stage_t(NT - 1)
            K.barrier()
        self.ssd_es.close()

    def layernorm(self, es, name, beng="pool", nbuf=3):
        nc, K = self.nc, self.K
        st = [self.sb(es, name + "st", [128, 2, 6], F32) for _ in range(nbuf)]
        mv = [self.sb(es, name + "mv", [128, 4], F32) for _ in range(nbuf)]
        bh = nc.gpsimd if beng == "pool" else nc.vector

        def stats(i, h, hk):
            mk = (name + "mv", i)
            for c in range(2):
                K.op("dve", lambda: nc.vector.bn_stats(out=st[i][:, c, :], in_=h[:, c * 512:(c + 1) * 512]), r=[hk], w=[(name + "st", i, c)])
            K.op("dve", lambda: nc.vector.bn_aggr(out=mv[i][:, 0:2], in_=st[i][:].rearrange("p a b -> p (a b)")),
                 r=[(name + "st", i, 0), (name + "st", i, 1)], w=[mk])
            K.op("dve", lambda: nc.vector.tensor_scalar(out=mv[i][:, 1:2], in0=mv[i][:, 1:2], scalar1=LN_EPS, scalar2=None, op0=ALU.add), r=[mk], w=[mk])
            K.op("act", lambda: nc.scalar.activation(out=mv[i][:, 1:2], in_=mv[i][:, 1:2], func=AF.Ln), r=[mk], w=[mk])
            K.op("act", lambda: nc.scalar.activation(out=mv[i][:, 2:3], in_=mv[i][:, 1:2], func=AF.Exp, scale=-0.5), r=[mk], w=[mk])
            K.op("dve", lambda: nc.vector.tensor_scalar(out=mv[i][:, 3:4], in0=mv[i][:, 0:1], scalar1=mv[i][:, 2:3], scalar2=-1.0, op0=ALU.mult, op1=ALU.mult),
                 r=[mk], w=[mk])

        def apply(i, h, hk, out, ok, g_bc, b_bc, pk):
            mk = (name + "mv", i)
            K.op("act", lambda: nc.scalar.activation(out=h, in_=h, func=AF.Identity, bias=mv[i][:, 3:4], scale=mv[i][:, 2:3]), r=[hk, mk], w=[hk])
            K.op("dve", lambda: nc.vector.tensor_tensor(out=h, in0=h, in1=g_bc, op=ALU.mult), r=[hk, pk], w=[hk])
            K.op(beng, lambda: bh.tensor_tensor(out=out, in0=h, in1=b_bc, op=ALU.add), r=[hk, pk], w=[ok])
        return stats, apply

    def phase_outln(self, l, src, dst):
        nc, K = self.nc, self.K
        self.exp_es = contextlib.ExitStack()
        self.exp_pre = None
        if self.with_moe:
            ees = self.exp_es
            self.exp_pre = (self.sb(ees, "wgu0", [128, 8, 2 * D], BF16), self.sb(ees, "wdn0", [128, 8, D], BF16), self.sb(ees, "bdn0", [1, D], BF16))
        with contextlib.ExitStack() as es:
            wo = self.sb(es, "wo", [128, 12, D], BF16)
            for c in range(3):
                K.dma("pool", wo[:, c * 4:(c + 1) * 4, :], self.w_out[l, c * 512:(c + 1) * 512, :].rearrange("(c p) d -> p c d", p=128), w=["wo"])
            wr = self.sb(es, "wr", [128, 8, NE], BF16)
            K.dma("pool", wr[:], self.router_w[l].rearrange("(k p) e -> p k e", p=128), w=["wr"])
            if self.exp_pre is not None:
                w0, d0, b0 = self.exp_pre
                for c in range(2):
                    K.dma("pool", w0[:, :, c * D:(c + 1) * D], self.w_gu[l, 0, :, c * D:(c + 1) * D].rearrange("(k p) f -> p k f", p=128), w=[("wgu", 0)])
                K.dma("pool", d0[:], self.w_dn[l, 0].rearrange("(k p) f -> p k f", p=128), w=[("wdn", 0)])
                K.dma("pool", b0[:], self.b_dn[l, 0:1, :], w=[("bdn", 0)])
            lnp = self.sb(es, "lnp", [128, 2 * D + NE], F32)
            K.dma("sp", lnp[:, 0:D], self.bc[l, BC_L1G:BC_L1G + D].partition_broadcast(128), w=["lnp"])
            K.dma("sp", lnp[:, D:2 * D], self.bc[l, BC_L1B:BC_L1B + D].partition_broadcast(128), w=["lnp"])
            K.dma("sp", lnp[:, 2 * D:], self.bc[l, BC_RB:BC_RB + NE].partition_broadcast(128), w=["lnp"])
            g_bc, b_bc, rb_bc = lnp[:, 0:D], lnp[:, D:2 * D], lnp[:, 2 * D:]
            ecap = self.sb(es, "ecap", [128, NE], F32)
            K.op("dve", lambda: nc.vector.tensor_scalar(out=ecap[:], in0=self.iota_e, scalar1=float(CAP), scalar2=None, op0=ALU.mult), r=["cf"], w=["ecap"])
            cntb = self.sb(es, "cntb", [128, NE], F32)
            K.op("dve", lambda: nc.vector.memset(cntb[:], 0.0), w=["cntb"])
            ln_stats, ln_apply = self.layernorm(es, "ln1", beng="dve")
            NB = 3
            TB = 4
            NXB = 4 * TB
            mt = [self.sb(es, "mt", [128, 12, 512], BF16) for _ in range(2)]
            xr = [self.sb(es, "xr", [128, D], F32) for _ in range(NB)]
            hb = [self.sb(es, "hb", [128, D], F32) for _ in range(NB)]
            x1t = [self.sb(es, "x1t", [128, D], F32) for _ in range(NB)]
            x1b = [self.sb(es, "x1b", [128, D], BF16) for _ in range(NXB)]
            x1T = [self.sb(es, "x1T", [128, 8, 128], BF16) for _ in range(2)]
            pm = [self.ps(es, "pm", [128, 512], F32) for _ in range(4)]
            tpr = [self.ps(es, "tpr", [128, 8, 128], BF16) for _ in range(2)]
            plg = self.ps(es, "plg", [128, TB, NE], F32)
            ppos = self.ps(es, "ppos", [128, 2, TB, NE], F32)
            lg = self.sb(es, "lg", [128, TB, NE], F32)
            mx8 = self.sb(es, "mx8", [128, TB, 8], F32)
            ix8 = self.sb(es, "ix8", [128, TB, 8], U32)
            e4 = self.sb(es, "e4", [128, TB, 4], F32)
            rs = self.sb(es, "rs", [128, TB], F32)
            g4 = [self.sb(es, "g4", [128, TB, 4], F32) for _ in range(2)]
            idxf = self.sb(es, "idxf", [128, TB, 4], F32)
            oh = self.sb(es, "oh", [128, TB, 4, NE], F32)
            mskb = self.sb(es, "mskb", [128, TB, NE], BF16)
            cs = self.sb(es, "cs", [128, NE, TB], F32)
            inc = self.sb(es, "inc", [128, NE, TB], F32)
            slot = self.sb(es, "slot", [128, TB, NE], F32)
            tmp = self.sb(es, "tmp", [128, TB, 4, NE], F32)
            off = self.sb(es, "off", [128, TB, 4], F32)
            offi = [self.sb(es, "offi", [128, TB, 4], I32) for _ in range(2)]

            def stage_a(t):
                b = t % NB
                bx = t % NXB
                tok = slice(t * 128, (t + 1) * 128)
                g4i = (t // 4) % 2
                if t == 0:
                    K.dma("sp", mt[0][:], self.mixT[:, :, 0:512].rearrange("c p t -> p c t"), w=[("mt", 0)])
                    K.dma("sp", xr[0][:], src[0:128, :], w=[("xr", 0)])
                if t % 4 == 0 and t + 4 < NT:
                    gn = t // 4 + 1
                    K.dma("sp", mt[gn % 2][:], self.mixT[:, :, gn * 512:(gn + 1) * 512].rearrange("c p t -> p c t"), w=[("mt", gn % 2)])
                if t + 1 < NT:
                    K.dma("sp", xr[(t + 1) % NB][:], src[(t + 1) * 128:(t + 2) * 128, :], w=[("xr", (t + 1) % NB)])
                for half in range(2):
                    pi = (t % 2) * 2 + half
                    for mc in range(12):
                        K.op("pe", lambda: nc.tensor.matmul(pm[pi][:], lhsT=mt[g4i][:, mc, (t % 4) * 128:(t % 4 + 1) * 128],
                                                            rhs=wo[:, mc, half * 512:(half + 1) * 512], start=(mc == 0), stop=(mc == 11)),
                             r=[("mt", g4i), "wo"], w=[("pm", pi)])
                    K.op("dve", lambda: nc.vector.scalar_tensor_tensor(out=hb[b][:, half * 512:(half + 1) * 512], in0=xr[b][:, half * 512:(half + 1) * 512],
                                                                        scalar=ALPHA, in1=pm[pi][:], op0=ALU.mult, op1=ALU.add),
                         r=[("xr", b), ("pm", pi)], w=[("hb", b)])
                ln_stats(b, hb[b][:], ("hb", b))

            def stage_a2(t):
                b = t % NB
                bx = t % NXB
                tok = slice(t * 128, (t + 1) * 128)
                ln_apply(b, hb[b][:], ("hb", b), x1t[b][:], ("x1t", b), g_bc, b_bc, "lnp")
                K.dma("sp", self.x1[tok, :], x1t[b][:], r=[("x1t", b)], w=[("x1", t)])
                K.op("act", lambda: nc.scalar.copy(out=x1b[bx][:], in_=x1t[b][:]), r=[("x1t", b)], w=[("x1b", bx)])

            def stage_r(t):
                b = t % 2
                bx = t % NXB
                for kc in range(8):
                    K.op("pe", lambda: nc.tensor.transpose(tpr[b][:, kc, :], x1b[bx][:, kc * 128:(kc + 1) * 128], self.ident), r=[("x1b", bx), "cbf"], w=[("tpr", b)])
                K.op("act", lambda: nc.scalar.copy(out=x1T[b][:], in_=tpr[b][:]), r=[("tpr", b)], w=[("x1T", b)])
                for kc in range(8):
                    K.op("pe", lambda: nc.tensor.matmul(plg[:, t % TB, :], lhsT=x1T[b][:, kc, :], rhs=wr[:, kc, :], start=(kc == 0), stop=(kc == 7)),
                         r=[("x1T", b), "wr"], w=["plg"])

            def stage_b(tb):
                gb = tb % 2
                bc4 = lambda ap: ap.unsqueeze(3).broadcast_to([128, TB, 4, NE])
                K.op("dve", lambda: nc.vector.tensor_tensor(out=lg[:], in0=plg[:], in1=rb_bc.unsqueeze(1).broadcast_to([128, TB, NE]), op=ALU.add),
                     r=["plg", "lnp"], w=["lg"])
                for i in range(TB):
                    K.op("dve", lambda: nc.vector.max(out=mx8[:, i, :], in_=lg[:, i, :]), r=["lg"], w=[("mx8", i)])
                yield
                for i in range(TB):
                    K.op("dve", lambda: nc.vector.max_index(out=ix8[:, i, :], in_max=mx8[:, i, :], in_values=lg[:, i, :]), r=["lg", ("mx8", i)], w=[("ix8", i)])
                yield
                allmx = [("mx8", i) for i in range(TB)]
                allix = [("ix8", i) for i in range(TB)]
                K.op("dve", lambda: nc.vector.tensor_tensor(out=e4[:], in0=mx8[:, :, 0:4], in1=mx8[:, :, 0:1].broadcast_to([128, TB, 4]), op=ALU.subtract),
                     r=allmx, w=["e4"])
                K.op("act", lambda: nc.scalar.activation(out=e4[:], in_=e4[:], func=AF.Exp), r=["e4"], w=["e4"])
                K.op("dve", lambda: nc.vector.tensor_reduce(out=rs[:], in_=e4[:], axis=AX.X, op=ALU.add), r=["e4"], w=["rs"])
                K.op("dve", lambda: nc.vector.reciprocal(out=rs[:], in_=rs[:]), r=["rs"], w=["rs"])
                K.op("dve", lambda: nc.vector.tensor_tensor(out=g4[gb][:], in0=e4[:], in1=rs[:].unsqueeze(2).broadcast_to([128, TB, 4]), op=ALU.mult),
                     r=["e4", "rs"], w=[("g4", gb)])
                K.dma("act", self.gate_s[tb * TB * 128:(tb + 1) * TB * 128, :].rearrange("(t p) k -> p t k", p=128), g4[gb][:], r=[("g4", gb)], w=[("gate_s", tb)])
                yield
                K.op("dve", lambda: nc.vector.tensor_copy(out=idxf[:], in_=ix8[:, :, 0:4]), r=allix, w=["idxf"])
                K.op("dve", lambda: nc.vector.tensor_tensor(out=oh[:], in0=self.iota_e.unsqueeze(1).unsqueeze(1).broadcast_to([128, TB, 4, NE]),
                                                            in1=bc4(idxf[:]), op=ALU.is_equal), r=["idxf", "cf"], w=["oh"])
                with nc.allow_low_precision(reason="0/1 one-hot sums are exact in bf16"):
                    K.op("dve", lambda: nc.vector.tensor_reduce(out=mskb[:], in_=oh[:].rearrange("p t k e -> p t e k"), axis=AX.X, op=ALU.add), r=["oh"], w=["mskb"])
                yield
                mflat = mskb[:].rearrange("p t e -> p (t e)")
                K.op("pe", lambda: nc.tensor.matmul(ppos[:, 0, :, :].rearrange("p t e -> p (t e)"), lhsT=self.sl_bf, rhs=mflat, start=True, stop=True),
                     r=["mskb", "cbf"], w=["ppos"])
                K.op("pe", lambda: nc.tensor.matmul(ppos[:, 1, :, :].rearrange("p t e -> p (t e)"), lhsT=self.ones_bf, rhs=mflat, start=True, stop=True),
                     r=["mskb", "cbf"], w=["ppos"])
                K.op("dve", lambda: nc.vector.tensor_copy(out=cs[:], in_=ppos[:, 1, :, :].rearrange("p t e -> p e t")), r=["ppos"], w=["cs"])
                K.op("dve", lambda: nc.vector.tensor_tensor_scan(out=inc[:].rearrange("p e t -> p (e t)"), data0=self.zr_f,
                                                                  data1=cs[:].rearrange("p e t -> p (e t)"), initial=0.0, op0=ALU.mult, op1=ALU.add),
                     r=["cs", "cf"], w=["inc"])
                yield
                K.op("dve", lambda: nc.vector.tensor_tensor(out=slot[:], in0=ppos[:, 0, :, :], in1=inc[:].rearrange("p e t -> p t e"), op=ALU.add),
                     r=["ppos", "inc"], w=["slot"])
                K.op("dve", lambda: nc.vector.tensor_tensor(out=slot[:], in0=slot[:], in1=cs[:].rearrange("p e t -> p t e"), op=ALU.subtract),
                     r=["slot", "cs"], w=["slot"])
                K.op("dve", lambda: nc.vector.tensor_tensor(out=slot[:], in0=slot[:], in1=cntb[:].unsqueeze(1).broadcast_to([128, TB, NE]), op=ALU.add),
                     r=["slot", "cntb"], w=["slot"])
                K.op("dve", lambda: nc.vector.tensor_scalar(out=slot[:], in0=slot[:], scalar1=float(CAP - 1), scalar2=None, op0=ALU.min),
                     r=["slot"], w=["slot"])
                K.op("dve", lambda: nc.vector.tensor_tensor(out=slot[:], in0=slot[:], in1=ecap[:].unsqueeze(1).broadcast_to([128, TB, NE]), op=ALU.add),
                     r=["slot", "ecap"], w=["slot"])
                K.op("dve", lambda: nc.vector.tensor_tensor(out=cntb[:], in0=cntb[:], in1=inc[:, :, TB - 1], op=ALU.add), r=["cntb", "inc", "slot"], w=["cntb"])
                yield
                K.op("dve", lambda: nc.vector.tensor_tensor(out=tmp[:], in0=oh[:], in1=slot[:].unsqueeze(2).broadcast_to([128, TB, 4, NE]), op=ALU.mult),
                     r=["oh", "slot"], w=["tmp"])
                K.op("dve", lambda: nc.vector.tensor_reduce(out=off[:], in_=tmp[:], axis=AX.X, op=ALU.add), r=["tmp"], w=["off"])
                K.op("dve", lambda: nc.vector.tensor_copy(out=offi[gb][:], in_=off[:]), r=["off"], w=[("offi", gb)])
                K.dma("act", self.offs[tb * TB * 128:(tb + 1) * TB * 128, :].rearrange("(t p) k -> p t k", p=128), offi[gb][:], r=[("offi", gb)], w=[("offs", tb)])
                yield
                for i in range(TB):
                    bx = (tb * TB + i) % NXB
                    for k in range(4):
                        K.dma("pool", self.xg[:, :], x1b[bx][:], r=[("x1b", bx), ("offi", gb)], w=["xg"],
                              indirect=dict(out_offset=bass.IndirectOffsetOnAxis(offi[gb][:, i, k:k + 1], 0), in_offset=None))

            pending = []
            for t in range(NT + 2):
                if 2 <= t <= NT + 1:
                    stage_r(t - 2)
                    if (t - 1) % TB == 0:
                        for _ in stage_b((t - 1) // TB - 1):
                            pass
                if 1 <= t <= NT:
                    stage_a2(t - 1)
                for g in list(pending):
                    try:
                        next(g)
                    except StopIteration:
                        pending.remove(g)
                if t < NT:
                    stage_a(t)
            for g in pending:
                for _ in g:
                    pass
            K.barrier()

    def phase_route(self, l, src, dst):
        pass

    def phase_experts(self, l, src, dst):
        nc, K = self.nc, self.K
        NCG, CGW = 2, CAP // 2
        NST = CAP // 128
        with contextlib.ExitStack() as es:
            bgu = self.sb(es, "bgu", [128, NE * 16], F32)
            K.dma("sp", bgu[:], self.pp[l, :, PP_BGU:PP_BGU + NE * 16], w=["bgu"])
            wgu = [self.exp_pre[0], self.sb(es, "wgu", [128, 8, 2 * D], BF16)]
            wdn = [self.exp_pre[1], self.sb(es, "wdn", [128, 8, D], BF16)]
            bdn = [self.exp_pre[2], self.sb(es, "bdn", [1, D], BF16)]
            xgt = [self.sb(es, "xgt", [128, XGW], BF16) for _ in range(CAP // 128)]
            xgT = [self.sb(es, "xgT", [128, 8, CAP], BF16) for _ in range(2)]
            hT = [self.sb(es, "hT", [128, 8, CAP], BF16) for _ in range(2)]
            g1 = [self.sb(es, "g1", [128, CGW], F32) for _ in range(2)]
            sg = [self.sb(es, "sg", [128, CGW], F32) for _ in range(2)]
            u1 = [self.sb(es, "u1", [128, CGW], F32) for _ in range(2)]
            yt = [self.sb(es, "yt", [128, D], F32) for _ in range(2)]
            tpx = [self.ps(es, "tpx", [128, 8, 128], BF16) for _ in range(2)]
            pg = [self.ps(es, "pg", [128, 512], F32) for _ in range(2)]
            pu = [self.ps(es, "pu", [128, 512], F32) for _ in range(2)]
            py = [self.ps(es, "py", [128, 512], F32) for _ in range(2)]
            cnt = {"xi": 0, "ei": 0, "yi": 0, "ti": 0}

            def prep_loads(e):
                wb = e % 2
                if e > 0:
                    for c in range(2):
                        K.dma("pool", wgu[wb][:, :, c * D:(c + 1) * D], self.w_gu[l, e, :, c * D:(c + 1) * D].rearrange("(k p) f -> p k f", p=128), w=[("wgu", wb)])
                    K.dma("pool", wdn[wb][:], self.w_dn[l, e].rearrange("(k p) f -> p k f", p=128), w=[("wdn", wb)])
                    K.dma("pool", bdn[wb][:], self.b_dn[l, e:e + 1, :], w=[("bdn", wb)])
                for i in range(NST):
                    r0 = e * CAP + i * 128
                    K.dma("sp", xgt[i][:], self.xg[r0:r0 + 128, :], w=[("xgt", i)])

            def prep_T(e):
                wb = e % 2
                for i in range(NST):
                    x_ = xgt[i]
                    xk = ("xgt", i)
                    ti = cnt["ti"] % 2
                    cnt["ti"] += 1
                    for kc in range(8):
                        K.op("pe", lambda: nc.tensor.transpose(tpx[ti][:, kc, :], x_[:, kc * 128:(kc + 1) * 128], self.ident), r=[xk, "cbf"], w=[("tpx", ti)])
                    K.op("act", lambda: nc.scalar.copy(out=xgT[wb][:, :, i * 128:(i + 1) * 128], in_=tpx[ti][:]), r=[("tpx", ti)], w=[("xgT", wb)])

            def gateup(e):
                wb = e % 2
                for cg in range(NCG):
                    cs = slice(cg * CGW, (cg + 1) * CGW)
                    for j in range(8):
                        p = cnt["ei"] % 2
                        cnt["ei"] += 1
                        for kc in range(8):
                            K.op("pe", lambda: nc.tensor.matmul(pg[p][:, 0:CGW], lhsT=wgu[wb][:, kc, j * 128:(j + 1) * 128], rhs=xgT[wb][:, kc, cs],
                                                                start=(kc == 0), stop=(kc == 7)), r=[("wgu", wb), ("xgT", wb)], w=[("pg", p)])
                        for kc in range(8):
                            K.op("pe", lambda: nc.tensor.matmul(pu[p][:, 0:CGW], lhsT=wgu[wb][:, kc, D + j * 128:D + (j + 1) * 128], rhs=xgT[wb][:, kc, cs],
                                                                start=(kc == 0), stop=(kc == 7)), r=[("wgu", wb), ("xgT", wb)], w=[("pu", p)])
                        bg = bgu[:, e * 16 + j:e * 16 + j + 1]
                        bu = bgu[:, e * 16 + 8 + j:e * 16 + 8 + j + 1]
                        K.op("dve", lambda: nc.vector.tensor_scalar(out=g1[p][:], in0=pg[p][:, 0:CGW], scalar1=bg, scalar2=7.0, op0=ALU.add, op1=ALU.min),
                             r=[("pg", p), "bgu"], w=[("g1", p)])
                        K.op("act", lambda: nc.scalar.activation(out=sg[p][:], in_=g1[p][:], func=AF.Sigmoid, scale=1.702), r=[("g1", p)], w=[("sg", p)])
                        K.op("dve", lambda: nc.vector.tensor_scalar(out=u1[p][:], in0=pu[p][:, 0:CGW], scalar1=bu, scalar2=7.0, op0=ALU.add, op1=ALU.min),
                             r=[("pu", p), "bgu"], w=[("u1", p)])
                        K.op("dve", lambda: nc.vector.tensor_scalar(out=u1[p][:], in0=u1[p][:], scalar1=-7.0, scalar2=1.0, op0=ALU.max, op1=ALU.add),
                             r=[("u1", p)], w=[("u1", p)])
                        K.op("dve", lambda: nc.vector.tensor_tensor(out=g1[p][:], in0=g1[p][:], in1=sg[p][:], op=ALU.mult), r=[("g1", p), ("sg", p)], w=[("g1", p)])
                        K.op("dve", lambda: nc.vector.tensor_tensor(out=hT[wb][:, j, cs], in0=g1[p][:], in1=u1[p][:], op=ALU.mult),
                             r=[("g1", p), ("u1", p)], w=[("hT", wb)])

            def down(e):
                wb = e % 2
                for i in range(NST):
                    yi = cnt["yi"]
                    cnt["yi"] += 1
                    yb_ = yt[yi % 2]
                    yk = ("yt", yi % 2)
                    for half in range(2):
                        hs = slice(half * 512, (half + 1) * 512)
                        K.op("pe", lambda: nc.tensor.matmul(py[half][:], lhsT=self.ones_bf[0:1, :], rhs=bdn[wb][0:1, hs], start=True, stop=False),
                             r=[("bdn", wb), "cbf"], w=[("py", half)])
                        for fc in range(8):
                            K.op("pe", lambda: nc.tensor.matmul(py[half][:], lhsT=hT[wb][:, fc, i * 128:(i + 1) * 128], rhs=wdn[wb][:, fc, hs],
                                                                start=False, stop=(fc == 7)), r=[("hT", wb), ("wdn", wb)], w=[("py", half)])
                        if half == 0:
                            K.op("act", lambda: nc.scalar.copy(out=yb_[:, hs], in_=py[half][:]), r=[("py", half)], w=[yk])
                        else:
                            K.op("dve", lambda: nc.vector.tensor_copy(out=yb_[:, hs], in_=py[half][:]), r=[("py", half)], w=[yk])
                    r0 = e * CAP + i * 128
                    K.dma("sp", self.yb[r0:r0 + 128, :], yb_[:], r=[yk], w=[("yb", e, i)])

            prep_loads(0)
            prep_T(0)
            for e in range(NE):
                if e + 1 < NE:
                    prep_loads(e + 1)
                gateup(e)
                if e + 1 < NE:
                    prep_T(e + 1)
                down(e)
            K.barrier()
        self.exp_es.close()

    def phase_combine(self, l, src, dst):
        nc, K = self.nc, self.K
        with contextlib.ExitStack() as es:
            lnp = self.sb(es, "lnp2", [128, 2 * D], F32)
            K.dma("sp", lnp[:, 0:D], self.bc[l, BC_L2G:BC_L2G + D].partition_broadcast(128), w=["lnp2"])
            K.dma("sp", lnp[:, D:2 * D], self.bc[l, BC_L2B:BC_L2B + D].partition_broadcast(128), w=["lnp2"])
            ln_stats, ln_apply = self.layernorm(es, "ln2", beng="dve")
            NB = 3
            offi = [self.sb(es, "offi2", [128, 4], I32) for _ in range(NB)]
            gt = [self.sb(es, "gt", [128, 4], F32) for _ in range(NB)]
            yg = [self.sb(es, "yg", [128, 4, D], F32) for _ in range(NB)]
            x1t = [self.sb(es, "x1c", [128, D], F32) for _ in range(NB)]
            hb = [self.sb(es, "hb2", [128, D], F32) for _ in range(3)]
            ot = [self.sb(es, "ot", [128, D], F32) for _ in range(2)]
            def loads(t):
                b = t % NB
                tok = slice(t * 128, (t + 1) * 128)
                K.dma("sp", offi[b][:], self.offs[tok, :], w=[("offi2", b)])
                K.dma("sp", gt[b][:], self.gate_s[tok, :], w=[("gt", b)])
                K.dma("sp", x1t[b][:], self.x1[tok, :], w=[("x1c", b)])

            def gathers(t):
                b = t % NB
                for k in range(4):
                    K.dma("pool", yg[b][:, k, :], self.yb[:, :], r=[("offi2", b)], w=[("yg", b, k)],
                          indirect=dict(out_offset=None, in_offset=bass.IndirectOffsetOnAxis(offi[b][:, k:k + 1], 0)))

            def c1(t):
                b = t % NB
                K.op("act", lambda: nc.scalar.activation(out=hb[b][:], in_=yg[b][:, 0, :], func=AF.Copy, scale=gt[b][:, 0:1]),
                     r=[("yg", b, 0), ("gt", b)], w=[("hb2", b)])
                for k in range(1, 4):
                    K.op("dve", lambda: nc.vector.scalar_tensor_tensor(out=hb[b][:], in0=yg[b][:, k, :], scalar=gt[b][:, k:k + 1], in1=hb[b][:],
                                                                        op0=ALU.mult, op1=ALU.add),
                         r=[("yg", b, k), ("gt", b), ("hb2", b)], w=[("hb2", b)])
                K.op("dve", lambda: nc.vector.scalar_tensor_tensor(out=hb[b][:], in0=x1t[b][:], scalar=ALPHA, in1=hb[b][:], op0=ALU.mult, op1=ALU.add),
                     r=[("x1c", b), ("hb2", b)], w=[("hb2", b)])
                ln_stats(b, hb[b][:], ("hb2", b))

            def c2(t):
                b = t % NB
                b2 = t % 2
                tok = slice(t * 128, (t + 1) * 128)
                ln_apply(b, hb[b][:], ("hb2", b), ot[b2][:], ("ot", b2), lnp[:, 0:D], lnp[:, D:2 * D], "lnp2")
                K.dma("act", dst[tok, :], ot[b2][:], r=[("ot", b2)], w=[("dst", t)])

            loads(0)
            loads(1)
            gathers(0)
            for t in range(NT + 1):
                if t + 2 < NT:
                    loads(t + 2)
                if t + 1 < NT:
                    gathers(t + 1)
                if t < NT:
                    c1(t)
                if t >= 1:
                    c2(t - 1)
            K.barrier()


def host_consts():
    bf = ml_dtypes.bfloat16
    k = np.arange(128)[:, None]
    q = np.arange(128)[None, :]
    cb = np.zeros((128, 1024), np.float32)
    cb[:, 0:128] = np.eye(128)
    cb[:, 128:256] = (k >= q)
    cb[:, 256:384] = (k <= q)
    cb[:, 384:512] = 1.0
    cb[:, 512:640] = (k < q)
    cb[:, 640:768] = (k <= q)
    cb[:, 768:896] = (k >= q)
    cf = np.zeros((128, 1024), np.float32)
    cf[:, 0:128] = (k <= q)
    cf[:, 128:256] = (k > q)
    cf[:, 256:384] = 1.0
    cf[:, 384:512] = (q >= k)
    cf[:, 512:544] = np.arange(32)[None, :]
    zr = np.ones((32, 8), np.float32)
    zr[:, 0] = 0.0
    cf[:, 544:800] = zr.reshape(1, 256)
    zr4 = np.ones((32, 4), np.float32)
    zr4[:, 0] = 0.0
    cf[:, 800:928] = zr4.reshape(1, 128)
    half = 32
    inv = (10000.0 ** (-np.arange(half, dtype=np.float32) / half)).astype(np.float32)
    ang = np.arange(S, dtype=np.float32)[None, :] * inv[:, None]
    cos = np.cos(ang).astype(np.float32)
    sin = np.sin(ang).astype(np.float32)
    cos_t = np.concatenate([cos, cos, cos, cos], 0)
    sin_t = np.concatenate([-sin, sin, -sin, sin], 0)
    return cb.astype(bf), cf, np.ascontiguousarray(cos_t), np.ascontiguousarray(sin_t)


def host_layout(inp):
    f = lambda a: np.ascontiguousarray(np.asarray(a, dtype=np.float32))
    w_in = f(inp["w_in"])
    qk = w_in[:, :, 0:1024].reshape(L, D, 16, 2, 32)
    w_qkp = np.ascontiguousarray(qk[:, :, :, ::-1, :].reshape(L, D, 1024))
    pp = np.zeros((L, 128, PPW), np.float32)
    cw = f(inp["ssd_conv_w"])
    pp[:, :, PP_CW:PP_CW + 32] = cw.reshape(L, 4, 8, 128).transpose(0, 3, 2, 1).reshape(L, 128, 32)
    pp[:, :, PP_CB:PP_CB + 8] = f(inp["ssd_conv_b"]).reshape(L, 8, 128).transpose(0, 2, 1)
    scw = f(inp["sc_conv_w"])
    pp[:, :, PP_SCW:PP_SCW + 12] = scw.reshape(L, 3, 4, 128).transpose(0, 3, 2, 1).reshape(L, 128, 12)
    bgu = f(inp["exp_b_gu"])
    pp[:, :, PP_BGU:] = bgu.reshape(L, NE, 16, 128).transpose(0, 3, 1, 2).reshape(L, 128, NE * 16)
    bc = np.concatenate([f(inp["ssd_dt_bias"]), f(inp["ssd_a_log"]), f(inp["ssd_d"]), f(inp["ssd_norm_g"]),
                         f(inp["router_b"]), f(inp["ln1_g"]), f(inp["ln1_b"]), f(inp["ln2_g"]), f(inp["ln2_b"])], axis=1)
    assert bc.shape == (L, BCW)
    cb, cf, cos_t, sin_t = host_consts()
    shared = {
        "w_in": w_in, "w_qkp": w_qkp, "w_out": f(inp["w_out"]), "router_w": f(inp["router_w"]),
        "exp_w_gu": f(inp["exp_w_gu"]), "exp_w_down": f(inp["exp_w_down"]), "exp_b_down": f(inp["exp_b_down"]),
        "pp": pp, "bc": np.ascontiguousarray(bc), "cos_t": cos_t, "sin_t": sin_t, "cst_bf": cb, "cst_f": cf,
    }
    return shared


def kernel(**inputs):
    x = np.ascontiguousarray(np.asarray(inputs["x"], dtype=np.float32))
    shared = host_layout(inputs)
    nc = Builder().build()
    in_maps = [dict(shared, x=x[b]) for b in range(8)]
    res = run_bass_kernel_spmd(nc, in_maps, core_ids=list(range(8)))
    return np.stack([np.asarray(r["y"], dtype=np.float32) for r in res.results], axis=0)
```
